# Optimizing a Trainium2 kernel written in Bass

```python
import math
import jax, jax.numpy as jnp
from jax import lax
import numpy as np

D_MODEL = 1024
BATCH = 8
SEQ = 2048
DEPTH = 1
DEC_BATCH = 128
DEC_SEQ = 4
PAST_LEN = 2048
PAGE_SIZE = 128

HEAD_DIM = 64
HEADS_PER_GROUP = 8
ATTN_GROUPS = ((128, 1), (512, 4), (2048, 16))
N_ATTN_GROUPS = len(ATTN_GROUPS)
N_ATTN_HEADS = N_ATTN_GROUPS * HEADS_PER_GROUP
ATTN_WIDTH = N_ATTN_HEADS * HEAD_DIM
ATTN_OUT_WIDTH = HEADS_PER_GROUP * HEAD_DIM
ROT_DIM = HEAD_DIM // 4
ROPE_THETA = 500000.0
Q_BLOCK = 128

SSM_GROUP_CH = 16
SSM_WIDTH = D_MODEL // 2
N_SSM_GROUPS = SSM_WIDTH // SSM_GROUP_CH
SSM_STATE = 64
DT_MIN = 1e-3
DT_MAX = 1e-1

IN_WIDTH = 3 * ATTN_WIDTH + SSM_WIDTH + 2 * D_MODEL

N_EXPERT_GROUPS = 4
EXPERTS_PER_GROUP = 8
N_EXPERTS = N_EXPERT_GROUPS * EXPERTS_PER_GROUP
TOP_K_INNER = 2
D_EXPERT = D_MODEL // 4
MOE_BLOCK = 128

NORM_EPS = 1e-6

kernel_name = 'dilated_s5_hmoe_hybrid_step'


def rms_norm(x, g):
    xf = x.astype(jnp.float32)
    var = jnp.mean(xf * xf, axis=-1, keepdims=True)
    return (xf * lax.rsqrt(var + NORM_EPS) * g.astype(jnp.float32)).astype(x.dtype)


def partial_rope(x, pos):
    half = ROT_DIM // 2
    inv_freq = ROPE_THETA ** (-(jnp.arange(half, dtype=jnp.float32) / half))
    ang = pos.astype(jnp.float32)[:, None] * inv_freq[None, :]
    cos = jnp.cos(ang)[None, :, None, :]
    sin = jnp.sin(ang)[None, :, None, :]
    xr = x[..., :ROT_DIM].astype(jnp.float32)
    x1, x2 = xr[..., :half], xr[..., half:]
    rot = jnp.concatenate([x1 * cos - x2 * sin, x2 * cos + x1 * sin], axis=-1).astype(x.dtype)
    return jnp.concatenate([rot, x[..., ROT_DIM:]], axis=-1)


def dilated_block(q_blk, q_idx, k_all, v_all, dilation, n_keys):
    key_idx = q_idx[:, None] - dilation * jnp.arange(n_keys, dtype=jnp.int32)[None, :]
    valid = key_idx >= 0
    safe = jnp.maximum(key_idx, 0)
    kg = jnp.take(k_all, safe, axis=1)
    vg = jnp.take(v_all, safe, axis=1)
    s = jnp.einsum('bqhd,bqkhd->bhqk', q_blk, kg, preferred_element_type=jnp.float32) * (HEAD_DIM ** -0.5)
    s = jnp.where(valid[None, None], s, -jnp.inf)
    lse = jax.nn.logsumexp(s, axis=-1, keepdims=True)
    p = jnp.exp(s - lse)
    o = jnp.einsum('bhqk,bqkhd->bqhd', p, vg.astype(jnp.float32))
    return o, jnp.transpose(lse[..., 0], (0, 2, 1))


def dilated_attention(q, k_all, v_all, q_offset, dilation, n_keys):
    bsz, t_len, n_h, hd = q.shape
    if t_len <= Q_BLOCK or t_len % Q_BLOCK != 0:
        return dilated_block(q, q_offset + jnp.arange(t_len, dtype=jnp.int32), k_all, v_all, dilation, n_keys)
    n_blk = t_len // Q_BLOCK
    qb = jnp.transpose(q.reshape(bsz, n_blk, Q_BLOCK, n_h, hd), (1, 0, 2, 3, 4))

    def body(args):
        i, q_i = args
        idx = q_offset + i * Q_BLOCK + jnp.arange(Q_BLOCK, dtype=jnp.int32)
        return dilated_block(q_i, idx, k_all, v_all, dilation, n_keys)

    o, lse = lax.map(body, (jnp.arange(n_blk, dtype=jnp.int32), qb))
    o = jnp.transpose(o, (1, 0, 2, 3, 4)).reshape(bsz, t_len, n_h, hd)
    lse = jnp.transpose(lse, (1, 0, 2, 3)).reshape(bsz, t_len, n_h)
    return o, lse


def s5_scan(u, h0, ssm_log_dt, ssm_a_re, ssm_a_im, ssm_b_re, ssm_b_im, ssm_c_re, ssm_c_im, ssm_d):
    f32 = jnp.float32
    bsz, t_len, _ = u.shape
    uf = u.astype(f32).reshape(bsz, t_len, N_SSM_GROUPS, SSM_GROUP_CH)
    dt = jnp.exp(ssm_log_dt.astype(f32))[:, None]
    lam = lax.complex(ssm_a_re.astype(f32), ssm_a_im.astype(f32))
    lam_bar = jnp.exp(lam * dt)
    b_c = lax.complex(ssm_b_re.astype(f32), ssm_b_im.astype(f32))
    b_bar = ((lam_bar - 1.0) / lam)[:, :, None] * b_c
    c_c = lax.complex(ssm_c_re.astype(f32), ssm_c_im.astype(f32))
    bu = jnp.einsum('btgc,gpc->btgp', uf.astype(jnp.complex64), b_bar)
    a = jnp.broadcast_to(lam_bar, bu.shape)

    def combine(left, right):
        a_l, b_l = left
        a_r, b_r = right
        return a_l * a_r, a_r * b_l + b_r

    _, h = lax.associative_scan(combine, (a, bu), axis=1)
    if h0 is not None:
        h0c = lax.complex(h0[..., 0].astype(f32), h0[..., 1].astype(f32))
        steps = jnp.arange(1, t_len + 1, dtype=f32)[:, None, None]
        powers = jnp.exp((lam * dt)[None] * steps)
        h = h + powers[None] * h0c[:, None]
    y = jnp.real(jnp.einsum('btgp,gcp->btgc', h, c_c)) + ssm_d.astype(f32)[None, None] * uf
    h_last = h[:, -1]
    new_state = jnp.stack([jnp.real(h_last), jnp.imag(h_last)], axis=-1)
    return y.reshape(bsz, t_len, SSM_WIDTH).astype(u.dtype), new_state.astype(u.dtype)


def hier_moe(x, w_router_group, b_router_group, w_router_expert, b_router_expert, w_exp_gate, w_exp_up, w_exp_down):
    f32 = jnp.float32
    n_tok = x.shape[0]
    lg = jnp.dot(x, w_router_group, preferred_element_type=f32) + b_router_group.astype(f32)
    grp = jnp.argmax(lg, axis=-1).astype(jnp.int32)
    p_grp = jnp.take_along_axis(jax.nn.softmax(lg, axis=-1), grp[:, None], axis=-1)
    le = (jnp.dot(x, w_router_expert, preferred_element_type=f32) + b_router_expert.astype(f32)).reshape(n_tok, N_EXPERT_GROUPS, EXPERTS_PER_GROUP)
    le_grp = jnp.take_along_axis(le, grp[:, None, None], axis=1)[:, 0]
    top_v, top_i = lax.top_k(le_grp, TOP_K_INNER)
    comb_w = jax.nn.softmax(top_v, axis=-1) * p_grp
    expert_id = grp[:, None] * EXPERTS_PER_GROUP + top_i.astype(jnp.int32)
    n_assign = n_tok * TOP_K_INNER
    flat_e = expert_id.reshape(-1)
    flat_tok = jnp.repeat(jnp.arange(n_tok, dtype=jnp.int32), TOP_K_INNER)
    flat_w = comb_w.reshape(-1)
    order = jnp.argsort(flat_e)
    e_sorted = flat_e[order]
    counts = jnp.bincount(flat_e, length=N_EXPERTS).astype(jnp.int32)
    padded = ((counts + MOE_BLOCK - 1) // MOE_BLOCK) * MOE_BLOCK
    pad_end = jnp.cumsum(padded)
    pad_start = pad_end - padded
    raw_start = jnp.cumsum(counts) - counts
    rank = jnp.arange(n_assign, dtype=jnp.int32) - raw_start[e_sorted]
    slot = pad_start[e_sorted] + rank
    n_blocks = -(-n_assign // MOE_BLOCK) + N_EXPERTS
    n_slots = n_blocks * MOE_BLOCK
    slot_tok = jnp.zeros((n_slots,), jnp.int32).at[slot].set(flat_tok[order])
    slot_w = jnp.zeros((n_slots,), f32).at[slot].set(flat_w[order])
    block_start = jnp.arange(n_blocks, dtype=jnp.int32) * MOE_BLOCK
    block_e = jnp.clip(jnp.searchsorted(pad_end, block_start, side='right'), 0, N_EXPERTS - 1).astype(jnp.int32)
    xs = x[slot_tok].reshape(n_blocks, MOE_BLOCK, D_MODEL)

    def expert_block(args):
        xb, e = args
        hmid = jax.nn.silu(xb @ w_exp_gate[e]) * (xb @ w_exp_up[e])
        return hmid @ w_exp_down[e]

    yb = lax.map(expert_block, (xs, block_e))
    y = jax.ops.segment_sum(yb.reshape(n_slots, D_MODEL).astype(f32) * slot_w[:, None], slot_tok, num_segments=n_tok)
    return y.astype(x.dtype)


def trunk_layer(x, pos0, kv_bufs, ssm_h0, g_attn_norm, w_in, ssm_log_dt, ssm_a_re, ssm_a_im, ssm_b_re, ssm_b_im, ssm_c_re, ssm_c_im, ssm_d, w_glu, w_attn_branch, w_out, g_ffn_norm, w_router_group, b_router_group, w_router_expert, b_router_expert, w_exp_gate, w_exp_up, w_exp_down):
    bsz, t_len, _ = x.shape
    xn = rms_norm(x, g_attn_norm)
    proj = xn @ w_in
    q, k, v, u, gates = jnp.split(proj, [ATTN_WIDTH, 2 * ATTN_WIDTH, 3 * ATTN_WIDTH, 3 * ATTN_WIDTH + SSM_WIDTH], axis=-1)
    pos = pos0 + jnp.arange(t_len, dtype=jnp.int32)
    q = partial_rope(q.reshape(bsz, t_len, N_ATTN_HEADS, HEAD_DIM), pos)
    k = partial_rope(k.reshape(bsz, t_len, N_ATTN_HEADS, HEAD_DIM), pos)
    v = v.reshape(bsz, t_len, N_ATTN_HEADS, HEAD_DIM)
    outs, lses, new_rows = [], [], []
    for gi, (win, dil) in enumerate(ATTN_GROUPS):
        hs = slice(gi * HEADS_PER_GROUP, (gi + 1) * HEADS_PER_GROUP)
        q_g, k_g, v_g = q[:, :, hs], k[:, :, hs], v[:, :, hs]
        if kv_bufs is None:
            k_all, v_all, off = k_g, v_g, 0
            keep = min(win, t_len)
            new_rows.append(jnp.stack([k_g[:, t_len - keep:], v_g[:, t_len - keep:]], axis=2))
        else:
            buf = kv_bufs[gi]
            k_all = jnp.concatenate([buf[:, :, 0].astype(k_g.dtype), k_g], axis=1)
            v_all = jnp.concatenate([buf[:, :, 1].astype(v_g.dtype), v_g], axis=1)
            off = buf.shape[1]
            new_rows.append(jnp.stack([k_g, v_g], axis=2))
        o, lse = dilated_attention(q_g, k_all, v_all, off, dil, win // dil + 1)
        outs.append(o)
        lses.append(lse)
    wts = jax.nn.softmax(jnp.stack(lses, axis=0), axis=0)
    attn = jnp.einsum('gbth,gbthd->bthd', wts, jnp.stack(outs, axis=0)).astype(x.dtype)
    attn_out = attn.reshape(bsz, t_len, ATTN_OUT_WIDTH) @ w_attn_branch
    y_ssm, ssm_new = s5_scan(u, ssm_h0, ssm_log_dt, ssm_a_re, ssm_a_im, ssm_b_re, ssm_b_im, ssm_c_re, ssm_c_im, ssm_d)
    glu = jax.nn.gelu(y_ssm) @ w_glu
    ssm_out = glu[..., :D_MODEL] * jax.nn.sigmoid(glu[..., D_MODEL:])
    gate_attn, gate_ssm = jnp.split(gates, 2, axis=-1)
    merged = jax.nn.sigmoid(gate_attn) * attn_out + jax.nn.sigmoid(gate_ssm) * ssm_out
    h = x + merged @ w_out
    hn = rms_norm(h, g_ffn_norm).reshape(bsz * t_len, D_MODEL)
    h = h + hier_moe(hn, w_router_group, b_router_group, w_router_expert, b_router_expert, w_exp_gate, w_exp_up, w_exp_down).reshape(bsz, t_len, D_MODEL)
    return h, new_rows, ssm_new


def setup_inputs(seed: int = 0) -> dict:
    key = jax.random.key(seed)
    ks = jax.random.split(key, 32)
    f32 = jnp.float32

    def nrm(k, shape, scale):
        return jax.random.normal(k, shape, f32) * scale

    wb = [min(w, PAST_LEN) for (w, _) in ATTN_GROUPS]
    kv_tail = (2, HEADS_PER_GROUP, HEAD_DIM)
    n_idx = jnp.arange(SSM_STATE, dtype=f32)
    ssm_shape = (DEPTH, N_SSM_GROUPS, SSM_STATE)
    return {
        'x_prompt': nrm(ks[0], (BATCH, SEQ, D_MODEL), 1.0),
        'x_sample': nrm(ks[1], (DEC_BATCH, DEC_SEQ, D_MODEL), 1.0),
        'cache_kv_w128': nrm(ks[2], (DEPTH, DEC_BATCH, wb[0]) + kv_tail, 1.0),
        'cache_kv_w512': nrm(ks[3], (DEPTH, DEC_BATCH, wb[1]) + kv_tail, 1.0),
        'cache_kv_w2048': nrm(ks[4], (DEPTH, DEC_BATCH, wb[2]) + kv_tail, 1.0),
        'state_ssm': nrm(ks[5], (DEPTH, DEC_BATCH, N_SSM_GROUPS, SSM_STATE, 2), 0.5),
        'g_attn_norm': 1.0 + nrm(ks[6], (DEPTH, D_MODEL), 0.02),
        'w_in': nrm(ks[7], (DEPTH, D_MODEL, IN_WIDTH), D_MODEL ** -0.5),
        'ssm_log_dt': jax.random.uniform(ks[8], (DEPTH, N_SSM_GROUPS), f32, math.log(DT_MIN), math.log(DT_MAX)),
        'ssm_a_re': -0.5 + nrm(ks[9], ssm_shape, 0.01),
        'ssm_a_im': math.pi * n_idx + nrm(ks[10], ssm_shape, 0.01),
        'ssm_b_re': nrm(ks[11], (DEPTH, N_SSM_GROUPS, SSM_STATE, SSM_GROUP_CH), (2 * SSM_GROUP_CH) ** -0.5),
        'ssm_b_im': nrm(ks[12], (DEPTH, N_SSM_GROUPS, SSM_STATE, SSM_GROUP_CH), (2 * SSM_GROUP_CH) ** -0.5),
        'ssm_c_re': nrm(ks[13], (DEPTH, N_SSM_GROUPS, SSM_GROUP_CH, SSM_STATE), 0.5),
        'ssm_c_im': nrm(ks[14], (DEPTH, N_SSM_GROUPS, SSM_GROUP_CH, SSM_STATE), 0.5),
        'ssm_d': nrm(ks[15], (DEPTH, N_SSM_GROUPS, SSM_GROUP_CH), 1.0),
        'w_glu': nrm(ks[16], (DEPTH, SSM_WIDTH, 2 * D_MODEL), SSM_WIDTH ** -0.5),
        'w_attn_branch': nrm(ks[17], (DEPTH, ATTN_OUT_WIDTH, D_MODEL), ATTN_OUT_WIDTH ** -0.5),
        'w_out': nrm(ks[18], (DEPTH, D_MODEL, D_MODEL), D_MODEL ** -0.5),
        'g_ffn_norm': 1.0 + nrm(ks[19], (DEPTH, D_MODEL), 0.02),
        'w_router_group': nrm(ks[20], (DEPTH, D_MODEL, N_EXPERT_GROUPS), D_MODEL ** -0.5),
        'b_router_group': nrm(ks[21], (DEPTH, N_EXPERT_GROUPS), 0.01),
        'w_router_expert': nrm(ks[22], (DEPTH, D_MODEL, N_EXPERTS), D_MODEL ** -0.5),
        'b_router_expert': nrm(ks[23], (DEPTH, N_EXPERTS), 0.01),
        'w_exp_gate': nrm(ks[24], (DEPTH, N_EXPERTS, D_MODEL, D_EXPERT), D_MODEL ** -0.5),
        'w_exp_up': nrm(ks[25], (DEPTH, N_EXPERTS, D_MODEL, D_EXPERT), D_MODEL ** -0.5),
        'w_exp_down': nrm(ks[26], (DEPTH, N_EXPERTS, D_EXPERT, D_MODEL), D_EXPERT ** -0.5),
        'g_final': 1.0 + nrm(ks[27], (D_MODEL,), 0.02),
    }


def reference(x_prompt, x_sample, cache_kv_w128, cache_kv_w512, cache_kv_w2048, state_ssm, g_attn_norm, w_in, ssm_log_dt, ssm_a_re, ssm_a_im, ssm_b_re, ssm_b_im, ssm_c_re, ssm_c_im, ssm_d, w_glu, w_attn_branch, w_out, g_ffn_norm, w_router_group, b_router_group, w_router_expert, b_router_expert, w_exp_gate, w_exp_up, w_exp_down, g_final):
    h_p, h_s = x_prompt, x_sample
    rows_p = [[] for _ in ATTN_GROUPS]
    rows_s = [[] for _ in ATTN_GROUPS]
    st_p, st_s = [], []
    for layer in range(DEPTH):
        params = dict(g_attn_norm=g_attn_norm[layer], w_in=w_in[layer], ssm_log_dt=ssm_log_dt[layer], ssm_a_re=ssm_a_re[layer], ssm_a_im=ssm_a_im[layer], ssm_b_re=ssm_b_re[layer], ssm_b_im=ssm_b_im[layer], ssm_c_re=ssm_c_re[layer], ssm_c_im=ssm_c_im[layer], ssm_d=ssm_d[layer], w_glu=w_glu[layer], w_attn_branch=w_attn_branch[layer], w_out=w_out[layer], g_ffn_norm=g_ffn_norm[layer], w_router_group=w_router_group[layer], b_router_group=b_router_group[layer], w_router_expert=w_router_expert[layer], b_router_expert=b_router_expert[layer], w_exp_gate=w_exp_gate[layer], w_exp_up=w_exp_up[layer], w_exp_down=w_exp_down[layer])
        h_p, kv_p, s_p = trunk_layer(h_p, 0, None, None, **params)
        bufs = (cache_kv_w128[layer], cache_kv_w512[layer], cache_kv_w2048[layer])
        h_s, kv_s, s_s = trunk_layer(h_s, PAST_LEN, bufs, state_ssm[layer], **params)
        for gi in range(N_ATTN_GROUPS):
            rows_p[gi].append(kv_p[gi])
            rows_s[gi].append(kv_s[gi])
        st_p.append(s_p)
        st_s.append(s_s)
    y_prompt = rms_norm(h_p, g_final)
    y_sample = rms_norm(h_s, g_final)
    kv_w128_prompt = jnp.stack(rows_p[0], axis=0)
    kv_w128_sample = jnp.stack(rows_s[0], axis=0)
    kv_w512_prompt = jnp.stack(rows_p[1], axis=0)
    kv_w512_sample = jnp.stack(rows_s[1], axis=0)
    kv_w2048_prompt = jnp.stack(rows_p[2], axis=0)
    kv_w2048_sample = jnp.stack(rows_s[2], axis=0)
    ssm_state_prompt = jnp.stack(st_p, axis=0)
    ssm_state_sample = jnp.stack(st_s, axis=0)
    return (y_prompt, y_sample, kv_w128_prompt, kv_w128_sample, kv_w512_prompt, kv_w512_sample, kv_w2048_prompt, kv_w2048_sample, ssm_state_prompt, ssm_state_sample)
```

```python
import numpy as np
from contextlib import ExitStack
import concourse.bass as bass
import concourse.mybir as mybir
from concourse.alu_op_type import AluOpType as ALU
from concourse.bass_utils import run_bass_kernel_spmd

F32 = mybir.dt.float32
BF16 = mybir.dt.bfloat16
I32 = mybir.dt.int32
AF = mybir.ActivationFunctionType
AX = mybir.AxisListType

NT = 2112
NPT = 2048
NS = 64
D = 1024
GROUPS = ((128, 1), (512, 4), (2048, 16))


class _Op:
    __slots__ = ("eng", "fn", "deps", "dma", "signal", "cnt", "sem", "val", "done", "isbar", "g")


class Sched:
    def __init__(self, nc, stack):
        self.nc = nc
        self.stack = stack
        self.eng = {"pe": nc.tensor, "act": nc.scalar, "dve": nc.vector, "pool": nc.gpsimd, "sp": nc.sync}
        self.ops = {e: [] for e in self.eng}
        self.state = {}
        self.dsem = {}
        self.last_dma = {}
        self.gcount = 0

    @staticmethod
    def _key(a):
        if isinstance(a, tuple):
            return a
        if isinstance(a, str):
            return (a, None)
        if hasattr(a, "tensor"):
            return (a.tensor.name, None)
        return (a.name, None)

    def _entries(self, key, create=True):
        name, sub = key
        d = self.state.setdefault(name, {})
        if create and sub not in d:
            d[sub] = [None, []]
        return [(s, e) for s, e in d.items() if s == sub or s is None or sub is None]

    def add(self, eng, fn, ins=(), outs=(), dma=False, semkey=None):
        op = _Op()
        op.eng, op.fn, op.dma, op.signal, op.deps = eng, fn, dma, False, set()
        op.cnt = op.val = 0
        op.sem = None
        for a in ins:
            if a is None:
                continue
            k = self._key(a)
            for s, e in self._entries(k):
                if e[0] is not None:
                    op.deps.add(e[0])
            self.state[k[0]][k[1]][1].append(op)
        for a in outs:
            if a is None:
                continue
            k = self._key(a)
            for s, e in self._entries(k):
                if e[0] is not None:
                    op.deps.add(e[0])
                for r in e[1]:
                    op.deps.add(r)
                if s == k[1] or k[1] is None:
                    e[0] = op
                    e[1] = []
            d = self.state[k[0]]
            for s in list(d.keys()):
                if s == k[1] or k[1] is None:
                    d[s] = [op, []]
        op.deps.discard(op)
        if dma:
            sk = self._key(semkey)
            if sk not in self.dsem:
                self.dsem[sk] = [self.stack.enter_context(self.nc.semaphore("d%d" % len(self.dsem))), 0]
            ent = self.dsem[sk]
            ent[1] += 16
            op.sem, op.val = ent[0], ent[1]
            self.last_dma[sk] = op
        op.g = self.gcount
        self.gcount += 1
        self.ops[eng].append(op)
        return op

    def interleave(self, g0, g1, g2):
        la, lb = g1 - g0, g2 - g1
        if la == 0 or lb == 0:
            return

        def pos(op):
            if op.g < g1:
                return (op.g - g0) * (la + lb) / la
            return (op.g - g1) * (la + lb) / lb + 0.5
        for e, lst in self.ops.items():
            head = [o for o in lst if getattr(o, "isbar", False) or o.g < g0]
            tail = [o for o in lst if not getattr(o, "isbar", False) and o.g >= g0]
            tail.sort(key=pos)
            self.ops[e] = head + tail

    def barrier(self, full=True):
        deps = set()
        for e, lst in self.ops.items():
            for op in reversed(lst):
                if not op.dma and not getattr(op, "isbar", False):
                    deps.add(op)
                    break
        bg = getattr(self, "bg_keys", set())
        deps |= set(op for sk, op in self.last_dma.items() if full or sk not in bg)
        for e in self.ops:
            op = _Op()
            op.eng, op.fn, op.dma, op.signal, op.deps = e, None, False, False, set(deps)
            op.cnt = op.val = 0
            op.sem = None
            op.isbar = True
            op.g = self.gcount
            self.ops[e].append(op)
        self.emit(final=False)

    def emit(self, final=True):
        nc = self.nc
        if not hasattr(self, "esem"):
            self.esem = {e: self.stack.enter_context(nc.semaphore("e_" + e)) for e in self.eng}
            self.ecount = {e: 0 for e in self.eng}
        esem = self.esem
        for e, lst in self.ops.items():
            for op in lst:
                for d in op.deps:
                    if d.dma or getattr(d, "done", False):
                        continue
                    if d.eng == "pe" and op.eng == "pe" and not op.dma:
                        continue
                    d.signal = True
        for e, lst in self.ops.items():
            c = self.ecount[e]
            for op in lst:
                if not op.dma and op.signal:
                    c += 1
                    op.sem, op.val = esem[e], c
            self.ecount[e] = c
        sched = self
        if not hasattr(self, "waited"):
            self.waited = {e: {} for e in self.eng}

        def run(e, h):
            waited = sched.waited[e]
            for op in sched.ops[e]:
                need = {}
                for d in op.deps:
                    if not d.dma and d.eng == "pe" and e == "pe" and not op.dma:
                        continue
                    if d.sem is None:
                        continue
                    sid = id(d.sem)
                    if sid not in need or need[sid][1] < d.val:
                        need[sid] = (d.sem, d.val)
                for sid, (s, v) in need.items():
                    if waited.get(sid, 0) < v:
                        h.wait_ge(s, v)
                        waited[sid] = v
                if op.fn is None:
                    continue
                ins = op.fn(h)
                if op.dma:
                    ins.then_inc(op.sem, 16)
                elif op.signal:
                    ins.then_inc(op.sem, 1)
            if e == "sp" and final:
                for sk, (s, tot) in sched.dsem.items():
                    if tot > 0:
                        h.wait_ge(s, tot)

        with nc.Block() as block:
            @block.tensor
            def _(h):
                run("pe", h)

            @block.scalar
            def _(h):
                run("act", h)

            @block.vector
            def _(h):
                run("dve", h)

            @block.gpsimd
            def _(h):
                run("pool", h)

            @block.sync
            def _(h):
                run("sp", h)
        for e in self.ops:
            for op in self.ops[e]:
                op.done = True
                op.fn = None
            self.ops[e] = []


class _Scope:
    def __init__(self, k, full=False):
        self.k = k
        self.full = full

    def __enter__(self):
        self.old = self.k.st
        self.es = ExitStack()
        self.es.__enter__()
        self.k.st = self.es
        return self

    def __exit__(self, *a):
        self.k.s.barrier(full=self.full)
        self.k.st = self.old
        return self.es.__exit__(*a)


def _is_sb(ap):
    return type(ap.tensor).__name__.startswith("SB")


class K:
    def __init__(self, nc, stack):
        self.nc = nc
        self.st = stack
        self.s = Sched(nc, stack)
        self.nps = 0

    def scope(self, full=False):
        return _Scope(self, full)

    def sb(self, name, shape, dt):
        return self.st.enter_context(self.nc.sbuf_tensor(name, list(shape), dt))

    def ps(self, name, shape, dt=F32):
        return self.st.enter_context(self.nc.psum_tensor(name, list(shape), dt))

    def dma(self, out, in_, q="sp", ins=None, outs=None, **kw):
        semkey = out if _is_sb(out) else in_
        if outs is not None and _is_sb(out):
            semkey = outs[0]
        elif ins is not None and not _is_sb(out):
            semkey = ins[0]
        return self.s.add(q, lambda h: h.dma_start(out=out, in_=in_, **kw),
                          ins if ins is not None else [in_], outs if outs is not None else [out],
                          dma=True, semkey=semkey)

    def mm(self, out, lhsT, rhs, start=True, stop=True, ins=None, outs=None, **kw):
        return self.s.add("pe", lambda h: h.matmul(out, lhsT, rhs, start=start, stop=stop, **kw),
                          ins if ins is not None else [lhsT, rhs], outs if outs is not None else [out])

    def tr(self, out, in_, ident, ins=None, outs=None):
        return self.s.add("pe", lambda h: h.transpose(out, in_, ident),
                          ins if ins is not None else [in_, ident], outs if outs is not None else [out])

    def act(self, out, in_, func, bias=None, scale=None, accum_out=None, ins=None, outs=None):
        kw = {}
        if bias is not None:
            kw["bias"] = bias
        if scale is not None:
            kw["scale"] = scale
        if accum_out is not None:
            kw["accum_out"] = accum_out
        i = [in_]
        for x in (bias, scale):
            if x is not None and not isinstance(x, (int, float)):
                i.append(x)
        o = [out] + ([accum_out] if accum_out is not None else [])
        return self.s.add("act", lambda h: h.activation(out, in_, func, **kw),
                          ins if ins is not None else i, outs if outs is not None else o)

    def tt(self, out, in0, in1, op, eng="dve", ins=None, outs=None):
        return self.s.add(eng, lambda h: h.tensor_tensor(out, in0, in1, op),
                          ins if ins is not None else [in0, in1], outs if outs is not None else [out])

    def ts(self, out, in0, s1, s2=None, op0=ALU.mult, op1=None, eng="dve", ins=None, outs=None):
        i = [in0] + [x for x in (s1, s2) if x is not None and not isinstance(x, (int, float))]
        if op1 is None:
            fn = lambda h: h.tensor_scalar(out, in0, s1, None, op0)
        else:
            fn = lambda h: h.tensor_scalar(out, in0, s1, s2, op0, op1)
        return self.s.add(eng, fn, ins if ins is not None else i, outs if outs is not None else [out])

    def stt(self, out, in0, scalar, in1, op0, op1, ins=None, outs=None):
        i = [in0, in1] + ([scalar] if not isinstance(scalar, (int, float)) else [])
        return self.s.add("dve", lambda h: h.scalar_tensor_tensor(out, in0, scalar, in1, op0, op1),
                          ins if ins is not None else i, outs if outs is not None else [out])

    def cp(self, out, in_, eng="dve", ins=None, outs=None):
        return self.s.add(eng, lambda h: h.tensor_copy(out, in_),
                          ins if ins is not None else [in_], outs if outs is not None else [out])

    def memset(self, ap, v, eng="dve"):
        return self.s.add(eng, lambda h: h.memset(ap, v), [], [ap])

    def recip(self, out, in_):
        return self.s.add("dve", lambda h: h.reciprocal(out, in_), [in_], [out])


TWO_PI = 6.283185
PW_SLOTS = list(range(9)) + [-4]


def build(dbg=False):
    nc = bass.Bass("TRN2", target_bir_lowering=False)
    dr = {}

    def din(name, shape, dt=F32):
        dr[name] = nc.dram_tensor(name, list(shape), dt, kind="ExternalInput").ap()
        return dr[name]

    def dout(name, shape, dt=F32):
        dr[name] = nc.dram_tensor(name, list(shape), dt, kind="ExternalOutput").ap()
        return dr[name]

    x = din("x", [NT, D])
    rope = din("rope", [NT, 16])
    ident_d = din("ident_in", [128, 128])
    diag_d = din("diag_in", [128, 32])
    g_attn = din("g_attn_in", [128, 8])
    w_in = din("w_in", [D, 7168])
    s_are = din("s_are", [64, 32]); s_aim = din("s_aim", [64, 32]); s_ldt = din("s_ldt", [64, 32])
    s_bre = din("s_bre", [64, 32, 16]); s_bim = din("s_bim", [64, 32, 16])
    s_cre = din("s_cre", [64, 32, 16]); s_cim = din("s_cim", [64, 32, 16])
    s_d = din("s_d", [128, 8])
    s_h0 = din("s_h0", [64, 2, 16, 32])
    masks_d = din("masks_in", [128, 384])
    w_ab = din("w_ab", [512, 1024]); w_glu = din("w_glu", [512, 2048]); w_out = din("w_out", [1024, 1024])
    g_ffn = din("g_ffn_in", [128, 8])
    w_rg = din("w_rg", [1024, 4]); w_re = din("w_re", [1024, 32]); rb_d = din("rbias_in", [128, 36])
    gfin_d = din("gfin_in", [128, 1024])
    w_all = din("w_all", [4096, 6144])
    tri_d = din("tri_in", [128, 128]); bidx_d = din("bidx_in", [128, 49, 32]); pcol_d = din("pcol_in", [128, 1])
    gffrow_d = din("gffrow_in", [128, 1024])
    hn_scr = nc.dram_tensor("hn_scr", [NT, D], BF16, kind="Internal").ap()
    rt_scr = nc.dram_tensor("rt_scr", [NT, 66], F32, kind="Internal").ap()
    sc_LAMr = nc.dram_tensor("sc_LAMr", [64, 10, 32], F32, kind="Internal").ap()
    sc_LAMi = nc.dram_tensor("sc_LAMi", [64, 10, 32], F32, kind="Internal").ap()
    sc_OutW = nc.dram_tensor("sc_OutW", [128, 8, 8, 4, 32], BF16, kind="Internal").ap()
    sc_Cm = nc.dram_tensor("sc_Cm", [128, 8, 4, 32], BF16, kind="Internal").ap()
    sc_KW = nc.dram_tensor("sc_KW", [128, 8, 8, 32], BF16, kind="Internal").ap()
    sc_SWc = nc.dram_tensor("sc_SWc", [128, 8, 8, 128], BF16, kind="Internal").ap()
    w_bf = nc.dram_tensor("w_bf", [4096, 6144], BF16, kind="Internal").ap()
    xs_scr = nc.dram_tensor("xs_scr", [49 * 256, D], BF16, kind="Internal").ap()
    ys_scr = nc.dram_tensor("ys_scr", [49 * 256, D], BF16, kind="Internal").ap()
    h_scr = nc.dram_tensor("h_scr", [NT, D], F32, kind="Internal").ap()
    y_out = dout("y_out", [NT, D])
    caches = [din("cache0", [16, 128, 2, 8, 64]), din("cache1", [16, 128, 4, 2, 8, 64]), din("cache2", [16, 128, 4, 2, 8, 64])]
    kvp = [dout("kvp0", [128, 2, 8, 64]), dout("kvp1", [512, 2, 8, 64]), dout("kvp2", [2048, 2, 8, 64])]
    kvs = [dout("kvs%d" % g, [16, 4, 2, 8, 64]) for g in range(3)]
    ssm_p = dout("ssm_p", [64, 2, 32])
    ssm_s = dout("ssm_s", [64, 2, 16, 32])
    if dbg:
        dbg_gy = dout("dbg_gy", [128, 8, NT], BF16)
        dbg_at = dout("dbg_at", [128, 4, NT], BF16)

    with ExitStack() as st:
        k = K(nc, st)
        ident = k.sb("ident", [128, 128], BF16)
        with k.scope():
            ident_f = k.sb("ident_f", [128, 128], F32)
            k.dma(ident_f[:], ident_d)
            k.cp(ident[:], ident_f[:])
        diag32 = k.sb("diag32", [128, 32], F32)
        k.dma(diag32[:], diag_d)
        gat = k.sb("gat", [128, 8], F32)
        k.dma(gat[:], g_attn)
        xnT = k.sb("xnT", [128, 8, NT], BF16)
        psb = [k.ps("psb%d" % i, [128, 512], F32) for i in range(8)]
        w_in_v = w_in.rearrange("(c p) n -> p c n", p=128)
        cvt = []
        cvs = {"e": 0}

        def conv_step(dep=None):
            e = cvs["e"]
            if e < 32:
                k.dma(cvt[e % 2][:].rearrange("p (a x) -> p a x", x=2048),
                      w_all[e * 128:(e + 1) * 128, :].rearrange("p (a x) -> p a x", x=2048), q="pool",
                      ins=([dep] if dep is not None else []))
            if 1 <= e <= 32:
                k.dma(w_bf[(e - 1) * 128:e * 128, :], cvt[(e - 1) % 2][:], q="pool", outs=[("w_bf", e)])
            cvs["e"] = e + 1

        tiles = [(i * 128, 128) for i in range(16)] + [(NPT, NS)]
        with k.scope():
            xt = [k.sb("xt%d" % i, [128, D], F32) for i in range(2)]
            xb = [k.sb("xb%d" % i, [128, D], BF16) for i in range(2)]
            junk = k.sb("junk", [128, D], BF16)
            ss = [k.sb("ss%d" % i, [128, 1], F32) for i in range(2)]
            rs = [k.sb("rs%d" % i, [128, 1], F32) for i in range(2)]
            _g0 = k.s.gcount
            LAMr = k.sb("LAMr", [64, 10, 32], F32); LAMi = k.sb("LAMi", [64, 10, 32], F32)
            OutW = k.sb("OutW", [128, 8, 8, 4, 32], BF16)
            Cm = k.sb("Cm", [128, 8, 4, 32], BF16)
            KW = k.sb("KW", [128, 8, 8, 32], BF16)
            SWc = k.sb("SWc", [128, 8, 8, 128], BF16)
            def T(name, shape, dt=F32):
                return k.sb(name, shape, dt)

            a_re = T("a_re", [64, 32]); a_im = T("a_im", [64, 32]); ldt = T("ldt", [64, 32])
            bre = T("bre", [64, 32, 16]); bim = T("bim", [64, 32, 16])
            cre = T("cre", [64, 32, 16]); cim = T("cim", [64, 32, 16])
            for t_, d_ in ((a_re, s_are), (a_im, s_aim), (ldt, s_ldt), (bre, s_bre), (bim, s_bim), (cre, s_cre), (cim, s_cim)):
                k.dma(t_[:], d_)
            dpad = T("dpad", [128, 8]); k.dma(dpad[:], s_d)
            lr = T("lr", [64, 32]); li = T("li", [64, 32])
            k.act(ldt[:], ldt[:], AF.Exp)
            k.tt(lr[:], a_re[:], ldt[:], ALU.mult)
            k.tt(li[:], a_im[:], ldt[:], ALU.mult)
            pass
            mag = T("mag", [64, 32]); yv = T("yv", [64, 32]); yi = T("yi", [64, 32], I32); yf = T("yf", [64, 32])
            fr = T("fr", [64, 32]); fc = T("fc", [64, 32]); msk = T("msk", [64, 32]); sn = T("sn", [64, 32]); cs = T("cs", [64, 32])
            for slot, tau in enumerate(PW_SLOTS):
                k.act(mag[:], lr[:], AF.Exp, scale=float(tau))
                k.ts(yv[:], li[:], float(tau) / (2 * np.pi), None, ALU.mult)
                k.cp(yi[:], yv[:])
                k.cp(yf[:], yi[:])
                k.tt(fr[:], yv[:], yf[:], ALU.subtract)
                k.ts(fc[:], fr[:], 0.25, None, ALU.add)
                k.ts(msk[:], fc[:], 0.5, None, ALU.is_gt)
                k.tt(fc[:], fc[:], msk[:], ALU.subtract)
                k.act(sn[:], fr[:], AF.Sin, scale=TWO_PI)
                k.act(cs[:], fc[:], AF.Sin, scale=TWO_PI)
                k.tt(LAMr[:, slot, :], mag[:], cs[:], ALU.mult)
                k.tt(LAMi[:, slot, :], mag[:], sn[:], ALU.mult)
            nre = T("nre", [64, 32]); den = T("den", [64, 32]); t0_ = T("t0_", [64, 32]); t1_ = T("t1_", [64, 32])
            fre = T("fre", [64, 32]); fim = T("fim", [64, 32])
            k.ts(nre[:], LAMr[:, 1, :], -1.0, None, ALU.add)
            k.tt(den[:], a_re[:], a_re[:], ALU.mult)
            k.tt(t0_[:], a_im[:], a_im[:], ALU.mult)
            k.tt(den[:], den[:], t0_[:], ALU.add)
            k.recip(den[:], den[:])
            k.tt(t0_[:], nre[:], a_re[:], ALU.mult)
            k.tt(t1_[:], LAMi[:, 1, :], a_im[:], ALU.mult)
            k.tt(t0_[:], t0_[:], t1_[:], ALU.add)
            k.tt(fre[:], t0_[:], den[:], ALU.mult)
            k.tt(t0_[:], LAMi[:, 1, :], a_re[:], ALU.mult)
            k.tt(t1_[:], nre[:], a_im[:], ALU.mult)
            k.tt(t0_[:], t0_[:], t1_[:], ALU.subtract)
            k.tt(fim[:], t0_[:], den[:], ALU.mult)
            bbr = T("bbr", [64, 32, 16]); bbi = T("bbi", [64, 32, 16])
            u1 = T("u1", [64, 32, 16]); u2 = T("u2", [64, 32, 16])

            def bc(ap2):
                return ap2.unsqueeze(2).to_broadcast([64, 32, 16])

            k.tt(u1[:], bre[:], bc(fre[:]), ALU.mult)
            k.tt(u2[:], bim[:], bc(fim[:]), ALU.mult)
            k.tt(bbr[:], u1[:], u2[:], ALU.subtract)
            k.tt(u1[:], bim[:], bc(fre[:]), ALU.mult)
            k.tt(u2[:], bre[:], bc(fim[:]), ALU.mult)
            k.tt(bbi[:], u1[:], u2[:], ALU.add)
            ZB = T("ZB", [128, 8, 8, 4, 32], BF16)
            pass
            pass
            k.memset(ZB[:], 0.0)
            k.memset(OutW[:], 0.0)
            k.memset(Cm[:], 0.0)

            def v4(ap3):
                return ap3.rearrange("p (c q) x -> p c q x", q=4)

            for tau in range(8):
                lrb, lib = bc(LAMr[:, tau, :]), bc(LAMi[:, tau, :])
                k.tt(u1[:], bbr[:], lrb, ALU.mult)
                k.tt(u2[:], bbi[:], lib, ALU.mult)
                k.tt(ZB[0:64, :, tau, :, 0:16], v4(u1[:]), v4(u2[:]), ALU.subtract, outs=[("ZB", tau)])
                k.tt(u1[:], bbi[:], lrb, ALU.mult)
                k.tt(u2[:], bbr[:], lib, ALU.mult)
                k.tt(ZB[64:128, :, tau, :, 0:16], v4(u1[:]), v4(u2[:]), ALU.add, outs=[("ZB", tau)])
            for j in range(8):
                lrb, lib = bc(LAMr[:, j + 1, :]), bc(LAMi[:, j + 1, :])
                k.tt(u1[:], cre[:], lrb, ALU.mult)
                k.tt(u2[:], cim[:], lib, ALU.mult)
                k.tt(OutW[0:64, :, j, :, 0:16], v4(u1[:]), v4(u2[:]), ALU.subtract, outs=[("OutW", j)])
                k.tt(u1[:], cre[:], lib, ALU.mult)
                k.tt(u2[:], cim[:], lrb, ALU.mult)
                k.tt(u1[:], u1[:], u2[:], ALU.add)
                k.ts(OutW[64:128, :, j, :, 0:16], v4(u1[:]), -1.0, None, ALU.mult, outs=[("OutW", j)])
            k.cp(Cm[0:64, :, :, 0:16], v4(cre[:]))
            k.ts(Cm[64:128, :, :, 0:16], v4(cim[:]), -1.0, None, ALU.mult)
            KWf = T("KWf", [128, 8, 8, 32], F32)
            pass
            for chunk in range(8):
                for tau in range(8):
                    slot = chunk * 8 + tau
                    bank = psb[2 + slot // 16]
                    c0 = (slot % 16) * 32
                    for gq in range(4):
                        k.mm(bank[32 * gq:32 * gq + 32, c0:c0 + 32], ZB[:, chunk, tau, gq, :], Cm[:, chunk, gq, :],
                             tile_position=(0, 32 * gq))
            for b4 in range(4):
                k.cp(KWf[:, 2 * b4:2 * b4 + 2, :, :], psb[2 + b4][:].rearrange("p (a t x) -> p a t x", a=2, t=8), eng="dve")
            for chunk in range(8):
                k.stt(KWf[:, chunk, 0, :], diag32[:], dpad[:, chunk:chunk + 1], KWf[:, chunk, 0, :], ALU.mult, ALU.add)
            k.cp(KW[:], KWf[:])
            pass
            for chunk in range(8):
                bankb = psb[4 + chunk % 4][:].bitcast(BF16)
                for i in range(8):
                    k.tr(bankb[:, i * 128:(i + 1) * 128], ZB[:, chunk, 7 - i, :, :].rearrange("p q x -> p (q x)"), ident[:])
                k.cp(SWc[:, chunk, :, :], bankb.rearrange("p (i x) -> p i x", i=8), eng=("dve" if chunk % 2 else "act") if False else "dve")

            for t_, d_ in ((LAMr, sc_LAMr), (LAMi, sc_LAMi), (OutW, sc_OutW), (Cm, sc_Cm), (KW, sc_KW), (SWc, sc_SWc)):
                k.s.add("pool", (lambda o_, i_: (lambda h: h.dma_start(out=o_, in_=i_)))(d_, t_[:]), [t_], [d_], dma=True, semkey="spill_st")
            _g1 = k.s.gcount
            for ti, (t0, n) in enumerate(tiles):
                b = ti % 2
                k.dma(xt[b][0:n, :], x[t0:t0 + n, :])
                k.act(junk[0:n, :], xt[b][0:n, :], AF.Square, accum_out=ss[b][0:n, :])
                k.ts(rs[b][0:n, :], ss[b][0:n, :], 1.0 / D, 1e-6, ALU.mult, ALU.add)
                k.act(rs[b][0:n, :], rs[b][0:n, :], AF.Sqrt)
                k.recip(rs[b][0:n, :], rs[b][0:n, :])
                k.act(xb[b][0:n, :], xt[b][0:n, :], AF.Copy, scale=rs[b][0:n, :])
                pt = psb[ti % 2]
                ptb = pt[:].bitcast(BF16)
                for c in range(8):
                    k.tr(ptb[:, c * 128:c * 128 + n], xb[b][0:n, c * 128:(c + 1) * 128], ident[0:n, 0:n])
                for c in range(8):
                    if c % 2 == 0:
                        k.act(xnT[:, c, t0:t0 + n], ptb[:, c * 128:c * 128 + n], AF.Copy, scale=gat[:, c:c + 1],
                              outs=[("xnT", ti)])
                    else:
                        k.ts(xnT[:, c, t0:t0 + n], ptb[:, c * 128:c * 128 + n], gat[:, c:c + 1], None, ALU.mult,
                             outs=[("xnT", ti)])

            k.s.interleave(_g0, _g1, k.s.gcount)
        with k.scope(full=True):
            attnT = k.sb("attnT", [128, 4, NT], BF16)
            with k.scope():
                wst = [k.sb("wst%d" % i, [128, 8, 256], F32) for i in range(3)]
                wbf = [k.sb("wbf0", [128, 8, 768], BF16)]
                qkf = [k.sb("qkf%d" % i, [128, 512], F32) for i in range(2)]
                vf = [k.sb("vf%d" % i, [128, 256], F32) for i in range(2)]
                qb = [k.sb("qb%d" % i, [128, 256], BF16) for i in range(2)]
                kb = [k.sb("kb%d" % i, [128, 256], BF16) for i in range(2)]
                rp = [k.sb("rp%d" % i, [128, 16], F32) for i in range(2)]
                tmp = [k.sb("rtmp%d" % i, [128, 8, 8], F32) for i in range(4)]
                mstage = k.sb("mstage", [128, 384], F32)
                maskb = k.sb("maskb", [128, 256], BF16)
                msamp = k.sb("msamp", [128, 2, 64], BF16)
                k.dma(mstage[:], masks_d)
                k.cp(maskb[:], mstage[:, 0:256])
                k.cp(msamp[:], mstage[:, 256:384].rearrange("p (a x) -> p a x", a=2))
                ones_f = k.sb("ones_f", [128, 64], F32)
                k.memset(ones_f[:], 1.0)
                QTz = k.sb("QTz", [128, 4, NT], BF16)
                KT = k.sb("KT", [128, 2, NT], BF16)
                Va = k.sb("Va", [128, 17, 4, 65], BF16)
                acc = k.sb("acc", [65, 4, NT], F32)
                PT = [k.sb("PT%d" % i, [128, 256], BF16) for i in range(4)]
                PTs4 = [k.sb("PTs%d" % i, [128, 64], BF16) for i in range(4)]
                cst = [k.sb("cst%d" % i, [128, 4, 2, 256], F32) for i in range(2)]
                kcb = [k.sb("kcb%d" % i, [128, 256], BF16) for i in range(4)]
                KcT = [k.sb("KcT%d" % i, [128, 2, 128], BF16) for i in range(4)]
                Vc = [k.sb("Vc%d" % i, [128, 4, 65], BF16) for i in range(4)]
                PTc = [k.sb("PTc%d" % i, [128, 4, 4], BF16) for i in range(4)]
                k.memset(QTz[:], 0.0, eng="pool")
                k.memset(Va[:], 1.0, eng="pool")
                for i in range(4):
                    k.memset(PTs4[i][:], 0.0, eng="pool")
                for i in range(4):
                    k.memset(Vc[i][:], 1.0, eng="pool")

                def pipeline(iters, skews):
                    n_ = len(iters)
                    for step in range(n_ + max(skews)):
                        for si, sk in enumerate(skews):
                            i_ = step - sk
                            if 0 <= i_ < n_:
                                iters[i_][si]()

                cnt = {"it": 0, "ai": 0, "ci": 0}
                for hh in range(2):
                    k.memset(acc[:], 0.0, eng="pool")
                    for g, (win, dil) in enumerate(GROUPS):
                        wb = wbf[0]
                        for j in range(3):
                            c0 = j * 1536 + g * 512 + hh * 256
                            k.dma(wst[j][:], w_in_v[:, :, c0:c0 + 256])
                            if j == 1:
                                k.act(wb[:, :, j * 256:(j + 1) * 256], wst[j][:], AF.Copy, outs=[(wb.name, j)])
                            else:
                                k.cp(wb[:, :, j * 256:(j + 1) * 256], wst[j][:], outs=[(wb.name, j)])
                        nblk = (NPT // dil) // 128
                        blocks = []
                        for r in range(dil):
                            for i in range(nblk):
                                blocks.append((r + dil * 128 * i, dil, 128, r, i))
                        blocks.append((NPT, 1, NS, None, None))

                        def mk_block(bi, tstart, tstep, n, r, i, hh=hh, g=g, win=win, dil=dil, wb=wb):
                            b = cnt["it"] % 2
                            cnt["it"] += 1
                            pq, pv = psb[2 + 2 * b], psb[3 + 2 * b]
                            tok = slice(tstart, tstart + tstep * (n - 1) + 1, tstep)
                            ptb = psb[6 + b][:].bitcast(BF16)
                            pc = slice(bi * 128, bi * 128 + n)

                            def s1():
                                for c in range(8):
                                    k.mm(pq[0:n, :], xnT[:, c, tok], wb[:, c, 0:512], start=(c == 0), stop=(c == 7),
                                         ins=["xnT", wb])
                                for c in range(8):
                                    k.mm(pv[0:n, 0:256], xnT[:, c, tok], wb[:, c, 512:768], start=(c == 0), stop=(c == 7),
                                         ins=["xnT", wb])
                                k.dma(rp[b][0:n, :], rope[tok, :])

                            def s2():
                                k.act(qkf[b][0:n, :], pq[0:n, :], AF.Copy)
                                k.act(vf[b][0:n, :], pv[0:n, 0:256], AF.Copy)
                                q3 = qkf[b][0:n, :].rearrange("p (h d) -> p h d", d=64)
                                x1, x2 = q3[:, :, 0:8], q3[:, :, 8:16]
                                cosb = rp[b][0:n, 0:8].unsqueeze(1).to_broadcast([n, 8, 8])
                                sinb = rp[b][0:n, 8:16].unsqueeze(1).to_broadcast([n, 8, 8])
                                t1, t2, t3, t4 = (t[0:n] for t in tmp)
                                k.tt(t1, x1, cosb, ALU.mult)
                                k.tt(t2, x2, sinb, ALU.mult)
                                k.tt(t3, x2, cosb, ALU.mult)
                                k.tt(t4, x1, sinb, ALU.mult)
                                k.tt(x1, t1, t2, ALU.subtract, outs=[qkf[b]])
                                k.tt(x2, t3, t4, ALU.add, outs=[qkf[b]])
                                kpart = qkf[b][0:n, 256:512].rearrange("p (h d) -> p h d", d=64)
                                vpart = vf[b][0:n, :].rearrange("p (h d) -> p h d", d=64)
                                hs = slice(hh * 4, hh * 4 + 4)
                                if r is None:
                                    dk = kvs[g][:, :, 0, hs, :].rearrange("s t h d -> (s t) h d")
                                    dv = kvs[g][:, :, 1, hs, :].rearrange("s t h d -> (s t) h d")
                                    k.dma(dk, kpart, q="pool")
                                    k.dma(dv, vpart, q="pool")
                                else:
                                    first = NPT - min(win, NPT)
                                    if tstart >= first:
                                        rows = slice(tstart - first, tstart - first + dil * 127 + 1, dil)
                                        k.dma(kvp[g][rows, 0, hs, :], kpart, q="pool")
                                        k.dma(kvp[g][rows, 1, hs, :], vpart, q="pool")
                                k.ts(qb[b][0:n, :], qkf[b][0:n, 0:256], 0.125, None, ALU.mult)
                                k.cp(kb[b][0:n, :], qkf[b][0:n, 256:512])
                                k.act(Va[0:n, bi, :, 0:64], vpart, AF.Copy, outs=[("Va", bi)])
                                for pr in range(2):
                                    k.tr(ptb[:, pr * 128:pr * 128 + n], qb[b][0:n, pr * 128:(pr + 1) * 128], ident[0:n, 0:n])
                                    k.tr(ptb[:, 256 + pr * 128:256 + pr * 128 + n], kb[b][0:n, pr * 128:(pr + 1) * 128],
                                         ident[0:n, 0:n])

                            def s3():
                                for pr in range(2):
                                    k.act(QTz[0:64, 2 * pr, pc], ptb[0:64, pr * 128:pr * 128 + n], AF.Copy, outs=[("QTz", bi)])
                                    k.cp(QTz[64:128, 2 * pr + 1, pc], ptb[64:128, pr * 128:pr * 128 + n], outs=[("QTz", bi)])
                                    if pr == 0:
                                        k.act(KT[:, pr, pc], ptb[:, 256 + pr * 128:256 + pr * 128 + n], AF.Copy, outs=[("KT", bi)])
                                    else:
                                        k.cp(KT[:, pr, pc], ptb[:, 256 + pr * 128:256 + pr * 128 + n], outs=[("KT", bi)])
                            return (s1, s2, s3)

                        pipeline([mk_block(bi, *blk) for bi, blk in enumerate(blocks)], (0, 1, 2))

                        def mk_att(h, r, i, g=g, dil=dil, nblk=nblk):
                            a = cnt["ai"] % 4
                            cnt["ai"] += 1
                            ps_s, ps_o = psb[a], psb[4 + a]
                            if r is None:
                                PTs = PTs4[a]
                                def a1():
                                    k.mm(ps_s[0:64, 0:64], KT[:, h // 2, NPT:NT], QTz[:, h, NPT:NT], start=True, stop=False,
                                         ins=["KT", "QTz"])
                                    k.mm(ps_s[0:64, 0:64], ident[:, 0:64], msamp[:, 0 if g == 0 else 1, :], start=False, stop=True)
                                    k.act(PTs[0:64, :], ps_s[0:64, 0:64], AF.Exp)

                                def a2():
                                    k.mm(ps_o[0:65, 0:64], Va[:, 16, h, :], PTs[:, :], ins=["Va", PTs])
                                    av = acc[0:65, h, NPT:NT]
                                    k.tt(av, av, ps_o[0:65, 0:64], ALU.add, ins=[acc, ps_o], outs=[acc])
                                return (a1, a2)
                            bi = r * nblk + i
                            nq = 2 if i + 1 < nblk else 1
                            kc = slice(bi * 128, bi * 128 + 128)
                            qc = slice(bi * 128, bi * 128 + 128 * nq)
                            N = 128 * nq

                            def a1():
                                k.mm(ps_s[:, 0:N], KT[:, h // 2, kc], QTz[:, h, qc], start=True, stop=False,
                                     ins=["KT", "QTz"])
                                k.mm(ps_s[:, 0:N], ident[:], maskb[:, 0:N], start=False, stop=True)
                                k.act(PT[a][:, 0:N], ps_s[:, 0:N], AF.Exp)

                            def a2():
                                k.mm(ps_o[0:65, 0:N], Va[:, bi, h, :], PT[a][:, 0:N], ins=["Va", PT[a]])
                                t0n = r + dil * 128 * i
                                av = acc[0:65, h, t0n:t0n + dil * (N - 1) + 1:dil]
                                k.tt(av, av, ps_o[0:65, 0:N], ALU.add, ins=[acc, ps_o], outs=[acc])
                            return (a1, a2)

                        its = [mk_att(h, r, i) for h in range(4) for r in range(dil) for i in range(nblk)]
                        its += [mk_att(h, None, None) for h in range(4)]
                        pipeline(its, (0, 2))

                        def mk_cache(s_, t_, nt_, nq, cb, g=g, hh=hh):
                            e = cnt["ci"] % 4
                            cnt["ci"] += 1
                            a = cnt["ai"] % 2
                            cnt["ai"] += 1
                            ps_s, ps_o = psb[a], psb[2 + a]
                            ptb = psb[6 + e % 2][:].bitcast(BF16)
                            q0 = NPT + 4 * s_ + (t_ if g > 0 else 0)

                            def c0():
                                if t_ == 0:
                                    if g == 0:
                                        k.dma(cst[cb][:, 0, :, :], caches[0][s_, :, :, hh * 4:hh * 4 + 4, :].rearrange("m a h d -> m a (h d)"))
                                    else:
                                        k.dma(cst[cb][:], caches[g][s_, :, :, :, hh * 4:hh * 4 + 4, :].rearrange("m t a h d -> m t a (h d)"))
                                k.cp(kcb[e][:], cst[cb][:, t_, 0, :])
                                k.act(Vc[e][:, :, 0:64], cst[cb][:, t_, 1, :].rearrange("p (h d) -> p h d", d=64), AF.Copy)
                                for pr in range(2):
                                    k.tr(ptb[:, pr * 128:(pr + 1) * 128], kcb[e][:, pr * 128:(pr + 1) * 128], ident[:])

                            def c1():
                                k.act(KcT[e][:], ptb[:, 0:256].rearrange("p (a x) -> p a x", a=2), AF.Copy)
                                for h in range(4):
                                    k.mm(ps_s[:, h * 4:h * 4 + nq], KcT[e][:, h // 2, :], QTz[:, h, q0:q0 + nq],
                                         start=True, stop=(g > 0), ins=[KcT[e], "QTz"])
                                    if g == 0:
                                        k.mm(ps_s[:, h * 4:h * 4 + nq], ident[:], maskb[:, 128:128 + nq], start=False, stop=True)
                                k.act(PTc[e][:, :, 0:nq], ps_s[:, 0:16].rearrange("p (h q) -> p h q", q=4)[:, :, 0:nq], AF.Exp)

                            def c2():
                                for h in range(4):
                                    k.mm(ps_o[0:65, h * 4:h * 4 + nq], Vc[e][:, h, :], PTc[e][:, h, 0:nq])
                                av = acc[0:65, :, q0:q0 + nq]
                                k.tt(av, av, ps_o[0:65, 0:16].rearrange("p (h q) -> p h q", q=4)[:, :, 0:nq], ALU.add,
                                     ins=[acc, ps_o], outs=[acc])
                            return (c0, c1, c2)

                        its = []
                        for s_ in range(16):
                            nt_, nq = (1, 4) if g == 0 else (4, 1)
                            for t_ in range(nt_):
                                its.append(mk_cache(s_, t_, nt_, nq, s_ % 2))
                        pipeline(its, (0, 1, 2))

                    for h in range(4):
                        k.recip(acc[64:65, h, :], acc[64:65, h, :])
                        for (t0, n) in [(i * 512, 512) for i in range(4)] + [(NPT, NS)]:
                            a = cnt["ai"] % 2
                            cnt["ai"] += 1
                            pbc = psb[a]
                            k.mm(pbc[0:64, 0:n], ones_f[64:65, 0:64], acc[64:65, h, t0:t0 + n], tile_position=(64, 0))
                            dst = attnT[(h % 2) * 64:(h % 2) * 64 + 64, hh * 2 + h // 2, t0:t0 + n]
                            k.tt(dst, acc[0:64, h, t0:t0 + n], pbc[0:64, 0:n], ALU.mult, outs=[attnT])
                if dbg:
                    k.dma(dbg_at, attnT[:], q="pool")
            uT = k.sb("uT", [128, 8, NT], BF16)
            cvt.extend([k.sb("cvt%d" % i, [128, 6144], BF16) for i in range(2)])
            k.s.bg_keys = set((c_.name, None) for c_ in cvt)

            with k.scope():
                LAMr = k.sb("LAMr_s", [64, 10, 32], F32); LAMi = k.sb("LAMi_s", [64, 10, 32], F32)
                OutW = k.sb("OutW_s", [128, 8, 8, 4, 32], BF16)
                Cm = k.sb("Cm_s", [128, 8, 4, 32], BF16)
                KW = k.sb("KW_s", [128, 8, 8, 32], BF16)
                SWc = k.sb("SWc_s", [128, 8, 8, 128], BF16)
                _lds = []
                for t_, d_ in ((LAMr, sc_LAMr), (LAMi, sc_LAMi), (OutW, sc_OutW), (Cm, sc_Cm), (KW, sc_KW), (SWc, sc_SWc)):
                    _lds.append(k.s.add("sp", (lambda o_, i_: (lambda h: h.dma_start(out=o_, in_=i_)))(t_[:], d_), [d_], [t_], dma=True, semkey="spill_ld"))
                for o_ in _lds:
                    o_.val = _lds[-1].val

                def T(name, shape, dt=F32):
                    return k.sb(name, shape, dt)

                with k.scope():
                    Wu = T("Wu", [128, 8, 8, 4, 32], BF16)
                    wst = [k.sb("wsu%d" % i, [128, 8, 256], F32) for i in range(2)]
                    k.memset(Wu[:], 0.0)
                    for h in range(2):
                        k.dma(wst[h][:], w_in_v[:, :, 4608 + 256 * h:4608 + 256 * h + 256])
                        for c in range(8):
                            k.cp(Wu[:, c, 4 * h:4 * h + 4, :, 0:16], wst[h][:, c, :].rearrange("p (a q x) -> p a q x", a=4, q=4),
                                 eng="dve")
                    pass
                    ranges = [(i * 512, 512) for i in range(4)] + [(NPT, NS)]
                    it = 0
                    for chunk in range(8):
                        for (t0, n) in ranges:
                            pb = psb[it % 4]
                            it += 1
                            for c in range(8):
                                k.mm(pb[:, 0:n], Wu[:, c, chunk, :, :].rearrange("p q x -> p (q x)"), xnT[:, c, t0:t0 + n],
                                     start=(c == 0), stop=(c == 7), ins=[Wu, "xnT"])
                            if it % 2:
                                k.act(uT[:, chunk, t0:t0 + n], pb[:, 0:n], AF.Copy, outs=[("uT", chunk)])
                            else:
                                k.cp(uT[:, chunk, t0:t0 + n], pb[:, 0:n], outs=[("uT", chunk)])
                HP = T("HP", [128, 32, 256], BF16)
                k.memset(HP[:, :, 0:1], 0.0)
                with k.scope():
                    SS = [T("SS0", [64, 64, 2, 32])]
                    Hr = T("Hr", [64, 64, 3, 32])
                    Z3 = T("Z3", [64, 3, 32]); k.memset(Z3[:], 0.0)
                    AR2 = T("AR2", [64, 2, 32]); AI2 = T("AI2", [64, 2, 32])
                    k.cp(AR2[:, 0, :], LAMr[:, 8, :]); k.cp(AR2[:, 1, :], LAMr[:, 8, :])
                    k.ts(AI2[:, 0, :], LAMi[:, 8, :], -1.0, None, ALU.mult); k.cp(AI2[:, 1, :], LAMi[:, 8, :])
                    sA = T("sA", [64, 2, 32]); sB = T("sB", [64, 2, 32])
                    fill = 0
                    for r in range(4):
                        S_ = SS[0]
                        for gq in range(4):
                            for ch in range(2):
                                bank = psb[fill % 8]
                                fill += 1
                                for cc in range(4):
                                    chunk = ch * 4 + cc
                                    for ri in range(2):
                                        c0 = (cc * 2 + ri) * 64
                                        for i in range(8):
                                            k.mm(bank[0:64, c0:c0 + 64],
                                                 SWc[32 * gq:32 * gq + 32, chunk, i, ri * 64:(ri + 1) * 64],
                                                 uT[32 * gq:32 * gq + 32, chunk, 512 * r + i:512 * r + 512:8],
                                                 start=(i == 0), stop=(i == 7), tile_position=(32 * gq, 0),
                                                 ins=[SWc, ("uT", chunk)])
                                g0 = gq + 16 * ch
                                src = bank[0:64, :].rearrange("p (c r k) -> p k r c", c=4, r=2)
                                if fill % 2:
                                    k.act(S_[:, :, :, g0:g0 + 13:4], src, AF.Copy, outs=[(S_.name, fill % 8)])
                                else:
                                    k.cp(S_[:, :, :, g0:g0 + 13:4], src, outs=[(S_.name, fill % 8)])
                        for kk in range(64):
                            prev = Z3[:] if (r == 0 and kk == 0) else (Hr[:, 63, :, :] if kk == 0 else Hr[:, kk - 1, :, :])
                            k.tt(sA[:], prev[:, 0:2, :], AR2[:], ALU.mult)
                            k.tt(sB[:], prev[:, 1:3, :], AI2[:], ALU.mult)
                            k.tt(sA[:], sA[:], sB[:], ALU.add)
                            k.tt(Hr[:, kk, 0:2, :], sA[:], S_[:, kk, :, :], ALU.add, ins=[sA, S_])
                            k.cp(Hr[:, kk, 2, :], Hr[:, kk, 0, :])
                            if kk % 32 == 31:
                                conv_step(Hr)
                        ncol = 64 if r < 3 else 63
                        k.cp(HP[0:64, :, 64 * r + 1:64 * r + 1 + ncol], Hr[:, 0:ncol, 0, :].rearrange("p k g -> p g k"))
                        k.cp(HP[64:128, :, 64 * r + 1:64 * r + 1 + ncol], Hr[:, 0:ncol, 1, :].rearrange("p k g -> p g k"))
                    k.dma(ssm_p, Hr[:, 63, 0:2, :], q="pool")
                HPs = T("HPs", [128, 32, 16], BF16)
                with k.scope():
                    SSs = T("SSs", [64, 2, 16, 32])
                    for gq in range(4):
                        for ri in range(2):
                            bank = psb[gq * 2 + ri]
                            for chunk in range(8):
                                for i in range(4):
                                    k.mm(bank[0:64, chunk * 16:(chunk + 1) * 16],
                                         SWc[32 * gq:32 * gq + 32, chunk, i, ri * 64:(ri + 1) * 64],
                                         uT[32 * gq:32 * gq + 32, chunk, NPT + i:NT:4],
                                         start=(i == 0), stop=(i == 3), tile_position=(32 * gq, 0),
                                         ins=[SWc, ("uT", chunk)])
                            k.cp(SSs[:, ri, :, gq:32:4], bank[0:64, 0:128].rearrange("p (c s) -> p s c", c=8))
                    h0P = T("h0P", [64, 2, 16, 32])
                    k.dma(h0P[:], s_h0)

                    def bs(ap2):
                        return ap2.unsqueeze(1).to_broadcast([64, 16, 32])

                    w1 = T("w1", [64, 16, 32]); w2 = T("w2", [64, 16, 32]); Wr_ = T("Wr_", [64, 16, 32]); Wi_ = T("Wi_", [64, 16, 32])
                    nsP = T("nsP", [64, 2, 16, 32])
                    ar8, ai8, l4r, l4i = bs(LAMr[:, 8, :]), bs(LAMi[:, 8, :]), bs(LAMr[:, 9, :]), bs(LAMi[:, 9, :])
                    k.tt(w1[:], h0P[:, 0], ar8, ALU.mult); k.tt(w2[:], h0P[:, 1], ai8, ALU.mult)
                    k.tt(w1[:], w1[:], w2[:], ALU.subtract); k.tt(Wr_[:], w1[:], SSs[:, 0], ALU.add)
                    k.tt(w1[:], h0P[:, 1], ar8, ALU.mult); k.tt(w2[:], h0P[:, 0], ai8, ALU.mult)
                    k.tt(w1[:], w1[:], w2[:], ALU.add); k.tt(Wi_[:], w1[:], SSs[:, 1], ALU.add)
                    k.tt(w1[:], Wr_[:], l4r, ALU.mult); k.tt(w2[:], Wi_[:], l4i, ALU.mult)
                    k.tt(nsP[:, 0], w1[:], w2[:], ALU.subtract)
                    k.tt(w1[:], Wi_[:], l4r, ALU.mult); k.tt(w2[:], Wr_[:], l4i, ALU.mult)
                    k.tt(nsP[:, 1], w1[:], w2[:], ALU.add)
                    k.dma(ssm_s, nsP[:], q="pool")
                    k.cp(HPs[0:64], h0P[:, 0].rearrange("p s g -> p g s"))
                    k.cp(HPs[64:128], h0P[:, 1].rearrange("p s g -> p g s"))
                for chunk in range(8):
                    for gq in range(4):
                        g = chunk * 4 + gq
                        for j in range(8):
                            bank = psb[j]
                            for i in range(j + 1):
                                k.mm(bank[32 * gq:32 * gq + 32, 0:256], KW[32 * gq:32 * gq + 32, chunk, j - i, :],
                                     uT[32 * gq:32 * gq + 32, chunk, i:NPT:8], start=(i == 0), stop=False,
                                     tile_position=(32 * gq, 32 * gq), ins=[KW, ("uT", chunk)])
                                if j < 4:
                                    k.mm(bank[32 * gq:32 * gq + 32, 256:272], KW[32 * gq:32 * gq + 32, chunk, j - i, :],
                                         uT[32 * gq:32 * gq + 32, chunk, NPT + i:NT:4], start=False, stop=False,
                                         tile_position=(32 * gq, 32 * gq), ins=[KW, ("uT", chunk)])
                    for gq in range(4):
                        g = chunk * 4 + gq
                        for j in range(8):
                            bank = psb[j]
                            k.mm(bank[32 * gq:32 * gq + 32, 0:256], OutW[:, chunk, j, gq, :], HP[:, g, :],
                                 start=False, stop=True, tile_position=(0, 32 * gq))
                            if j < 4:
                                k.mm(bank[32 * gq:32 * gq + 32, 256:272], OutW[:, chunk, j, gq, :], HPs[:, g, :],
                                     start=False, stop=True, tile_position=(0, 32 * gq))
                    for j in range(8):
                        k.act(uT[:, chunk, j:NPT:8], psb[j][:, 0:256], AF.Gelu_apprx_tanh, outs=[("uT", chunk)])
                        if j < 4:
                            k.act(uT[:, chunk, NPT + j:NT:4], psb[j][:, 256:272], AF.Gelu_apprx_tanh, outs=[("uT", chunk)])
                    conv_step(("uT", chunk))
                if dbg:
                    k.dma(dbg_gy, uT[:], q="pool")


            mergedT = k.sb("mergedT", [128, 8, NT], BF16)
            ranges = [(i * 512, 512) for i in range(4)] + [(NPT, NS)]
            wab_v = w_ab.rearrange("(c p) n -> p c n", p=128)
            wglu_v = w_glu.rearrange("(ch q c) n -> q c ch n", q=4, c=16)
            ring = [0]

            def nbank():
                ring[0] += 1
                return psb[ring[0] % 8]

            with k.scope():
                sAB = k.sb("sAB", [128, 4, 128], F32)
                sG = k.sb("sG", [128, 8, 2, 128], F32)
                sL = k.sb("sL", [128, 8, 2, 128], F32)
                k.memset(sL[:], 0.0)
                mw = [(k.sb("mAB%d" % i, [128, 4, 128], BF16), k.sb("mG%d" % i, [128, 8, 2, 128], BF16),
                       k.sb("mL%d" % i, [128, 8, 2, 128], BF16)) for i in range(2)]
                mt = [[k.sb("mt%d_%d" % (i, j), [128, 512], F32) for j in range(5)] for i in range(2)]
                mi = 0
                for fc in range(8):
                    AB, G_, L_ = mw[fc % 2]
                    k.dma(sAB[:], wab_v[:, :, fc * 128:(fc + 1) * 128])
                    for j in range(2):
                        k.dma(sG[:, :, j, :], w_in_v[:, :, 5120 + j * 1024 + fc * 128:5120 + j * 1024 + (fc + 1) * 128], outs=[("sG", j)])
                        for gq in range(4):
                            k.dma(sL[32 * gq:32 * gq + 16, :, j, :], wglu_v[gq, :, :, j * 1024 + fc * 128:j * 1024 + (fc + 1) * 128],
                                  outs=[("sL", j * 4 + gq)])
                    k.cp(AB[:], sAB[:])
                    k.act(G_[:], sG[:], AF.Copy)
                    k.cp(L_[:], sL[:])
                    for (t0, n) in ranges:
                        tl = slice(t0, t0 + n)
                        s1, s2, s3, m1, m2 = mt[mi % 2]
                        mi += 1
                        p_ao, p_ga, p_gs, p_la, p_lb = nbank(), nbank(), nbank(), nbank(), nbank()
                        for c in range(4):
                            k.mm(p_ao[:, 0:n], AB[:, c, :], attnT[:, c, tl], start=(c == 0), stop=(c == 3))
                        for j, pb in ((0, p_ga), (1, p_gs)):
                            for c in range(8):
                                k.mm(pb[:, 0:n], G_[:, c, j, :], xnT[:, c, tl], start=(c == 0), stop=(c == 7), ins=[G_, "xnT"])
                        for j, pb in ((0, p_la), (1, p_lb)):
                            for c in range(8):
                                k.mm(pb[:, 0:n], L_[:, c, j, :], uT[:, c, tl], start=(c == 0), stop=(c == 7), ins=[L_, "uT"])
                        k.act(s1[:, 0:n], p_ga[:, 0:n], AF.Sigmoid)
                        k.act(s2[:, 0:n], p_gs[:, 0:n], AF.Sigmoid)
                        k.act(s3[:, 0:n], p_lb[:, 0:n], AF.Sigmoid)
                        k.tt(m1[:, 0:n], p_ao[:, 0:n], s1[:, 0:n], ALU.mult)
                        k.tt(m2[:, 0:n], p_la[:, 0:n], s3[:, 0:n], ALU.mult)
                        k.tt(m2[:, 0:n], m2[:, 0:n], s2[:, 0:n], ALU.mult)
                        k.tt(mergedT[:, fc, tl], m1[:, 0:n], m2[:, 0:n], ALU.add, outs=[("mergedT", fc)])
                        if mi % 2 == 0:
                            conv_step(("mergedT", fc))
            while cvs["e"] <= 32:
                conv_step()
            with k.scope():
                wos = [k.sb("wos%d" % i, [128, 8, 256], F32) for i in range(2)]
                Wo = k.sb("Wo", [128, 8, 1024], BF16)
                wout_v = w_out.rearrange("(c p) n -> p c n", p=128)
                for j in range(4):
                    k.dma(wos[j % 2][:], wout_v[:, :, j * 256:(j + 1) * 256])
                    k.act(Wo[:, :, j * 256:(j + 1) * 256], wos[j % 2][:], AF.Copy, outs=[("Wo", j)])
                gffrow = k.sb("gffrow", [128, D], F32)
                k.dma(gffrow[:], gffrow_d)
                hnb = [k.sb("hnb%d" % i, [128, D], BF16) for i in range(2)]
                gff = k.sb("gff", [128, 8], F32)
                k.dma(gff[:], g_ffn)
                xt = [k.sb("xu%d" % i, [128, D], F32) for i in range(2)]
                ht = [k.sb("ht%d" % i, [128, D], F32) for i in range(2)]
                hb = [k.sb("hb%d" % i, [128, D], BF16) for i in range(2)]
                junk = k.sb("junk2", [128, D], BF16)
                ss = [k.sb("su%d" % i, [128, 1], F32) for i in range(2)]
                rs = [k.sb("ru%d" % i, [128, 1], F32) for i in range(2)]
                def mk_wo(ti, t0, n):
                    b = ti % 2
                    tl = slice(t0, t0 + n)
                    pbs = (nbank(), nbank())
                    ptb = nbank()[:].bitcast(BF16)

                    def w1():
                        k.dma(xt[b][0:n, :], x[tl, :])
                        for sl_ in range(2):
                            for c in range(8):
                                k.mm(pbs[sl_][0:n, :], mergedT[:, c, tl], Wo[:, c, sl_ * 512:(sl_ + 1) * 512],
                                     start=(c == 0), stop=(c == 7), ins=["mergedT", "Wo"])

                    def w2():
                        for sl_ in range(2):
                            k.tt(ht[b][0:n, sl_ * 512:(sl_ + 1) * 512], pbs[sl_][0:n, :], xt[b][0:n, sl_ * 512:(sl_ + 1) * 512], ALU.add)
                        k.dma(h_scr[tl, :], ht[b][0:n, :], q="pool")
                        k.act(junk[0:n, :], ht[b][0:n, :], AF.Square, accum_out=ss[b][0:n, :])
                        k.ts(rs[b][0:n, :], ss[b][0:n, :], 1.0 / D, 1e-6, ALU.mult, ALU.add)
                        k.act(rs[b][0:n, :], rs[b][0:n, :], AF.Sqrt)
                        k.recip(rs[b][0:n, :], rs[b][0:n, :])
                        k.act(hb[b][0:n, :], ht[b][0:n, :], AF.Copy, scale=rs[b][0:n, :])
                        k.tt(hnb[b][0:n, :], hb[b][0:n, :], gffrow[0:n, :], ALU.mult)
                        k.dma(hn_scr[tl, :], hnb[b][0:n, :], q="pool")
                        for c in range(8):
                            k.tr(ptb[:, c * 128:c * 128 + n], hb[b][0:n, c * 128:(c + 1) * 128], ident[0:n, 0:n])

                    def w3():
                        for c in range(8):
                            if c % 2 == 0:
                                k.act(xnT[:, c, tl], ptb[:, c * 128:c * 128 + n], AF.Copy, scale=gff[:, c:c + 1], outs=[("xnT", ti)])
                            else:
                                k.ts(xnT[:, c, tl], ptb[:, c * 128:c * 128 + n], gff[:, c:c + 1], None, ALU.mult, outs=[("xnT", ti)])

                    return (w1, w2, w3)

                wos_ = [mk_wo(ti, t0, n) for ti, (t0, n) in enumerate(tiles)]
                for step in range(len(wos_) + 2):
                    for si_, sk_ in enumerate((0, 1, 2)):
                        if 0 <= step - sk_ < len(wos_):
                            wos_[step - sk_][si_]()
        hnT = xnT
        with k.scope():
            gfin = k.sb("gfin", [128, D], F32)
            k.dma(gfin[:], gfin_d)
            A1s = k.sb("A1s", [128, 17, 32], F32); A2s = k.sb("A2s", [128, 17, 32], F32)
            WW = k.sb("WW", [128, 17, 2], F32)
            k.memset(A1s[:], 0.0); k.memset(A2s[:], 0.0); k.memset(WW[:], 0.0)
            with k.scope():
                rst = k.sb("rst", [128, 8, 36], F32)
                Wr = k.sb("Wr", [128, 8, 36], BF16)
                k.dma(rst[:, :, 0:4], w_rg.rearrange("(c p) n -> p c n", p=128))
                k.dma(rst[:, :, 4:36], w_re.rearrange("(c p) n -> p c n", p=128))
                k.cp(Wr[:], rst[:])
                rbias = k.sb("rbias", [128, 36], F32)
                k.dma(rbias[:], rb_d)
                LG = k.sb("LG", [128, 17, 36], F32)
                k.memset(LG[:], 0.0)
                for ti, (t0, n) in enumerate(tiles):
                    pb = nbank()
                    for c in range(8):
                        k.mm(pb[0:n, 0:36], hnT[:, c, t0:t0 + n], Wr[:, c, :], start=(c == 0), stop=(c == 7), ins=["xnT", Wr])
                    k.tt(LG[0:n, ti, :], pb[0:n, 0:36], rbias[0:n, :], ALU.add, outs=[("LG", ti)])

                def R(name, shape):
                    return k.sb(name, shape, F32)

                def red(out, in_, op):
                    return k.s.add("dve", lambda h: h.tensor_reduce(out, in_, AX.X, op), [in_], [out])

                def b3(ap2, n3):
                    return ap2.unsqueeze(2).to_broadcast([128, 17, n3])
                lg4 = LG[:, :, 0:4]
                le4 = LG[:, :, 4:36].rearrange("p t (g e) -> p t g e", e=8)
                mx = R("r_mx", [128, 17]); ohb = R("r_oh", [128, 17, 4]); e4b = R("r_e4", [128, 17, 4])
                se = R("r_se", [128, 17]); pg = R("r_pg", [128, 17])
                red(mx[:], lg4, ALU.max)
                k.tt(ohb[:], lg4, b3(mx[:], 4), ALU.is_equal)
                k.tt(e4b[:], lg4, b3(mx[:], 4), ALU.subtract)
                k.act(e4b[:], e4b[:], AF.Exp)
                red(se[:], e4b[:], ALU.add)
                k.recip(pg[:], se[:])
                legb = R("r_leg", [128, 17, 8]); tmp8 = R("r_t8", [128, 17, 8])
                for g_ in range(4):
                    ohg = ohb[:, :, g_:g_ + 1].to_broadcast([128, 17, 8])
                    if g_ == 0:
                        k.tt(legb[:], le4[:, :, 0, :], ohg, ALU.mult)
                    else:
                        k.tt(tmp8[:], le4[:, :, g_, :], ohg, ALU.mult)
                        k.tt(legb[:], legb[:], tmp8[:], ALU.add)
                v1 = R("r_v1", [128, 17]); v2 = R("r_v2", [128, 17]); m1b = R("r_m1", [128, 17, 8]); m2b = R("r_m2", [128, 17, 8])
                red(v1[:], legb[:], ALU.max)
                k.tt(m1b[:], legb[:], b3(v1[:], 8), ALU.is_equal)
                k.stt(tmp8[:], m1b[:], -1.0e30, legb[:], ALU.mult, ALU.add)
                red(v2[:], tmp8[:], ALU.max)
                k.tt(m2b[:], tmp8[:], b3(v2[:], 8), ALU.is_equal)
                ex = R("r_ex", [128, 17]); w1_ = R("r_w1", [128, 17]); w2_ = R("r_w2", [128, 17])
                k.tt(ex[:], v2[:], v1[:], ALU.subtract)
                k.act(ex[:], ex[:], AF.Exp)
                k.ts(w1_[:], ex[:], 1.0, None, ALU.add)
                k.recip(w1_[:], w1_[:])
                k.tt(w2_[:], ex[:], w1_[:], ALU.mult)
                k.tt(WW[:, :, 0], w1_[:], pg[:], ALU.mult)
                k.tt(WW[:, :, 1], w2_[:], pg[:], ALU.mult)
                for g_ in range(4):
                    ohg = ohb[:, :, g_:g_ + 1].to_broadcast([128, 17, 8])
                    k.tt(A1s[:, :, g_ * 8:(g_ + 1) * 8], m1b[:], ohg, ALU.mult)
                    k.tt(A2s[:, :, g_ * 8:(g_ + 1) * 8], m2b[:], ohg, ALU.mult)
                k.memset(A1s[NS:128, 16, :], 0.0)
                k.memset(A2s[NS:128, 16, :], 0.0)
                k.memset(WW[NS:128, 16, :], 0.0)
            NB = 49
            SLi = [k.sb("SLi%d" % i, [128, 17], I32) for i in range(2)]
            BEi = k.sb("BEi", [128, NB], I32)
            with k.scope():
                Ab = k.sb("Ab", [128, 17, 32], BF16)
                Asum = k.sb("Asum", [128, 17, 32], F32)
                k.tt(Asum[:], A1s[:], A2s[:], ALU.add)
                k.cp(Ab[:], Asum[:])
                onesb = k.sb("onesb", [128, 128], BF16); k.memset(onesb[:], 1.0)
                trif = k.sb("trif", [128, 128], F32); trib = k.sb("trib", [128, 128], BF16)
                k.dma(trif[:], tri_d); k.cp(trib[:], trif[:])
                CS = k.sb("CS", [128, 17, 32], F32); RK = k.sb("RK", [128, 17, 32], F32); OFF = k.sb("OFF", [128, 17, 32], F32)
                Abf = Ab[:].rearrange("p t e -> p (t e)")
                for (lhs, dst) in ((onesb, CS), (trib, RK)):
                    p0, p1 = nbank(), nbank()
                    k.mm(p0[:, 0:512], lhs[:], Abf[:, 0:512])
                    k.mm(p1[:, 0:32], lhs[:], Abf[:, 512:544])
                    dflat = dst[:].rearrange("p t e -> p (t e)")
                    k.cp(dflat[:, 0:512], p0[:, 0:512])
                    k.cp(dflat[:, 512:544], p1[:, 0:32])
                k.memset(OFF[:, 0, :], 0.0)
                for ti in range(1, 17):
                    k.tt(OFF[:, ti, :], OFF[:, ti - 1, :], CS[:, ti - 1, :], ALU.add)
                CNT = k.sb("CNT", [128, 32], F32); NBK = k.sb("NBK", [128, 32], F32); NBI = k.sb("NBI", [128, 32], I32)
                PEND = k.sb("PEND", [128, 32], F32); PST = k.sb("PST", [128, 32], F32); ONE32 = k.sb("ONE32", [128, 32], F32)
                k.memset(ONE32[:], 1.0)
                k.tt(CNT[:], OFF[:, 16, :], CS[:, 16, :], ALU.add)
                k.ts(NBK[:], CNT[:], 1.0 / 256.0, 255.0 / 256.0 - 0.498046875, ALU.mult, ALU.add)
                k.cp(NBI[:], NBK[:])
                k.cp(NBK[:], NBI[:])
                k.s.add("dve", lambda h: h.tensor_tensor_scan(PEND[:], ONE32[:], NBK[:], 0.0, ALU.mult, ALU.add), [ONE32, NBK], [PEND])
                k.tt(PST[:], PEND[:], NBK[:], ALU.subtract)
                SLT = k.sb("SLT", [128, 17, 32], F32)
                k.tt(SLT[:], OFF[:], RK[:], ALU.add)
                k.ts(PST[:], PST[:], 256.0, None, ALU.mult)
                k.tt(SLT[:], SLT[:], PST[:].unsqueeze(1).to_broadcast([128, 17, 32]), ALU.add)
                SL = [k.sb("SL%d" % i, [128, 17], F32) for i in range(2)]
                for i_, Ax in enumerate((A1s, A2s)):
                    k.tt(Asum[:], Ax[:], SLT[:], ALU.mult)
                    k.s.add("dve", (lambda o_, i2: (lambda h: h.tensor_reduce(o_, i2, AX.X, ALU.add)))(SL[i_][:], Asum[:]), [Asum], [SL[i_]])
                    k.cp(SLi[i_][:], SL[i_][:])
                BIX = k.sb("BIX", [128, NB, 32], F32)
                k.dma(BIX[:], bidx_d)
                k.tt(BIX[:], PEND[:].unsqueeze(1).to_broadcast([128, NB, 32]), BIX[:], ALU.is_le)
                BE = k.sb("BE", [128, NB], F32)
                k.s.add("dve", lambda h: h.tensor_reduce(BE[:], BIX[:], AX.X, ALU.add), [BIX], [BE])
                pcol = k.sb("pcol", [128, 1], F32); k.dma(pcol[:], pcol_d)
                k.ts(BE[:], BE[:], 128.0, pcol[:, 0:1], ALU.mult, ALU.add)
                k.cp(BEi[:], BE[:])
            with k.scope():
                hld = [k.sb("hld%d" % i, [128, D], BF16) for i in range(4)]
                for ti, (t0, n) in enumerate(tiles):
                    hb_ = hld[ti % 4]
                    k.dma(hb_[0:n, :], hn_scr[t0:t0 + n, :])
                    for i_ in range(2):
                        k.s.add("pool", (lambda src, idx: (lambda h: h.indirect_dma_start(
                            out=xs_scr[:, :], out_offset=bass.IndirectOffsetOnAxis(ap=idx, axis=0), in_=src, in_offset=None)))(
                            hb_[0:n, :], SLi[i_][0:n, ti:ti + 1]), [hb_, SLi[i_]], [("xs_scr", 2 * ti + i_)], dma=True, semkey=(hb_.name, i_))
                wbe = [k.sb("wbe%d" % i, [128, 6144], BF16) for i in range(4)]
                for i in range(4):
                    k.memset(wbe[i][:], 0.0)
                xsb = [k.sb("xsb%d" % i, [128, 2, D], BF16) for i in range(3)]
                xsT = [k.sb("xsT%d" % i, [128, 8, 256], BF16) for i in range(2)]
                hmT = [k.sb("hmT%d" % i, [128, 2, 256], BF16) for i in range(2)]
                sgt = [k.sb("sgt%d" % i, [128, 256], F32) for i in range(2)]
                ybt = [k.sb("ybt%d" % i, [128, D], BF16) for i in range(2)]
                _bcc = {}

                def _bc(h):
                    if "r" not in _bcc:
                        _bcc["r"] = h.to_reg(4095)
                    return _bcc["r"]

                def mk_blk(bidx):
                    wb_ = wbe[bidx % 4]
                    Wg_ = wb_[:, 0:2048].rearrange("p (c n) -> p c n", c=8)
                    Wu_ = wb_[:, 2048:4096].rearrange("p (c n) -> p c n", c=8)
                    Wd_ = wb_[:, 4096:6144].rearrange("p (c n) -> p c n", c=2)
                    xb_, xT_, hm_ = xsb[bidx % 3], xsT[bidx % 2], hmT[bidx % 2]

                    def bl():
                        k.dma(xb_[:], xs_scr[bidx * 256:(bidx + 1) * 256, :].rearrange("(s p) d -> p s d", p=128), ins=["xs_scr"])

                    def bg():
                        k.s.add("pool", (lambda idx_: (lambda h: h.indirect_dma_start(
                            out=wb_[:, :], out_offset=None, in_=w_bf[:, :], in_offset=bass.IndirectOffsetOnAxis(ap=idx_, axis=0),
                            bounds_check=_bc(h), oob_is_err=False)))(BEi[:, bidx:bidx + 1]), [BEi, "w_bf"], [wb_], dma=True, semkey=wb_)

                    def b0():
                        for sub in range(2):
                            ptb = nbank()[:].bitcast(BF16)
                            for c in range(8):
                                k.tr(ptb[:, c * 128:(c + 1) * 128], xb_[:, sub, c * 128:(c + 1) * 128], ident[:])
                            if sub == 0:
                                k.act(xT_[:, :, 0:128], ptb.rearrange("p (c x) -> p c x", c=8), AF.Copy)
                            else:
                                k.cp(xT_[:, :, 128:256], ptb.rearrange("p (c x) -> p c x", c=8))

                    def b1():
                        for fcx in range(2):
                            pg_, pu_ = nbank(), nbank()
                            for c in range(8):
                                k.mm(pg_[:, 0:256], Wg_[:, c, fcx * 128:(fcx + 1) * 128], xT_[:, c, :], start=(c == 0), stop=(c == 7))
                            for c in range(8):
                                k.mm(pu_[:, 0:256], Wu_[:, c, fcx * 128:(fcx + 1) * 128], xT_[:, c, :], start=(c == 0), stop=(c == 7))
                            sg_ = sgt[fcx]
                            k.act(sg_[:], pg_[:, 0:256], AF.Silu)
                            k.tt(hm_[:, fcx, :], sg_[:], pu_[:, 0:256], ALU.mult)

                    def b2():
                        for sub in range(2):
                            yb_ = ybt[sub]
                            for sl_ in range(2):
                                py = nbank()
                                for fcx in range(2):
                                    k.mm(py[:, :], hm_[:, fcx, sub * 128:(sub + 1) * 128], Wd_[:, fcx, sl_ * 512:(sl_ + 1) * 512],
                                         start=(fcx == 0), stop=(fcx == 1))
                                if sub == 0:
                                    k.act(yb_[:, sl_ * 512:(sl_ + 1) * 512], py[:, :], AF.Copy)
                                else:
                                    k.cp(yb_[:, sl_ * 512:(sl_ + 1) * 512], py[:, :])
                            k.dma(ys_scr[bidx * 256 + sub * 128:bidx * 256 + (sub + 1) * 128, :], yb_[:], outs=["ys_scr"],
                                  q="act")
                    return (bl, bg, b0, b1, b2)

                blks = [mk_blk(b_) for b_ in range(NB)]
                for step in range(NB + 4):
                    for si_, sk_ in enumerate((0, 1, 2, 3, 4)):
                        if 0 <= step - sk_ < NB:
                            blks[step - sk_][si_]()
            with k.scope():
                ygt = [k.sb("ygt%d" % i, [128, D], BF16) for i in range(4)]
                hfin = [k.sb("hfin%d" % i, [128, D], F32) for i in range(2)]
                yo = [k.sb("yo%d" % i, [128, D], F32) for i in range(2)]
                junk = k.sb("junk3", [128, D], BF16)
                fs = [k.sb("fs%d" % i, [128, 1], F32) for i in range(2)]
                for ti, (t0, n) in enumerate(tiles):
                    b = ti % 2
                    k.dma(hfin[b][0:n, :], h_scr[t0:t0 + n, :])
                    for i_ in range(2):
                        yg = ygt[(2 * ti + i_) % 4]
                        k.s.add("pool", (lambda dst, idx_: (lambda h: h.indirect_dma_start(
                            out=dst, out_offset=None, in_=ys_scr[:, :], in_offset=bass.IndirectOffsetOnAxis(ap=idx_, axis=0))))(
                            yg[0:n, :], SLi[i_][0:n, ti:ti + 1]), ["ys_scr", SLi[i_]], [yg], dma=True, semkey=yg)
                        k.stt(hfin[b][0:n, :], yg[0:n, :], WW[0:n, ti, i_:i_ + 1], hfin[b][0:n, :], ALU.mult, ALU.add)
                    k.act(junk[0:n, :], hfin[b][0:n, :], AF.Square, accum_out=fs[b][0:n, :])
                    k.ts(fs[b][0:n, :], fs[b][0:n, :], 1.0 / D, 1e-6, ALU.mult, ALU.add)
                    k.act(fs[b][0:n, :], fs[b][0:n, :], AF.Sqrt)
                    k.recip(fs[b][0:n, :], fs[b][0:n, :])
                    k.stt(yo[b][0:n, :], hfin[b][0:n, :], fs[b][0:n, :], gfin[0:n, :], ALU.mult, ALU.mult)
                    k.dma(y_out[t0:t0 + n, :], yo[b][0:n, :], q="act")

        k.s.emit()
    return nc


_NC = {}


def _host_consts():
    pos = np.concatenate([np.arange(NPT), np.tile(2048 + np.arange(4), 16)]).astype(np.float32)
    half = 8
    inv = (500000.0 ** (-(np.arange(half, dtype=np.float32) / half))).astype(np.float32)
    ang = pos[:, None] * inv[None, :]
    rope = np.concatenate([np.cos(ang), np.sin(ang)], axis=1).astype(np.float32)
    diag = np.zeros((128, 32), np.float32)
    for gq in range(4):
        for c in range(16):
            diag[32 * gq + c, c] = 1.0
    NEG = -30000.0
    masks = np.zeros((128, 384), np.float32)
    kk = np.arange(128)[:, None]
    qq = np.arange(128)[None, :]
    masks[:, 0:128] = np.where(kk <= qq, 0.0, NEG)
    masks[:, 128:256] = np.where(kk >= qq, 0.0, NEG)
    k64 = np.arange(128)[:, None]
    q64 = np.arange(64)[None, :]
    same = (k64 // 4 == q64 // 4) & (k64 < 64)
    masks[:, 256:320] = np.where(same & (k64 % 4 <= q64 % 4), 0.0, NEG)
    masks[:, 320:384] = np.where(k64 == q64, 0.0, NEG)
    return rope, diag, masks


def kernel(_dbg=False, **inp):
    if _dbg not in _NC:
        _NC[_dbg] = build(_dbg)
    nc = _NC[_dbg]
    f = lambda a: np.ascontiguousarray(np.asarray(a, dtype=np.float32))
    rope, diag, masks = _host_consts()
    ident = np.eye(128, dtype=np.float32)
    w_in = f(inp["w_in"][0])
    g_attn = f(np.asarray(inp["g_attn_norm"][0]).reshape(8, 128).T)
    xp = np.asarray(inp["x_prompt"])
    xs = np.asarray(inp["x_sample"])
    s_are = f(np.asarray(inp["ssm_a_re"][0]).T)
    s_aim = f(np.asarray(inp["ssm_a_im"][0]).T)
    s_ldt = f(np.broadcast_to(np.asarray(inp["ssm_log_dt"][0])[None, :], (64, 32)))
    s_bre = f(np.transpose(np.asarray(inp["ssm_b_re"][0]), (1, 0, 2)))
    s_bim = f(np.transpose(np.asarray(inp["ssm_b_im"][0]), (1, 0, 2)))
    s_cre = f(np.transpose(np.asarray(inp["ssm_c_re"][0]), (2, 0, 1)))
    s_cim = f(np.transpose(np.asarray(inp["ssm_c_im"][0]), (2, 0, 1)))
    dd = np.asarray(inp["ssm_d"][0])
    s_d = np.zeros((128, 8), np.float32)
    for g in range(32):
        s_d[32 * (g % 4):32 * (g % 4) + 16, g // 4] = dd[g]
    st_all = np.asarray(inp["state_ssm"][0])
    common = {"rope": rope, "ident_in": ident, "diag_in": diag, "g_attn_in": g_attn, "w_in": w_in,
              "s_are": s_are, "s_aim": s_aim, "s_ldt": s_ldt, "s_bre": s_bre, "s_bim": s_bim,
              "s_cre": s_cre, "s_cim": s_cim, "s_d": s_d, "masks_in": masks}
    weg = np.asarray(inp["w_exp_gate"][0]).reshape(32, 8, 128, 256).transpose(0, 2, 1, 3).reshape(32, 128, 2048)
    weu = np.asarray(inp["w_exp_up"][0]).reshape(32, 8, 128, 256).transpose(0, 2, 1, 3).reshape(32, 128, 2048)
    wed = np.asarray(inp["w_exp_down"][0]).reshape(32, 2, 128, 1024).transpose(0, 2, 1, 3).reshape(32, 128, 2048)
    w_all = f(np.concatenate([weg, weu, wed], axis=2).reshape(4096, 6144))
    tri = (np.arange(128)[:, None] < np.arange(128)[None, :]).astype(np.float32)
    bidx = f(np.broadcast_to(np.arange(49, dtype=np.float32)[None, :, None], (128, 49, 32)))
    pcol = np.arange(128, dtype=np.float32).reshape(128, 1)
    common.update({"w_ab": f(inp["w_attn_branch"][0]), "w_glu": f(inp["w_glu"][0]), "w_out": f(inp["w_out"][0]),
                   "g_ffn_in": f(np.asarray(inp["g_ffn_norm"][0]).reshape(8, 128).T),
                   "w_rg": f(inp["w_router_group"][0]), "w_re": f(inp["w_router_expert"][0]),
                   "rbias_in": f(np.broadcast_to(np.concatenate([np.asarray(inp["b_router_group"][0]), np.asarray(inp["b_router_expert"][0])])[None, :], (128, 36))),
                   "gfin_in": f(np.broadcast_to(np.asarray(inp["g_final"])[None, :], (128, 1024))),
                   "w_all": w_all, "tri_in": tri, "bidx_in": bidx, "pcol_in": pcol,
                   "gffrow_in": f(np.broadcast_to(np.asarray(inp["g_ffn_norm"][0])[None, :], (128, 1024)))})
    c128 = np.asarray(inp["cache_kv_w128"][0])
    c512 = np.asarray(inp["cache_kv_w512"][0])
    c2048 = np.asarray(inp["cache_kv_w2048"][0])
    in_maps = []
    for c in range(8):
        xc = np.concatenate([xp[c], xs[16 * c:16 * c + 16].reshape(64, D)], axis=0)
        h0 = f(np.transpose(st_all[16 * c:16 * c + 16], (2, 3, 0, 1)))
        m = dict(common)
        sl = slice(16 * c, 16 * c + 16)
        m.update({"x": f(xc), "s_h0": h0,
                  "cache0": f(c128[sl]),
                  "cache1": f(c512[sl].reshape(16, 128, 4, 2, 8, 64)),
                  "cache2": f(c2048[sl].reshape(16, 128, 16, 2, 8, 64)[:, :, 0:4])})
        in_maps.append(m)
    res = run_bass_kernel_spmd(nc, in_maps, core_ids=list(range(8)))
    R = res.results
    if _dbg:
        kernel.dbg = R
    outs = []
    y_prompt = np.stack([R[c]["y_out"][0:NPT] for c in range(8)], axis=0)
    y_sample = np.concatenate([R[c]["y_out"][NPT:NT].reshape(16, 4, D) for c in range(8)], axis=0)
    outs += [y_prompt, y_sample]
    for g in range(3):
        outs.append(np.stack([R[c]["kvp%d" % g] for c in range(8)], axis=0)[None])
        outs.append(np.concatenate([R[c]["kvs%d" % g] for c in range(8)], axis=0)[None])
    sp = np.stack([np.transpose(R[c]["ssm_p"], (2, 0, 1)) for c in range(8)], axis=0)[None]
    ssv = np.concatenate([np.transpose(R[c]["ssm_s"], (2, 3, 0, 1)) for c in range(8)], axis=0)[None]
    outs.append(np.ascontiguousarray(sp.astype(np.float32)))
    outs.append(np.ascontiguousarray(ssv.astype(np.float32)))
    return tuple(outs)
```

```python
import numpy as np
from contextlib import ExitStack
import concourse.bass as bass
import concourse.mybir as mybir
from concourse.alu_op_type import AluOpType as ALU
from concourse.bass_utils import run_bass_kernel_spmd

F32 = mybir.dt.float32
BF16 = mybir.dt.bfloat16
I32 = mybir.dt.int32
AF = mybir.ActivationFunctionType
AX = mybir.AxisListType

NT = 2112
NPT = 2048
NS = 64
D = 1024
GROUPS = ((128, 1), (512, 4), (2048, 16))


class _Op:
    __slots__ = ("eng", "fn", "deps", "dma", "signal", "cnt", "sem", "val", "done", "isbar", "g")


class Sched:
    def __init__(self, nc, stack):
        self.nc = nc
        self.stack = stack
        self.eng = {"pe": nc.tensor, "act": nc.scalar, "dve": nc.vector, "pool": nc.gpsimd, "sp": nc.sync}
        self.ops = {e: [] for e in self.eng}
        self.state = {}
        self.dsem = {}
        self.last_dma = {}
        self.gcount = 0

    @staticmethod
    def _key(a):
        if isinstance(a, tuple):
            return a
        if isinstance(a, str):
            return (a, None)
        if hasattr(a, "tensor"):
            return (a.tensor.name, None)
        return (a.name, None)

    def _entries(self, key, create=True):
        name, sub = key
        d = self.state.setdefault(name, {})
        if create and sub not in d:
            d[sub] = [None, []]
        return [(s, e) for s, e in d.items() if s == sub or s is None or sub is None]

    def add(self, eng, fn, ins=(), outs=(), dma=False, semkey=None):
        op = _Op()
        op.eng, op.fn, op.dma, op.signal, op.deps = eng, fn, dma, False, set()
        op.cnt = op.val = 0
        op.sem = None
        for a in ins:
            if a is None:
                continue
            k = self._key(a)
            for s, e in self._entries(k):
                if e[0] is not None:
                    op.deps.add(e[0])
            self.state[k[0]][k[1]][1].append(op)
        for a in outs:
            if a is None:
                continue
            k = self._key(a)
            for s, e in self._entries(k):
                if e[0] is not None:
                    op.deps.add(e[0])
                for r in e[1]:
                    op.deps.add(r)
                if s == k[1] or k[1] is None:
                    e[0] = op
                    e[1] = []
            d = self.state[k[0]]
            for s in list(d.keys()):
                if s == k[1] or k[1] is None:
                    d[s] = [op, []]
        op.deps.discard(op)
        if dma:
            sk = self._key(semkey)
            if sk not in self.dsem:
                self.dsem[sk] = [self.stack.enter_context(self.nc.semaphore("d%d" % len(self.dsem))), 0]
            ent = self.dsem[sk]
            ent[1] += 16
            op.sem, op.val = ent[0], ent[1]
            self.last_dma[sk] = op
        op.g = self.gcount
        self.gcount += 1
        self.ops[eng].append(op)
        return op

    def interleave(self, g0, g1, g2):
        la, lb = g1 - g0, g2 - g1
        if la == 0 or lb == 0:
            return

        def pos(op):
            if op.g < g1:
                return (op.g - g0) * (la + lb) / la
            return (op.g - g1) * (la + lb) / lb + 0.5
        for e, lst in self.ops.items():
            head = [o for o in lst if getattr(o, "isbar", False) or o.g < g0]
            tail = [o for o in lst if not getattr(o, "isbar", False) and o.g >= g0]
            tail.sort(key=pos)
            self.ops[e] = head + tail

    def barrier(self, full=True):
        deps = set()
        for e, lst in self.ops.items():
            for op in reversed(lst):
                if not op.dma and not getattr(op, "isbar", False):
                    deps.add(op)
                    break
        bg = getattr(self, "bg_keys", set())
        deps |= set(op for sk, op in self.last_dma.items() if full or sk not in bg)
        for e in self.ops:
            op = _Op()
            op.eng, op.fn, op.dma, op.signal, op.deps = e, None, False, False, set(deps)
            op.cnt = op.val = 0
            op.sem = None
            op.isbar = True
            op.g = self.gcount
            self.ops[e].append(op)
        self.emit(final=False)

    def emit(self, final=True):
        nc = self.nc
        if not hasattr(self, "esem"):
            self.esem = {e: self.stack.enter_context(nc.semaphore("e_" + e)) for e in self.eng}
            self.ecount = {e: 0 for e in self.eng}
        esem = self.esem
        for e, lst in self.ops.items():
            for op in lst:
                for d in op.deps:
                    if d.dma or getattr(d, "done", False):
                        continue
                    if d.eng == "pe" and op.eng == "pe" and not op.dma:
                        continue
                    d.signal = True
        for e, lst in self.ops.items():
            c = self.ecount[e]
            for op in lst:
                if not op.dma and op.signal:
                    c += 1
                    op.sem, op.val = esem[e], c
            self.ecount[e] = c
        sched = self
        if not hasattr(self, "waited"):
            self.waited = {e: {} for e in self.eng}

        def run(e, h):
            waited = sched.waited[e]
            for op in sched.ops[e]:
                need = {}
                for d in op.deps:
                    if not d.dma and d.eng == "pe" and e == "pe" and not op.dma:
                        continue
                    if d.sem is None:
                        continue
                    sid = id(d.sem)
                    if sid not in need or need[sid][1] < d.val:
                        need[sid] = (d.sem, d.val)
                for sid, (s, v) in need.items():
                    if waited.get(sid, 0) < v:
                        h.wait_ge(s, v)
                        waited[sid] = v
                if op.fn is None:
                    continue
                ins = op.fn(h)
                if op.dma:
                    ins.then_inc(op.sem, 16)
                elif op.signal:
                    ins.then_inc(op.sem, 1)
            if e == "sp" and final:
                for sk, (s, tot) in sched.dsem.items():
                    if tot > 0:
                        h.wait_ge(s, tot)

        with nc.Block() as block:
            @block.tensor
            def _(h):
                run("pe", h)

            @block.scalar
            def _(h):
                run("act", h)

            @block.vector
            def _(h):
                run("dve", h)

            @block.gpsimd
            def _(h):
                run("pool", h)

            @block.sync
            def _(h):
                run("sp", h)
        for e in self.ops:
            for op in self.ops[e]:
                op.done = True
                op.fn = None
            self.ops[e] = []


class _Scope:
    def __init__(self, k, full=False):
        self.k = k
        self.full = full

    def __enter__(self):
        self.old = self.k.st
        self.es = ExitStack()
        self.es.__enter__()
        self.k.st = self.es
        return self

    def __exit__(self, *a):
        self.k.s.barrier(full=self.full)
        self.k.st = self.old
        return self.es.__exit__(*a)


def _is_sb(ap):
    return type(ap.tensor).__name__.startswith("SB")


class K:
    def __init__(self, nc, stack):
        self.nc = nc
        self.st = stack
        self.s = Sched(nc, stack)
        self.nps = 0

    def scope(self, full=False):
        return _Scope(self, full)

    def sb(self, name, shape, dt):
        return self.st.enter_context(self.nc.sbuf_tensor(name, list(shape), dt))

    def ps(self, name, shape, dt=F32):
        return self.st.enter_context(self.nc.psum_tensor(name, list(shape), dt))

    def dma(self, out, in_, q="sp", ins=None, outs=None, **kw):
        semkey = out if _is_sb(out) else in_
        if outs is not None and _is_sb(out):
            semkey = outs[0]
        elif ins is not None and not _is_sb(out):
            semkey = ins[0]
        return self.s.add(q, lambda h: h.dma_start(out=out, in_=in_, **kw),
                          ins if ins is not None else [in_], outs if outs is not None else [out],
                          dma=True, semkey=semkey)

    def mm(self, out, lhsT, rhs, start=True, stop=True, ins=None, outs=None, **kw):
        return self.s.add("pe", lambda h: h.matmul(out, lhsT, rhs, start=start, stop=stop, **kw),
                          ins if ins is not None else [lhsT, rhs], outs if outs is not None else [out])

    def tr(self, out, in_, ident, ins=None, outs=None):
        return self.s.add("pe", lambda h: h.transpose(out, in_, ident),
                          ins if ins is not None else [in_, ident], outs if outs is not None else [out])

    def act(self, out, in_, func, bias=None, scale=None, accum_out=None, ins=None, outs=None):
        kw = {}
        if bias is not None:
            kw["bias"] = bias
        if scale is not None:
            kw["scale"] = scale
        if accum_out is not None:
            kw["accum_out"] = accum_out
        i = [in_]
        for x in (bias, scale):
            if x is not None and not isinstance(x, (int, float)):
                i.append(x)
        o = [out] + ([accum_out] if accum_out is not None else [])
        return self.s.add("act", lambda h: h.activation(out, in_, func, **kw),
                          ins if ins is not None else i, outs if outs is not None else o)

    def tt(self, out, in0, in1, op, eng="dve", ins=None, outs=None):
        return self.s.add(eng, lambda h: h.tensor_tensor(out, in0, in1, op),
                          ins if ins is not None else [in0, in1], outs if outs is not None else [out])

    def ts(self, out, in0, s1, s2=None, op0=ALU.mult, op1=None, eng="dve", ins=None, outs=None):
        i = [in0] + [x for x in (s1, s2) if x is not None and not isinstance(x, (int, float))]
        if op1 is None:
            fn = lambda h: h.tensor_scalar(out, in0, s1, None, op0)
        else:
            fn = lambda h: h.tensor_scalar(out, in0, s1, s2, op0, op1)
        return self.s.add(eng, fn, ins if ins is not None else i, outs if outs is not None else [out])

    def stt(self, out, in0, scalar, in1, op0, op1, ins=None, outs=None):
        i = [in0, in1] + ([scalar] if not isinstance(scalar, (int, float)) else [])
        return self.s.add("dve", lambda h: h.scalar_tensor_tensor(out, in0, scalar, in1, op0, op1),
                          ins if ins is not None else i, outs if outs is not None else [out])

    def cp(self, out, in_, eng="dve", ins=None, outs=None):
        return self.s.add(eng, lambda h: h.tensor_copy(out, in_),
                          ins if ins is not None else [in_], outs if outs is not None else [out])

    def memset(self, ap, v, eng="dve"):
        return self.s.add(eng, lambda h: h.memset(ap, v), [], [ap])

    def recip(self, out, in_):
        return self.s.add("dve", lambda h: h.reciprocal(out, in_), [in_], [out])


TWO_PI = 6.283185
PW_SLOTS = list(range(9)) + [-4]


def build(dbg=False):
    nc = bass.Bass("TRN2", target_bir_lowering=False)
    dr = {}

    def din(name, shape, dt=F32):
        dr[name] = nc.dram_tensor(name, list(shape), dt, kind="ExternalInput").ap()
        return dr[name]

    def dout(name, shape, dt=F32):
        dr[name] = nc.dram_tensor(name, list(shape), dt, kind="ExternalOutput").ap()
        return dr[name]

    x = din("x", [NT, D])
    rope = din("rope", [NT, 16])
    ident_d = din("ident_in", [128, 128])
    diag_d = din("diag_in", [128, 32])
    g_attn = din("g_attn_in", [128, 8])
    w_in = din("w_in", [D, 7168])
    s_are = din("s_are", [64, 32]); s_aim = din("s_aim", [64, 32]); s_ldt = din("s_ldt", [64, 32])
    s_bre = din("s_bre", [64, 32, 16]); s_bim = din("s_bim", [64, 32, 16])
    s_cre = din("s_cre", [64, 32, 16]); s_cim = din("s_cim", [64, 32, 16])
    s_d = din("s_d", [128, 8])
    s_h0 = din("s_h0", [64, 2, 16, 32])
    masks_d = din("masks_in", [128, 384])
    w_ab = din("w_ab", [512, 1024]); w_glu = din("w_glu", [512, 2048]); w_out = din("w_out", [1024, 1024])
    g_ffn = din("g_ffn_in", [128, 8])
    w_rg = din("w_rg", [1024, 4]); w_re = din("w_re", [1024, 32]); rb_d = din("rbias_in", [128, 36])
    gfin_d = din("gfin_in", [128, 1024])
    w_all = din("w_all", [4096, 6144])
    tri_d = din("tri_in", [128, 128]); bidx_d = din("bidx_in", [128, 49, 32]); pcol_d = din("pcol_in", [128, 1])
    gffrow_d = din("gffrow_in", [128, 1024])
    hn_scr = nc.dram_tensor("hn_scr", [NT, D], BF16, kind="Internal").ap()
    rt_scr = nc.dram_tensor("rt_scr", [NT, 66], F32, kind="Internal").ap()
    sc_LAMr = nc.dram_tensor("sc_LAMr", [64, 10, 32], F32, kind="Internal").ap()
    sc_LAMi = nc.dram_tensor("sc_LAMi", [64, 10, 32], F32, kind="Internal").ap()
    sc_OutW = nc.dram_tensor("sc_OutW", [128, 8, 8, 4, 32], BF16, kind="Internal").ap()
    sc_Cm = nc.dram_tensor("sc_Cm", [128, 8, 4, 32], BF16, kind="Internal").ap()
    sc_KW = nc.dram_tensor("sc_KW", [128, 8, 8, 32], BF16, kind="Internal").ap()
    sc_SWc = nc.dram_tensor("sc_SWc", [128, 8, 8, 128], BF16, kind="Internal").ap()
    w_bf = nc.dram_tensor("w_bf", [4096, 6144], BF16, kind="Internal").ap()
    xs_scr = nc.dram_tensor("xs_scr", [49 * 256, D], BF16, kind="Internal").ap()
    ys_scr = nc.dram_tensor("ys_scr", [49 * 256, D], BF16, kind="Internal").ap()
    h_scr = nc.dram_tensor("h_scr", [NT, D], F32, kind="Internal").ap()
    y_out = dout("y_out", [NT, D])
    caches = [din("cache0", [16, 128, 2, 8, 64]), din("cache1", [16, 128, 4, 2, 8, 64]), din("cache2", [16, 128, 4, 2, 8, 64])]
    kvp = [dout("kvp0", [128, 2, 8, 64]), dout("kvp1", [512, 2, 8, 64]), dout("kvp2", [2048, 2, 8, 64])]
    kvs = [dout("kvs%d" % g, [16, 4, 2, 8, 64]) for g in range(3)]
    ssm_p = dout("ssm_p", [64, 2, 32])
    ssm_s = dout("ssm_s", [64, 2, 16, 32])
    if dbg:
        dbg_gy = dout("dbg_gy", [128, 8, NT], BF16)
        dbg_at = dout("dbg_at", [128, 4, NT], BF16)

    with ExitStack() as st:
        k = K(nc, st)
        ident = k.sb("ident", [128, 128], BF16)
        with k.scope():
            ident_f = k.sb("ident_f", [128, 128], F32)
            k.dma(ident_f[:], ident_d)
            k.cp(ident[:], ident_f[:])
        diag32 = k.sb("diag32", [128, 32], F32)
        k.dma(diag32[:], diag_d)
        gat = k.sb("gat", [128, 8], F32)
        k.dma(gat[:], g_attn)
        xnT = k.sb("xnT", [128, 8, NT], BF16)
        psb = [k.ps("psb%d" % i, [128, 512], F32) for i in range(8)]
        w_in_v = w_in.rearrange("(c p) n -> p c n", p=128)
        cvt = []
        cvs = {"e": 0}

        def conv_step(dep=None):
            e = cvs["e"]
            if e < 32:
                k.dma(cvt[e % 2][:].rearrange("p (a x) -> p a x", x=2048),
                      w_all[e * 128:(e + 1) * 128, :].rearrange("p (a x) -> p a x", x=2048), q="pool",
                      ins=([dep] if dep is not None else []))
            if 1 <= e <= 32:
                k.dma(w_bf[(e - 1) * 128:e * 128, :], cvt[(e - 1) % 2][:], q="pool", outs=[("w_bf", e)])
            cvs["e"] = e + 1

        tiles = [(i * 128, 128) for i in range(16)] + [(NPT, NS)]
        with k.scope():
            xt = [k.sb("xt%d" % i, [128, D], F32) for i in range(2)]
            xb = [k.sb("xb%d" % i, [128, D], BF16) for i in range(2)]
            junk = k.sb("junk", [128, D], BF16)
            ss = [k.sb("ss%d" % i, [128, 1], F32) for i in range(2)]
            rs = [k.sb("rs%d" % i, [128, 1], F32) for i in range(2)]
            _g0 = k.s.gcount
            LAMr = k.sb("LAMr", [64, 10, 32], F32); LAMi = k.sb("LAMi", [64, 10, 32], F32)
            OutW = k.sb("OutW", [128, 8, 8, 4, 32], BF16)
            Cm = k.sb("Cm", [128, 8, 4, 32], BF16)
            KW = k.sb("KW", [128, 8, 8, 32], BF16)
            SWc = k.sb("SWc", [128, 8, 8, 128], BF16)
            def T(name, shape, dt=F32):
                return k.sb(name, shape, dt)

            a_re = T("a_re", [64, 32]); a_im = T("a_im", [64, 32]); ldt = T("ldt", [64, 32])
            bre = T("bre", [64, 32, 16]); bim = T("bim", [64, 32, 16])
            cre = T("cre", [64, 32, 16]); cim = T("cim", [64, 32, 16])
            for t_, d_ in ((a_re, s_are), (a_im, s_aim), (ldt, s_ldt), (bre, s_bre), (bim, s_bim), (cre, s_cre), (cim, s_cim)):
                k.dma(t_[:], d_)
            dpad = T("dpad", [128, 8]); k.dma(dpad[:], s_d)
            lr = T("lr", [64, 32]); li = T("li", [64, 32])
            k.act(ldt[:], ldt[:], AF.Exp)
            k.tt(lr[:], a_re[:], ldt[:], ALU.mult)
            k.tt(li[:], a_im[:], ldt[:], ALU.mult)
            pass
            mag = T("mag", [64, 32]); yv = T("yv", [64, 32]); yi = T("yi", [64, 32], I32); yf = T("yf", [64, 32])
            fr = T("fr", [64, 32]); fc = T("fc", [64, 32]); msk = T("msk", [64, 32]); sn = T("sn", [64, 32]); cs = T("cs", [64, 32])
            for slot, tau in enumerate(PW_SLOTS):
                k.act(mag[:], lr[:], AF.Exp, scale=float(tau))
                k.ts(yv[:], li[:], float(tau) / (2 * np.pi), None, ALU.mult)
                k.cp(yi[:], yv[:])
                k.cp(yf[:], yi[:])
                k.tt(fr[:], yv[:], yf[:], ALU.subtract)
                k.ts(fc[:], fr[:], 0.25, None, ALU.add)
                k.ts(msk[:], fc[:], 0.5, None, ALU.is_gt)
                k.tt(fc[:], fc[:], msk[:], ALU.subtract)
                k.act(sn[:], fr[:], AF.Sin, scale=TWO_PI)
                k.act(cs[:], fc[:], AF.Sin, scale=TWO_PI)
                k.tt(LAMr[:, slot, :], mag[:], cs[:], ALU.mult)
                k.tt(LAMi[:, slot, :], mag[:], sn[:], ALU.mult)
            nre = T("nre", [64, 32]); den = T("den", [64, 32]); t0_ = T("t0_", [64, 32]); t1_ = T("t1_", [64, 32])
            fre = T("fre", [64, 32]); fim = T("fim", [64, 32])
            k.ts(nre[:], LAMr[:, 1, :], -1.0, None, ALU.add)
            k.tt(den[:], a_re[:], a_re[:], ALU.mult)
            k.tt(t0_[:], a_im[:], a_im[:], ALU.mult)
            k.tt(den[:], den[:], t0_[:], ALU.add)
            k.recip(den[:], den[:])
            k.tt(t0_[:], nre[:], a_re[:], ALU.mult)
            k.tt(t1_[:], LAMi[:, 1, :], a_im[:], ALU.mult)
            k.tt(t0_[:], t0_[:], t1_[:], ALU.add)
            k.tt(fre[:], t0_[:], den[:], ALU.mult)
            k.tt(t0_[:], LAMi[:, 1, :], a_re[:], ALU.mult)
            k.tt(t1_[:], nre[:], a_im[:], ALU.mult)
            k.tt(t0_[:], t0_[:], t1_[:], ALU.subtract)
            k.tt(fim[:], t0_[:], den[:], ALU.mult)
            bbr = T("bbr", [64, 32, 16]); bbi = T("bbi", [64, 32, 16])
            u1 = T("u1", [64, 32, 16]); u2 = T("u2", [64, 32, 16])

            def bc(ap2):
                return ap2.unsqueeze(2).to_broadcast([64, 32, 16])

            k.tt(u1[:], bre[:], bc(fre[:]), ALU.mult)
            k.tt(u2[:], bim[:], bc(fim[:]), ALU.mult)
            k.tt(bbr[:], u1[:], u2[:], ALU.subtract)
            k.tt(u1[:], bim[:], bc(fre[:]), ALU.mult)
            k.tt(u2[:], bre[:], bc(fim[:]), ALU.mult)
            k.tt(bbi[:], u1[:], u2[:], ALU.add)
            ZB = T("ZB", [128, 8, 8, 4, 32], BF16)
            pass
            pass
            k.memset(ZB[:], 0.0)
            k.memset(OutW[:], 0.0)
            k.memset(Cm[:], 0.0)

            def v4(ap3):
                return ap3.rearrange("p (c q) x -> p c q x", q=4)

            for tau in range(8):
                lrb, lib = bc(LAMr[:, tau, :]), bc(LAMi[:, tau, :])
                k.tt(u1[:], bbr[:], lrb, ALU.mult)
                k.tt(u2[:], bbi[:], lib, ALU.mult)
                k.tt(ZB[0:64, :, tau, :, 0:16], v4(u1[:]), v4(u2[:]), ALU.subtract, outs=[("ZB", tau)])
                k.tt(u1[:], bbi[:], lrb, ALU.mult)
                k.tt(u2[:], bbr[:], lib, ALU.mult)
                k.tt(ZB[64:128, :, tau, :, 0:16], v4(u1[:]), v4(u2[:]), ALU.add, outs=[("ZB", tau)])
            for j in range(8):
                lrb, lib = bc(LAMr[:, j + 1, :]), bc(LAMi[:, j + 1, :])
                k.tt(u1[:], cre[:], lrb, ALU.mult)
                k.tt(u2[:], cim[:], lib, ALU.mult)
                k.tt(OutW[0:64, :, j, :, 0:16], v4(u1[:]), v4(u2[:]), ALU.subtract, outs=[("OutW", j)])
                k.tt(u1[:], cre[:], lib, ALU.mult)
                k.tt(u2[:], cim[:], lrb, ALU.mult)
                k.tt(u1[:], u1[:], u2[:], ALU.add)
                k.ts(OutW[64:128, :, j, :, 0:16], v4(u1[:]), -1.0, None, ALU.mult, outs=[("OutW", j)])
            k.cp(Cm[0:64, :, :, 0:16], v4(cre[:]))
            k.ts(Cm[64:128, :, :, 0:16], v4(cim[:]), -1.0, None, ALU.mult)
            KWf = T("KWf", [128, 8, 8, 32], F32)
            pass
            for chunk in range(8):
                for tau in range(8):
                    slot = chunk * 8 + tau
                    bank = psb[2 + slot // 16]
                    c0 = (slot % 16) * 32
                    for gq in range(4):
                        k.mm(bank[32 * gq:32 * gq + 32, c0:c0 + 32], ZB[:, chunk, tau, gq, :], Cm[:, chunk, gq, :],
                             tile_position=(0, 32 * gq))
            for b4 in range(4):
                k.cp(KWf[:, 2 * b4:2 * b4 + 2, :, :], psb[2 + b4][:].rearrange("p (a t x) -> p a t x", a=2, t=8), eng="dve")
            for chunk in range(8):
                k.stt(KWf[:, chunk, 0, :], diag32[:], dpad[:, chunk:chunk + 1], KWf[:, chunk, 0, :], ALU.mult, ALU.add)
            k.cp(KW[:], KWf[:])
            pass
            for chunk in range(8):
                bankb = psb[4 + chunk % 4][:].bitcast(BF16)
                for i in range(8):
                    k.tr(bankb[:, i * 128:(i + 1) * 128], ZB[:, chunk, 7 - i, :, :].rearrange("p q x -> p (q x)"), ident[:])
                k.cp(SWc[:, chunk, :, :], bankb.rearrange("p (i x) -> p i x", i=8), eng=("dve" if chunk % 2 else "act") if False else "dve")

            for t_, d_ in ((LAMr, sc_LAMr), (LAMi, sc_LAMi), (OutW, sc_OutW), (Cm, sc_Cm), (KW, sc_KW), (SWc, sc_SWc)):
                k.s.add("pool", (lambda o_, i_: (lambda h: h.dma_start(out=o_, in_=i_)))(d_, t_[:]), [t_], [d_], dma=True, semkey="spill_st")
            _g1 = k.s.gcount
            for ti, (t0, n) in enumerate(tiles):
                b = ti % 2
                k.dma(xt[b][0:n, :], x[t0:t0 + n, :])
                k.act(junk[0:n, :], xt[b][0:n, :], AF.Square, accum_out=ss[b][0:n, :])
                k.ts(rs[b][0:n, :], ss[b][0:n, :], 1.0 / D, 1e-6, ALU.mult, ALU.add)
                k.act(rs[b][0:n, :], rs[b][0:n, :], AF.Sqrt)
                k.recip(rs[b][0:n, :], rs[b][0:n, :])
                k.act(xb[b][0:n, :], xt[b][0:n, :], AF.Copy, scale=rs[b][0:n, :])
                pt = psb[ti % 2]
                ptb = pt[:].bitcast(BF16)
                for c in range(8):
                    k.tr(ptb[:, c * 128:c * 128 + n], xb[b][0:n, c * 128:(c + 1) * 128], ident[0:n, 0:n])
                for c in range(8):
                    if c % 2 == 0:
                        k.act(xnT[:, c, t0:t0 + n], ptb[:, c * 128:c * 128 + n], AF.Copy, scale=gat[:, c:c + 1],
                              outs=[("xnT", ti)])
                    else:
                        k.ts(xnT[:, c, t0:t0 + n], ptb[:, c * 128:c * 128 + n], gat[:, c:c + 1], None, ALU.mult,
                             outs=[("xnT", ti)])

            k.s.interleave(_g0, _g1, k.s.gcount)
        with k.scope(full=True):
            attnT = k.sb("attnT", [128, 4, NT], BF16)
            with k.scope():
                wst = [k.sb("wst%d" % i, [128, 8, 256], F32) for i in range(3)]
                wbf = [k.sb("wbf0", [128, 8, 768], BF16)]
                qkf = [k.sb("qkf%d" % i, [128, 512], F32) for i in range(2)]
                vf = [k.sb("vf%d" % i, [128, 256], F32) for i in range(2)]
                qb = [k.sb("qb%d" % i, [128, 256], BF16) for i in range(2)]
                kb = [k.sb("kb%d" % i, [128, 256], BF16) for i in range(2)]
                rp = [k.sb("rp%d" % i, [128, 16], F32) for i in range(2)]
                tmp = [k.sb("rtmp%d" % i, [128, 8, 8], F32) for i in range(4)]
                mstage = k.sb("mstage", [128, 384], F32)
                maskb = k.sb("maskb", [128, 256], BF16)
                msamp = k.sb("msamp", [128, 2, 64], BF16)
                k.dma(mstage[:], masks_d)
                k.cp(maskb[:], mstage[:, 0:256])
                k.cp(msamp[:], mstage[:, 256:384].rearrange("p (a x) -> p a x", a=2))
                ones_f = k.sb("ones_f", [128, 64], F32)
                k.memset(ones_f[:], 1.0)
                QTz = k.sb("QTz", [128, 4, NT], BF16)
                KT = k.sb("KT", [128, 2, NT], BF16)
                Va = k.sb("Va", [128, 17, 4, 65], BF16)
                acc = k.sb("acc", [65, 4, NT], F32)
                PT = [k.sb("PT%d" % i, [128, 256], BF16) for i in range(4)]
                PTs4 = [k.sb("PTs%d" % i, [128, 64], BF16) for i in range(4)]
                cst = [k.sb("cst%d" % i, [128, 4, 2, 256], F32) for i in range(2)]
                kcb = [k.sb("kcb%d" % i, [128, 256], BF16) for i in range(4)]
                KcT = [k.sb("KcT%d" % i, [128, 2, 128], BF16) for i in range(4)]
                Vc = [k.sb("Vc%d" % i, [128, 4, 65], BF16) for i in range(4)]
                PTc = [k.sb("PTc%d" % i, [128, 4, 4], BF16) for i in range(4)]
                k.memset(QTz[:], 0.0, eng="pool")
                k.memset(Va[:], 1.0, eng="pool")
                for i in range(4):
                    k.memset(PTs4[i][:], 0.0, eng="pool")
                for i in range(4):
                    k.memset(Vc[i][:], 1.0, eng="pool")

                def pipeline(iters, skews):
                    n_ = len(iters)
                    for step in range(n_ + max(skews)):
                        for si, sk in enumerate(skews):
                            i_ = step - sk
                            if 0 <= i_ < n_:
                                iters[i_][si]()

                cnt = {"it": 0, "ai": 0, "ci": 0}
                for hh in range(2):
                    k.memset(acc[:], 0.0, eng="pool")
                    for g, (win, dil) in enumerate(GROUPS):
                        wb = wbf[0]
                        for j in range(3):
                            c0 = j * 1536 + g * 512 + hh * 256
                            k.dma(wst[j][:], w_in_v[:, :, c0:c0 + 256])
                            if j == 1:
                                k.act(wb[:, :, j * 256:(j + 1) * 256], wst[j][:], AF.Copy, outs=[(wb.name, j)])
                            else:
                                k.cp(wb[:, :, j * 256:(j + 1) * 256], wst[j][:], outs=[(wb.name, j)])
                        nblk = (NPT // dil) // 128
                        blocks = []
                        for r in range(dil):
                            for i in range(nblk):
                                blocks.append((r + dil * 128 * i, dil, 128, r, i))
                        blocks.append((NPT, 1, NS, None, None))

                        def mk_block(bi, tstart, tstep, n, r, i, hh=hh, g=g, win=win, dil=dil, wb=wb):
                            b = cnt["it"] % 2
                            cnt["it"] += 1
                            pq, pv = psb[2 + 2 * b], psb[3 + 2 * b]
                            tok = slice(tstart, tstart + tstep * (n - 1) + 1, tstep)
                            ptb = psb[6 + b][:].bitcast(BF16)
                            pc = slice(bi * 128, bi * 128 + n)

                            def s1():
                                for c in range(8):
                                    k.mm(pq[0:n, :], xnT[:, c, tok], wb[:, c, 0:512], start=(c == 0), stop=(c == 7),
                                         ins=["xnT", wb])
                                for c in range(8):
                                    k.mm(pv[0:n, 0:256], xnT[:, c, tok], wb[:, c, 512:768], start=(c == 0), stop=(c == 7),
                                         ins=["xnT", wb])
                                k.dma(rp[b][0:n, :], rope[tok, :])

                            def s2():
                                k.act(qkf[b][0:n, :], pq[0:n, :], AF.Copy)
                                k.act(vf[b][0:n, :], pv[0:n, 0:256], AF.Copy)
                                q3 = qkf[b][0:n, :].rearrange("p (h d) -> p h d", d=64)
                                x1, x2 = q3[:, :, 0:8], q3[:, :, 8:16]
                                cosb = rp[b][0:n, 0:8].unsqueeze(1).to_broadcast([n, 8, 8])
                                sinb = rp[b][0:n, 8:16].unsqueeze(1).to_broadcast([n, 8, 8])
                                t1, t2, t3, t4 = (t[0:n] for t in tmp)
                                k.tt(t1, x1, cosb, ALU.mult)
                                k.tt(t2, x2, sinb, ALU.mult)
                                k.tt(t3, x2, cosb, ALU.mult)
                                k.tt(t4, x1, sinb, ALU.mult)
                                k.tt(x1, t1, t2, ALU.subtract, outs=[qkf[b]])
                                k.tt(x2, t3, t4, ALU.add, outs=[qkf[b]])
                                kpart = qkf[b][0:n, 256:512].rearrange("p (h d) -> p h d", d=64)
                                vpart = vf[b][0:n, :].rearrange("p (h d) -> p h d", d=64)
                                hs = slice(hh * 4, hh * 4 + 4)
                                if r is None:
                                    dk = kvs[g][:, :, 0, hs, :].rearrange("s t h d -> (s t) h d")
                                    dv = kvs[g][:, :, 1, hs, :].rearrange("s t h d -> (s t) h d")
                                    k.dma(dk, kpart, q="pool")
                                    k.dma(dv, vpart, q="pool")
                                else:
                                    first = NPT - min(win, NPT)
                                    if tstart >= first:
                                        rows = slice(tstart - first, tstart - first + dil * 127 + 1, dil)
                                        k.dma(kvp[g][rows, 0, hs, :], kpart, q="pool")
                                        k.dma(kvp[g][rows, 1, hs, :], vpart, q="pool")
                                k.ts(qb[b][0:n, :], qkf[b][0:n, 0:256], 0.125, None, ALU.mult)
                                k.cp(kb[b][0:n, :], qkf[b][0:n, 256:512])
                                k.act(Va[0:n, bi, :, 0:64], vpart, AF.Copy, outs=[("Va", bi)])
                                for pr in range(2):
                                    k.tr(ptb[:, pr * 128:pr * 128 + n], qb[b][0:n, pr * 128:(pr + 1) * 128], ident[0:n, 0:n])
                                    k.tr(ptb[:, 256 + pr * 128:256 + pr * 128 + n], kb[b][0:n, pr * 128:(pr + 1) * 128],
                                         ident[0:n, 0:n])

                            def s3():
                                for pr in range(2):
                                    k.act(QTz[0:64, 2 * pr, pc], ptb[0:64, pr * 128:pr * 128 + n], AF.Copy, outs=[("QTz", bi)])
                                    k.cp(QTz[64:128, 2 * pr + 1, pc], ptb[64:128, pr * 128:pr * 128 + n], outs=[("QTz", bi)])
                                    if pr == 0:
                                        k.act(KT[:, pr, pc], ptb[:, 256 + pr * 128:256 + pr * 128 + n], AF.Copy, outs=[("KT", bi)])
                                    else:
                                        k.cp(KT[:, pr, pc], ptb[:, 256 + pr * 128:256 + pr * 128 + n], outs=[("KT", bi)])
                            return (s1, s2, s3)

                        pipeline([mk_block(bi, *blk) for bi, blk in enumerate(blocks)], (0, 1, 2))

                        def mk_att(h, r, i, g=g, dil=dil, nblk=nblk):
                            a = cnt["ai"] % 4
                            cnt["ai"] += 1
                            ps_s, ps_o = psb[a], psb[4 + a]
                            if r is None:
                                PTs = PTs4[a]
                                def a1():
                                    k.mm(ps_s[0:64, 0:64], KT[:, h // 2, NPT:NT], QTz[:, h, NPT:NT], start=True, stop=False,
                                         ins=["KT", "QTz"])
                                    k.mm(ps_s[0:64, 0:64], ident[:, 0:64], msamp[:, 0 if g == 0 else 1, :], start=False, stop=True)
                                    k.act(PTs[0:64, :], ps_s[0:64, 0:64], AF.Exp)

                                def a2():
                                    k.mm(ps_o[0:65, 0:64], Va[:, 16, h, :], PTs[:, :], ins=["Va", PTs])
                                    av = acc[0:65, h, NPT:NT]
                                    k.tt(av, av, ps_o[0:65, 0:64], ALU.add, ins=[acc, ps_o], outs=[acc])
                                return (a1, a2)
                            bi = r * nblk + i
                            nq = 2 if i + 1 < nblk else 1
                            kc = slice(bi * 128, bi * 128 + 128)
                            qc = slice(bi * 128, bi * 128 + 128 * nq)
                            N = 128 * nq

                            def a1():
                                k.mm(ps_s[:, 0:N], KT[:, h // 2, kc], QTz[:, h, qc], start=True, stop=False,
                                     ins=["KT", "QTz"])
                                k.mm(ps_s[:, 0:N], ident[:], maskb[:, 0:N], start=False, stop=True)
                                k.act(PT[a][:, 0:N], ps_s[:, 0:N], AF.Exp)

                            def a2():
                                k.mm(ps_o[0:65, 0:N], Va[:, bi, h, :], PT[a][:, 0:N], ins=["Va", PT[a]])
                                t0n = r + dil * 128 * i
                                av = acc[0:65, h, t0n:t0n + dil * (N - 1) + 1:dil]
                                k.tt(av, av, ps_o[0:65, 0:N], ALU.add, ins=[acc, ps_o], outs=[acc])
                            return (a1, a2)

                        its = [mk_att(h, r, i) for h in range(4) for r in range(dil) for i in range(nblk)]
                        its += [mk_att(h, None, None) for h in range(4)]
                        pipeline(its, (0, 2))

                        def mk_cache(s_, t_, nt_, nq, cb, g=g, hh=hh):
                            e = cnt["ci"] % 4
                            cnt["ci"] += 1
                            a = cnt["ai"] % 2
                            cnt["ai"] += 1
                            ps_s, ps_o = psb[a], psb[2 + a]
                            ptb = psb[6 + e % 2][:].bitcast(BF16)
                            q0 = NPT + 4 * s_ + (t_ if g > 0 else 0)

                            def c0():
                                if t_ == 0:
                                    if g == 0:
                                        k.dma(cst[cb][:, 0, :, :], caches[0][s_, :, :, hh * 4:hh * 4 + 4, :].rearrange("m a h d -> m a (h d)"))
                                    else:
                                        k.dma(cst[cb][:], caches[g][s_, :, :, :, hh * 4:hh * 4 + 4, :].rearrange("m t a h d -> m t a (h d)"))
                                k.cp(kcb[e][:], cst[cb][:, t_, 0, :])
                                k.act(Vc[e][:, :, 0:64], cst[cb][:, t_, 1, :].rearrange("p (h d) -> p h d", d=64), AF.Copy)
                                for pr in range(2):
                                    k.tr(ptb[:, pr * 128:(pr + 1) * 128], kcb[e][:, pr * 128:(pr + 1) * 128], ident[:])

                            def c1():
                                k.act(KcT[e][:], ptb[:, 0:256].rearrange("p (a x) -> p a x", a=2), AF.Copy)
                                for h in range(4):
                                    k.mm(ps_s[:, h * 4:h * 4 + nq], KcT[e][:, h // 2, :], QTz[:, h, q0:q0 + nq],
                                         start=True, stop=(g > 0), ins=[KcT[e], "QTz"])
                                    if g == 0:
                                        k.mm(ps_s[:, h * 4:h * 4 + nq], ident[:], maskb[:, 128:128 + nq], start=False, stop=True)
                                k.act(PTc[e][:, :, 0:nq], ps_s[:, 0:16].rearrange("p (h q) -> p h q", q=4)[:, :, 0:nq], AF.Exp)

                            def c2():
                                for h in range(4):
                                    k.mm(ps_o[0:65, h * 4:h * 4 + nq], Vc[e][:, h, :], PTc[e][:, h, 0:nq])
                                av = acc[0:65, :, q0:q0 + nq]
                                k.tt(av, av, ps_o[0:65, 0:16].rearrange("p (h q) -> p h q", q=4)[:, :, 0:nq], ALU.add,
                                     ins=[acc, ps_o], outs=[acc])
                            return (c0, c1, c2)

                        its = []
                        for s_ in range(16):
                            nt_, nq = (1, 4) if g == 0 else (4, 1)
                            for t_ in range(nt_):
                                its.append(mk_cache(s_, t_, nt_, nq, s_ % 2))
                        pipeline(its, (0, 1, 2))

                    for h in range(4):
                        k.recip(acc[64:65, h, :], acc[64:65, h, :])
                        for (t0, n) in [(i * 512, 512) for i in range(4)] + [(NPT, NS)]:
                            a = cnt["ai"] % 2
                            cnt["ai"] += 1
                            pbc = psb[a]
                            k.mm(pbc[0:64, 0:n], ones_f[64:65, 0:64], acc[64:65, h, t0:t0 + n], tile_position=(64, 0))
                            dst = attnT[(h % 2) * 64:(h % 2) * 64 + 64, hh * 2 + h // 2, t0:t0 + n]
                            k.tt(dst, acc[0:64, h, t0:t0 + n], pbc[0:64, 0:n], ALU.mult, outs=[attnT])
                if dbg:
                    k.dma(dbg_at, attnT[:], q="pool")
            uT = k.sb("uT", [128, 8, NT], BF16)
            cvt.extend([k.sb("cvt%d" % i, [128, 6144], BF16) for i in range(2)])
            k.s.bg_keys = set((c_.name, None) for c_ in cvt)

            with k.scope():
                LAMr = k.sb("LAMr_s", [64, 10, 32], F32); LAMi = k.sb("LAMi_s", [64, 10, 32], F32)
                OutW = k.sb("OutW_s", [128, 8, 8, 4, 32], BF16)
                Cm = k.sb("Cm_s", [128, 8, 4, 32], BF16)
                KW = k.sb("KW_s", [128, 8, 8, 32], BF16)
                SWc = k.sb("SWc_s", [128, 8, 8, 128], BF16)
                _lds = []
                for t_, d_ in ((LAMr, sc_LAMr), (LAMi, sc_LAMi), (OutW, sc_OutW), (Cm, sc_Cm), (KW, sc_KW), (SWc, sc_SWc)):
                    _lds.append(k.s.add("sp", (lambda o_, i_: (lambda h: h.dma_start(out=o_, in_=i_)))(t_[:], d_), [d_], [t_], dma=True, semkey="spill_ld"))
                for o_ in _lds:
                    o_.val = _lds[-1].val

                def T(name, shape, dt=F32):
                    return k.sb(name, shape, dt)

                with k.scope():
                    Wu = T("Wu", [128, 8, 8, 4, 32], BF16)
                    wst = [k.sb("wsu%d" % i, [128, 8, 256], F32) for i in range(2)]
                    k.memset(Wu[:], 0.0)
                    for h in range(2):
                        k.dma(wst[h][:], w_in_v[:, :, 4608 + 256 * h:4608 + 256 * h + 256])
                        for c in range(8):
                            k.cp(Wu[:, c, 4 * h:4 * h + 4, :, 0:16], wst[h][:, c, :].rearrange("p (a q x) -> p a q x", a=4, q=4),
                                 eng="dve")
                    pass
                    ranges = [(i * 512, 512) for i in range(4)] + [(NPT, NS)]
                    it = 0
                    for chunk in range(8):
                        for (t0, n) in ranges:
                            pb = psb[it % 4]
                            it += 1
                            for c in range(8):
                                k.mm(pb[:, 0:n], Wu[:, c, chunk, :, :].rearrange("p q x -> p (q x)"), xnT[:, c, t0:t0 + n],
                                     start=(c == 0), stop=(c == 7), ins=[Wu, "xnT"])
                            if it % 2:
                                k.act(uT[:, chunk, t0:t0 + n], pb[:, 0:n], AF.Copy, outs=[("uT", chunk)])
                            else:
                                k.cp(uT[:, chunk, t0:t0 + n], pb[:, 0:n], outs=[("uT", chunk)])
                HP = T("HP", [128, 32, 256], BF16)
                k.memset(HP[:, :, 0:1], 0.0)
                with k.scope():
                    SS = [T("SS0", [64, 64, 2, 32])]
                    Hr = T("Hr", [64, 64, 3, 32])
                    Z3 = T("Z3", [64, 3, 32]); k.memset(Z3[:], 0.0)
                    AR2 = T("AR2", [64, 2, 32]); AI2 = T("AI2", [64, 2, 32])
                    k.cp(AR2[:, 0, :], LAMr[:, 8, :]); k.cp(AR2[:, 1, :], LAMr[:, 8, :])
                    k.ts(AI2[:, 0, :], LAMi[:, 8, :], -1.0, None, ALU.mult); k.cp(AI2[:, 1, :], LAMi[:, 8, :])
                    sA = T("sA", [64, 2, 32]); sB = T("sB", [64, 2, 32])
                    fill = 0
                    for r in range(4):
                        S_ = SS[0]
                        for gq in range(4):
                            for ch in range(2):
                                bank = psb[fill % 8]
                                fill += 1
                                for cc in range(4):
                                    chunk = ch * 4 + cc
                                    for ri in range(2):
                                        c0 = (cc * 2 + ri) * 64
                                        for i in range(8):
                                            k.mm(bank[0:64, c0:c0 + 64],
                                                 SWc[32 * gq:32 * gq + 32, chunk, i, ri * 64:(ri + 1) * 64],
                                                 uT[32 * gq:32 * gq + 32, chunk, 512 * r + i:512 * r + 512:8],
                                                 start=(i == 0), stop=(i == 7), tile_position=(32 * gq, 0),
                                                 ins=[SWc, ("uT", chunk)])
                                g0 = gq + 16 * ch
                                src = bank[0:64, :].rearrange("p (c r k) -> p k r c", c=4, r=2)
                                if fill % 2:
                                    k.act(S_[:, :, :, g0:g0 + 13:4], src, AF.Copy, outs=[(S_.name, fill % 8)])
                                else:
                                    k.cp(S_[:, :, :, g0:g0 + 13:4], src, outs=[(S_.name, fill % 8)])
                        for kk in range(64):
                            prev = Z3[:] if (r == 0 and kk == 0) else (Hr[:, 63, :, :] if kk == 0 else Hr[:, kk - 1, :, :])
                            k.tt(sA[:], prev[:, 0:2, :], AR2[:], ALU.mult)
                            k.tt(sB[:], prev[:, 1:3, :], AI2[:], ALU.mult)
                            k.tt(sA[:], sA[:], sB[:], ALU.add)
                            k.tt(Hr[:, kk, 0:2, :], sA[:], S_[:, kk, :, :], ALU.add, ins=[sA, S_])
                            k.cp(Hr[:, kk, 2, :], Hr[:, kk, 0, :])
                            if kk % 32 == 31:
                                conv_step(Hr)
                        ncol = 64 if r < 3 else 63
                        k.cp(HP[0:64, :, 64 * r + 1:64 * r + 1 + ncol], Hr[:, 0:ncol, 0, :].rearrange("p k g -> p g k"))
                        k.cp(HP[64:128, :, 64 * r + 1:64 * r + 1 + ncol], Hr[:, 0:ncol, 1, :].rearrange("p k g -> p g k"))
                    k.dma(ssm_p, Hr[:, 63, 0:2, :], q="pool")
                HPs = T("HPs", [128, 32, 16], BF16)
                with k.scope():
                    SSs = T("SSs", [64, 2, 16, 32])
                    for gq in range(4):
                        for ri in range(2):
                            bank = psb[gq * 2 + ri]
                            for chunk in range(8):
                                for i in range(4):
                                    k.mm(bank[0:64, chunk * 16:(chunk + 1) * 16],
                                         SWc[32 * gq:32 * gq + 32, chunk, i, ri * 64:(ri + 1) * 64],
                                         uT[32 * gq:32 * gq + 32, chunk, NPT + i:NT:4],
                                         start=(i == 0), stop=(i == 3), tile_position=(32 * gq, 0),
                                         ins=[SWc, ("uT", chunk)])
                            k.cp(SSs[:, ri, :, gq:32:4], bank[0:64, 0:128].rearrange("p (c s) -> p s c", c=8))
                    h0P = T("h0P", [64, 2, 16, 32])
                    k.dma(h0P[:], s_h0)

                    def bs(ap2):
                        return ap2.unsqueeze(1).to_broadcast([64, 16, 32])

                    w1 = T("w1", [64, 16, 32]); w2 = T("w2", [64, 16, 32]); Wr_ = T("Wr_", [64, 16, 32]); Wi_ = T("Wi_", [64, 16, 32])
                    nsP = T("nsP", [64, 2, 16, 32])
                    ar8, ai8, l4r, l4i = bs(LAMr[:, 8, :]), bs(LAMi[:, 8, :]), bs(LAMr[:, 9, :]), bs(LAMi[:, 9, :])
                    k.tt(w1[:], h0P[:, 0], ar8, ALU.mult); k.tt(w2[:], h0P[:, 1], ai8, ALU.mult)
                    k.tt(w1[:], w1[:], w2[:], ALU.subtract); k.tt(Wr_[:], w1[:], SSs[:, 0], ALU.add)
                    k.tt(w1[:], h0P[:, 1], ar8, ALU.mult); k.tt(w2[:], h0P[:, 0], ai8, ALU.mult)
                    k.tt(w1[:], w1[:], w2[:], ALU.add); k.tt(Wi_[:], w1[:], SSs[:, 1], ALU.add)
                    k.tt(w1[:], Wr_[:], l4r, ALU.mult); k.tt(w2[:], Wi_[:], l4i, ALU.mult)
                    k.tt(nsP[:, 0], w1[:], w2[:], ALU.subtract)
                    k.tt(w1[:], Wi_[:], l4r, ALU.mult); k.tt(w2[:], Wr_[:], l4i, ALU.mult)
                    k.tt(nsP[:, 1], w1[:], w2[:], ALU.add)
                    k.dma(ssm_s, nsP[:], q="pool")
                    k.cp(HPs[0:64], h0P[:, 0].rearrange("p s g -> p g s"))
                    k.cp(HPs[64:128], h0P[:, 1].rearrange("p s g -> p g s"))
                KWbd = T("KWbd", [128, 8, 8, 128], BF16)
                k.memset(KWbd[:], 0.0)
                for gq in range(4):
                    k.cp(KWbd[32 * gq:32 * gq + 32, :, :, 32 * gq:32 * gq + 32], KW[32 * gq:32 * gq + 32, :, :, :])
                for chunk in range(8):
                    for j in range(8):
                        bank = psb[j]
                        for i in range(j + 1):
                            k.mm(bank[:, 0:256], KWbd[:, chunk, j - i, :], uT[:, chunk, i:NPT:8], start=(i == 0), stop=False,
                                 ins=[KWbd, ("uT", chunk)])
                            if j < 4:
                                k.mm(bank[:, 256:272], KWbd[:, chunk, j - i, :], uT[:, chunk, NPT + i:NT:4], start=False, stop=False,
                                     ins=[KWbd, ("uT", chunk)])
                    for gq in range(4):
                        g = chunk * 4 + gq
                        for j in range(8):
                            bank = psb[j]
                            k.mm(bank[32 * gq:32 * gq + 32, 0:256], OutW[:, chunk, j, gq, :], HP[:, g, :],
                                 start=False, stop=True, tile_position=(0, 32 * gq))
                            if j < 4:
                                k.mm(bank[32 * gq:32 * gq + 32, 256:272], OutW[:, chunk, j, gq, :], HPs[:, g, :],
                                     start=False, stop=True, tile_position=(0, 32 * gq))
                    for j in range(8):
                        k.act(uT[:, chunk, j:NPT:8], psb[j][:, 0:256], AF.Gelu_apprx_tanh, outs=[("uT", chunk)])
                        if j < 4:
                            k.act(uT[:, chunk, NPT + j:NT:4], psb[j][:, 256:272], AF.Gelu_apprx_tanh, outs=[("uT", chunk)])
                    conv_step(("uT", chunk))
                if dbg:
                    k.dma(dbg_gy, uT[:], q="pool")


            mergedT = k.sb("mergedT", [128, 8, NT], BF16)
            ranges = [(i * 512, 512) for i in range(4)] + [(NPT, NS)]
            wab_v = w_ab.rearrange("(c p) n -> p c n", p=128)
            wglu_v = w_glu.rearrange("(ch q c) n -> q c ch n", q=4, c=16)
            ring = [0]

            def nbank():
                ring[0] += 1
                return psb[ring[0] % 8]

            with k.scope():
                sAB = k.sb("sAB", [128, 4, 128], F32)
                sG = k.sb("sG", [128, 8, 2, 128], F32)
                sL = k.sb("sL", [128, 8, 2, 128], F32)
                k.memset(sL[:], 0.0)
                mw = [(k.sb("mAB%d" % i, [128, 4, 128], BF16), k.sb("mG%d" % i, [128, 8, 2, 128], BF16),
                       k.sb("mL%d" % i, [128, 8, 2, 128], BF16)) for i in range(2)]
                mt = [[k.sb("mt%d_%d" % (i, j), [128, 512], F32) for j in range(5)] for i in range(2)]
                mi = 0
                for fc in range(8):
                    AB, G_, L_ = mw[fc % 2]
                    k.dma(sAB[:], wab_v[:, :, fc * 128:(fc + 1) * 128])
                    for j in range(2):
                        k.dma(sG[:, :, j, :], w_in_v[:, :, 5120 + j * 1024 + fc * 128:5120 + j * 1024 + (fc + 1) * 128], outs=[("sG", j)])
                        for gq in range(4):
                            k.dma(sL[32 * gq:32 * gq + 16, :, j, :], wglu_v[gq, :, :, j * 1024 + fc * 128:j * 1024 + (fc + 1) * 128],
                                  outs=[("sL", j * 4 + gq)])
                    k.cp(AB[:], sAB[:])
                    k.act(G_[:], sG[:], AF.Copy)
                    k.cp(L_[:], sL[:])
                    for (t0, n) in ranges:
                        tl = slice(t0, t0 + n)
                        s1, s2, s3, m1, m2 = mt[mi % 2]
                        mi += 1
                        p_ao, p_ga, p_gs, p_la, p_lb = nbank(), nbank(), nbank(), nbank(), nbank()
                        for c in range(4):
                            k.mm(p_ao[:, 0:n], AB[:, c, :], attnT[:, c, tl], start=(c == 0), stop=(c == 3))
                        for j, pb in ((0, p_ga), (1, p_gs)):
                            for c in range(8):
                                k.mm(pb[:, 0:n], G_[:, c, j, :], xnT[:, c, tl], start=(c == 0), stop=(c == 7), ins=[G_, "xnT"])
                        for j, pb in ((0, p_la), (1, p_lb)):
                            for c in range(8):
                                k.mm(pb[:, 0:n], L_[:, c, j, :], uT[:, c, tl], start=(c == 0), stop=(c == 7), ins=[L_, "uT"])
                        k.act(s1[:, 0:n], p_ga[:, 0:n], AF.Sigmoid)
                        k.act(s2[:, 0:n], p_gs[:, 0:n], AF.Sigmoid)
                        k.act(s3[:, 0:n], p_lb[:, 0:n], AF.Sigmoid)
                        k.tt(m1[:, 0:n], p_ao[:, 0:n], s1[:, 0:n], ALU.mult)
                        k.tt(m2[:, 0:n], p_la[:, 0:n], s3[:, 0:n], ALU.mult)
                        k.tt(m2[:, 0:n], m2[:, 0:n], s2[:, 0:n], ALU.mult)
                        k.tt(mergedT[:, fc, tl], m1[:, 0:n], m2[:, 0:n], ALU.add, outs=[("mergedT", fc)])
                        if mi % 2 == 0:
                            conv_step(("mergedT", fc))
            while cvs["e"] <= 32:
                conv_step()
            with k.scope():
                wos = [k.sb("wos%d" % i, [128, 8, 256], F32) for i in range(2)]
                Wo = k.sb("Wo", [128, 8, 1024], BF16)
                wout_v = w_out.rearrange("(c p) n -> p c n", p=128)
                for j in range(4):
                    k.dma(wos[j % 2][:], wout_v[:, :, j * 256:(j + 1) * 256])
                    k.act(Wo[:, :, j * 256:(j + 1) * 256], wos[j % 2][:], AF.Copy, outs=[("Wo", j)])
                gffrow = k.sb("gffrow", [128, D], F32)
                k.dma(gffrow[:], gffrow_d)
                hnb = [k.sb("hnb%d" % i, [128, D], BF16) for i in range(2)]
                gff = k.sb("gff", [128, 8], F32)
                k.dma(gff[:], g_ffn)
                xt = [k.sb("xu%d" % i, [128, D], F32) for i in range(2)]
                ht = [k.sb("ht%d" % i, [128, D], F32) for i in range(2)]
                hb = [k.sb("hb%d" % i, [128, D], BF16) for i in range(2)]
                junk = k.sb("junk2", [128, D], BF16)
                ss = [k.sb("su%d" % i, [128, 1], F32) for i in range(2)]
                rs = [k.sb("ru%d" % i, [128, 1], F32) for i in range(2)]
                def mk_wo(ti, t0, n):
                    b = ti % 2
                    tl = slice(t0, t0 + n)
                    pbs = (nbank(), nbank())
                    ptb = nbank()[:].bitcast(BF16)

                    def w1():
                        k.dma(xt[b][0:n, :], x[tl, :])
                        for sl_ in range(2):
                            for c in range(8):
                                k.mm(pbs[sl_][0:n, :], mergedT[:, c, tl], Wo[:, c, sl_ * 512:(sl_ + 1) * 512],
                                     start=(c == 0), stop=(c == 7), ins=["mergedT", "Wo"])

                    def w2():
                        for sl_ in range(2):
                            k.tt(ht[b][0:n, sl_ * 512:(sl_ + 1) * 512], pbs[sl_][0:n, :], xt[b][0:n, sl_ * 512:(sl_ + 1) * 512], ALU.add)
                        k.dma(h_scr[tl, :], ht[b][0:n, :], q="pool")
                        k.act(junk[0:n, :], ht[b][0:n, :], AF.Square, accum_out=ss[b][0:n, :])
                        k.ts(rs[b][0:n, :], ss[b][0:n, :], 1.0 / D, 1e-6, ALU.mult, ALU.add)
                        k.act(rs[b][0:n, :], rs[b][0:n, :], AF.Sqrt)
                        k.recip(rs[b][0:n, :], rs[b][0:n, :])
                        k.act(hb[b][0:n, :], ht[b][0:n, :], AF.Copy, scale=rs[b][0:n, :])
                        k.tt(hnb[b][0:n, :], hb[b][0:n, :], gffrow[0:n, :], ALU.mult)
                        k.dma(hn_scr[tl, :], hnb[b][0:n, :], q="pool")
                        for c in range(8):
                            k.tr(ptb[:, c * 128:c * 128 + n], hb[b][0:n, c * 128:(c + 1) * 128], ident[0:n, 0:n])

                    def w3():
                        for c in range(8):
                            if c % 2 == 0:
                                k.act(xnT[:, c, tl], ptb[:, c * 128:c * 128 + n], AF.Copy, scale=gff[:, c:c + 1], outs=[("xnT", ti)])
                            else:
                                k.ts(xnT[:, c, tl], ptb[:, c * 128:c * 128 + n], gff[:, c:c + 1], None, ALU.mult, outs=[("xnT", ti)])

                    return (w1, w2, w3)

                wos_ = [mk_wo(ti, t0, n) for ti, (t0, n) in enumerate(tiles)]
                for step in range(len(wos_) + 2):
                    for si_, sk_ in enumerate((0, 1, 2)):
                        if 0 <= step - sk_ < len(wos_):
                            wos_[step - sk_][si_]()
        hnT = xnT
        with k.scope():
            gfin = k.sb("gfin", [128, D], F32)
            k.dma(gfin[:], gfin_d)
            A1s = k.sb("A1s", [128, 17, 32], F32); A2s = k.sb("A2s", [128, 17, 32], F32)
            WW = k.sb("WW", [128, 17, 2], F32)
            k.memset(A1s[:], 0.0); k.memset(A2s[:], 0.0); k.memset(WW[:], 0.0)
            with k.scope():
                rst = k.sb("rst", [128, 8, 36], F32)
                Wr = k.sb("Wr", [128, 8, 36], BF16)
                k.dma(rst[:, :, 0:4], w_rg.rearrange("(c p) n -> p c n", p=128))
                k.dma(rst[:, :, 4:36], w_re.rearrange("(c p) n -> p c n", p=128))
                k.cp(Wr[:], rst[:])
                rbias = k.sb("rbias", [128, 36], F32)
                k.dma(rbias[:], rb_d)
                LG = k.sb("LG", [128, 17, 36], F32)
                k.memset(LG[:], 0.0)
                for ti, (t0, n) in enumerate(tiles):
                    pb = nbank()
                    for c in range(8):
                        k.mm(pb[0:n, 0:36], hnT[:, c, t0:t0 + n], Wr[:, c, :], start=(c == 0), stop=(c == 7), ins=["xnT", Wr])
                    k.tt(LG[0:n, ti, :], pb[0:n, 0:36], rbias[0:n, :], ALU.add, outs=[("LG", ti)])

                def R(name, shape):
                    return k.sb(name, shape, F32)

                def red(out, in_, op):
                    return k.s.add("dve", lambda h: h.tensor_reduce(out, in_, AX.X, op), [in_], [out])

                def b3(ap2, n3):
                    return ap2.unsqueeze(2).to_broadcast([128, 17, n3])
                lg4 = LG[:, :, 0:4]
                le4 = LG[:, :, 4:36].rearrange("p t (g e) -> p t g e", e=8)
                mx = R("r_mx", [128, 17]); ohb = R("r_oh", [128, 17, 4]); e4b = R("r_e4", [128, 17, 4])
                se = R("r_se", [128, 17]); pg = R("r_pg", [128, 17])
                red(mx[:], lg4, ALU.max)
                k.tt(ohb[:], lg4, b3(mx[:], 4), ALU.is_equal)
                k.tt(e4b[:], lg4, b3(mx[:], 4), ALU.subtract)
                k.act(e4b[:], e4b[:], AF.Exp)
                red(se[:], e4b[:], ALU.add)
                k.recip(pg[:], se[:])
                legb = R("r_leg", [128, 17, 8]); tmp8 = R("r_t8", [128, 17, 8])
                for g_ in range(4):
                    ohg = ohb[:, :, g_:g_ + 1].to_broadcast([128, 17, 8])
                    if g_ == 0:
                        k.tt(legb[:], le4[:, :, 0, :], ohg, ALU.mult)
                    else:
                        k.tt(tmp8[:], le4[:, :, g_, :], ohg, ALU.mult)
                        k.tt(legb[:], legb[:], tmp8[:], ALU.add)
                v1 = R("r_v1", [128, 17]); v2 = R("r_v2", [128, 17]); m1b = R("r_m1", [128, 17, 8]); m2b = R("r_m2", [128, 17, 8])
                red(v1[:], legb[:], ALU.max)
                k.tt(m1b[:], legb[:], b3(v1[:], 8), ALU.is_equal)
                k.stt(tmp8[:], m1b[:], -1.0e30, legb[:], ALU.mult, ALU.add)
                red(v2[:], tmp8[:], ALU.max)
                k.tt(m2b[:], tmp8[:], b3(v2[:], 8), ALU.is_equal)
                ex = R("r_ex", [128, 17]); w1_ = R("r_w1", [128, 17]); w2_ = R("r_w2", [128, 17])
                k.tt(ex[:], v2[:], v1[:], ALU.subtract)
                k.act(ex[:], ex[:], AF.Exp)
                k.ts(w1_[:], ex[:], 1.0, None, ALU.add)
                k.recip(w1_[:], w1_[:])
                k.tt(w2_[:], ex[:], w1_[:], ALU.mult)
                k.tt(WW[:, :, 0], w1_[:], pg[:], ALU.mult)
                k.tt(WW[:, :, 1], w2_[:], pg[:], ALU.mult)
                for g_ in range(4):
                    ohg = ohb[:, :, g_:g_ + 1].to_broadcast([128, 17, 8])
                    k.tt(A1s[:, :, g_ * 8:(g_ + 1) * 8], m1b[:], ohg, ALU.mult)
                    k.tt(A2s[:, :, g_ * 8:(g_ + 1) * 8], m2b[:], ohg, ALU.mult)
                k.memset(A1s[NS:128, 16, :], 0.0)
                k.memset(A2s[NS:128, 16, :], 0.0)
                k.memset(WW[NS:128, 16, :], 0.0)
            NB = 49
            SLi = [k.sb("SLi%d" % i, [128, 17], I32) for i in range(2)]
            BEi = k.sb("BEi", [128, NB], I32)
            with k.scope():
                Ab = k.sb("Ab", [128, 17, 32], BF16)
                Asum = k.sb("Asum", [128, 17, 32], F32)
                k.tt(Asum[:], A1s[:], A2s[:], ALU.add)
                k.cp(Ab[:], Asum[:])
                onesb = k.sb("onesb", [128, 128], BF16); k.memset(onesb[:], 1.0)
                trif = k.sb("trif", [128, 128], F32); trib = k.sb("trib", [128, 128], BF16)
                k.dma(trif[:], tri_d); k.cp(trib[:], trif[:])
                CS = k.sb("CS", [128, 17, 32], F32); RK = k.sb("RK", [128, 17, 32], F32); OFF = k.sb("OFF", [128, 17, 32], F32)
                Abf = Ab[:].rearrange("p t e -> p (t e)")
                for (lhs, dst) in ((onesb, CS), (trib, RK)):
                    p0, p1 = nbank(), nbank()
                    k.mm(p0[:, 0:512], lhs[:], Abf[:, 0:512])
                    k.mm(p1[:, 0:32], lhs[:], Abf[:, 512:544])
                    dflat = dst[:].rearrange("p t e -> p (t e)")
                    k.cp(dflat[:, 0:512], p0[:, 0:512])
                    k.cp(dflat[:, 512:544], p1[:, 0:32])
                k.memset(OFF[:, 0, :], 0.0)
                for ti in range(1, 17):
                    k.tt(OFF[:, ti, :], OFF[:, ti - 1, :], CS[:, ti - 1, :], ALU.add)
                CNT = k.sb("CNT", [128, 32], F32); NBK = k.sb("NBK", [128, 32], F32); NBI = k.sb("NBI", [128, 32], I32)
                PEND = k.sb("PEND", [128, 32], F32); PST = k.sb("PST", [128, 32], F32); ONE32 = k.sb("ONE32", [128, 32], F32)
                k.memset(ONE32[:], 1.0)
                k.tt(CNT[:], OFF[:, 16, :], CS[:, 16, :], ALU.add)
                k.ts(NBK[:], CNT[:], 1.0 / 256.0, 255.0 / 256.0 - 0.498046875, ALU.mult, ALU.add)
                k.cp(NBI[:], NBK[:])
                k.cp(NBK[:], NBI[:])
                k.s.add("dve", lambda h: h.tensor_tensor_scan(PEND[:], ONE32[:], NBK[:], 0.0, ALU.mult, ALU.add), [ONE32, NBK], [PEND])
                k.tt(PST[:], PEND[:], NBK[:], ALU.subtract)
                SLT = k.sb("SLT", [128, 17, 32], F32)
                k.tt(SLT[:], OFF[:], RK[:], ALU.add)
                k.ts(PST[:], PST[:], 256.0, None, ALU.mult)
                k.tt(SLT[:], SLT[:], PST[:].unsqueeze(1).to_broadcast([128, 17, 32]), ALU.add)
                SL = [k.sb("SL%d" % i, [128, 17], F32) for i in range(2)]
                for i_, Ax in enumerate((A1s, A2s)):
                    k.tt(Asum[:], Ax[:], SLT[:], ALU.mult)
                    k.s.add("dve", (lambda o_, i2: (lambda h: h.tensor_reduce(o_, i2, AX.X, ALU.add)))(SL[i_][:], Asum[:]), [Asum], [SL[i_]])
                    k.cp(SLi[i_][:], SL[i_][:])
                BIX = k.sb("BIX", [128, NB, 32], F32)
                k.dma(BIX[:], bidx_d)
                k.tt(BIX[:], PEND[:].unsqueeze(1).to_broadcast([128, NB, 32]), BIX[:], ALU.is_le)
                BE = k.sb("BE", [128, NB], F32)
                k.s.add("dve", lambda h: h.tensor_reduce(BE[:], BIX[:], AX.X, ALU.add), [BIX], [BE])
                pcol = k.sb("pcol", [128, 1], F32); k.dma(pcol[:], pcol_d)
                k.ts(BE[:], BE[:], 128.0, pcol[:, 0:1], ALU.mult, ALU.add)
                k.cp(BEi[:], BE[:])
            with k.scope():
                hld = [k.sb("hld%d" % i, [128, D], BF16) for i in range(4)]
                for ti, (t0, n) in enumerate(tiles):
                    hb_ = hld[ti % 4]
                    k.dma(hb_[0:n, :], hn_scr[t0:t0 + n, :])
                    for i_ in range(2):
                        k.s.add("pool", (lambda src, idx: (lambda h: h.indirect_dma_start(
                            out=xs_scr[:, :], out_offset=bass.IndirectOffsetOnAxis(ap=idx, axis=0), in_=src, in_offset=None)))(
                            hb_[0:n, :], SLi[i_][0:n, ti:ti + 1]), [hb_, SLi[i_]], [("xs_scr", 2 * ti + i_)], dma=True, semkey=(hb_.name, i_))
                wbe = [k.sb("wbe%d" % i, [128, 6144], BF16) for i in range(4)]
                for i in range(4):
                    k.memset(wbe[i][:], 0.0)
                xsb = [k.sb("xsb%d" % i, [128, 2, D], BF16) for i in range(3)]
                xsT = [k.sb("xsT%d" % i, [128, 8, 256], BF16) for i in range(2)]
                hmT = [k.sb("hmT%d" % i, [128, 2, 256], BF16) for i in range(2)]
                sgt = [k.sb("sgt%d" % i, [128, 256], F32) for i in range(2)]
                ybt = [k.sb("ybt%d" % i, [128, D], BF16) for i in range(2)]
                _bcc = {}

                def _bc(h):
                    if "r" not in _bcc:
                        _bcc["r"] = h.to_reg(4095)
                    return _bcc["r"]

                def mk_blk(bidx):
                    wb_ = wbe[bidx % 4]
                    Wg_ = wb_[:, 0:2048].rearrange("p (c n) -> p c n", c=8)
                    Wu_ = wb_[:, 2048:4096].rearrange("p (c n) -> p c n", c=8)
                    Wd_ = wb_[:, 4096:6144].rearrange("p (c n) -> p c n", c=2)
                    xb_, xT_, hm_ = xsb[bidx % 3], xsT[bidx % 2], hmT[bidx % 2]

                    def bl():
                        k.dma(xb_[:], xs_scr[bidx * 256:(bidx + 1) * 256, :].rearrange("(s p) d -> p s d", p=128), ins=["xs_scr"])

                    def bg():
                        k.s.add("pool", (lambda idx_: (lambda h: h.indirect_dma_start(
                            out=wb_[:, :], out_offset=None, in_=w_bf[:, :], in_offset=bass.IndirectOffsetOnAxis(ap=idx_, axis=0),
                            bounds_check=_bc(h), oob_is_err=False)))(BEi[:, bidx:bidx + 1]), [BEi, "w_bf"], [wb_], dma=True, semkey=wb_)

                    def b0():
                        for sub in range(2):
                            ptb = nbank()[:].bitcast(BF16)
                            for c in range(8):
                                k.tr(ptb[:, c * 128:(c + 1) * 128], xb_[:, sub, c * 128:(c + 1) * 128], ident[:])
                            if sub == 0:
                                k.act(xT_[:, :, 0:128], ptb.rearrange("p (c x) -> p c x", c=8), AF.Copy)
                            else:
                                k.cp(xT_[:, :, 128:256], ptb.rearrange("p (c x) -> p c x", c=8))

                    def b1():
                        for fcx in range(2):
                            pg_, pu_ = nbank(), nbank()
                            for c in range(8):
                                k.mm(pg_[:, 0:256], Wg_[:, c, fcx * 128:(fcx + 1) * 128], xT_[:, c, :], start=(c == 0), stop=(c == 7))
                            for c in range(8):
                                k.mm(pu_[:, 0:256], Wu_[:, c, fcx * 128:(fcx + 1) * 128], xT_[:, c, :], start=(c == 0), stop=(c == 7))
                            sg_ = sgt[fcx]
                            k.act(sg_[:], pg_[:, 0:256], AF.Silu)
                            k.tt(hm_[:, fcx, :], sg_[:], pu_[:, 0:256], ALU.mult)

                    def b2():
                        for sub in range(2):
                            yb_ = ybt[sub]
                            for sl_ in range(2):
                                py = nbank()
                                for fcx in range(2):
                                    k.mm(py[:, :], hm_[:, fcx, sub * 128:(sub + 1) * 128], Wd_[:, fcx, sl_ * 512:(sl_ + 1) * 512],
                                         start=(fcx == 0), stop=(fcx == 1))
                                if sub == 0:
                                    k.act(yb_[:, sl_ * 512:(sl_ + 1) * 512], py[:, :], AF.Copy)
                                else:
                                    k.cp(yb_[:, sl_ * 512:(sl_ + 1) * 512], py[:, :])
                            k.dma(ys_scr[bidx * 256 + sub * 128:bidx * 256 + (sub + 1) * 128, :], yb_[:], outs=["ys_scr"],
                                  q="act")
                    return (bl, bg, b0, b1, b2)

                blks = [mk_blk(b_) for b_ in range(NB)]
                for step in range(NB + 4):
                    for si_, sk_ in enumerate((0, 1, 2, 3, 4)):
                        if 0 <= step - sk_ < NB:
                            blks[step - sk_][si_]()
            with k.scope():
                ygt = [k.sb("ygt%d" % i, [128, D], BF16) for i in range(4)]
                hfin = [k.sb("hfin%d" % i, [128, D], F32) for i in range(2)]
                yo = [k.sb("yo%d" % i, [128, D], F32) for i in range(2)]
                junk = k.sb("junk3", [128, D], BF16)
                fs = [k.sb("fs%d" % i, [128, 1], F32) for i in range(2)]
                for ti, (t0, n) in enumerate(tiles):
                    b = ti % 2
                    k.dma(hfin[b][0:n, :], h_scr[t0:t0 + n, :])
                    for i_ in range(2):
                        yg = ygt[(2 * ti + i_) % 4]
                        k.s.add("pool", (lambda dst, idx_: (lambda h: h.indirect_dma_start(
                            out=dst, out_offset=None, in_=ys_scr[:, :], in_offset=bass.IndirectOffsetOnAxis(ap=idx_, axis=0))))(
                            yg[0:n, :], SLi[i_][0:n, ti:ti + 1]), ["ys_scr", SLi[i_]], [yg], dma=True, semkey=yg)
                        k.stt(hfin[b][0:n, :], yg[0:n, :], WW[0:n, ti, i_:i_ + 1], hfin[b][0:n, :], ALU.mult, ALU.add)
                    k.act(junk[0:n, :], hfin[b][0:n, :], AF.Square, accum_out=fs[b][0:n, :])
                    k.ts(fs[b][0:n, :], fs[b][0:n, :], 1.0 / D, 1e-6, ALU.mult, ALU.add)
                    k.act(fs[b][0:n, :], fs[b][0:n, :], AF.Sqrt)
                    k.recip(fs[b][0:n, :], fs[b][0:n, :])
                    k.stt(yo[b][0:n, :], hfin[b][0:n, :], fs[b][0:n, :], gfin[0:n, :], ALU.mult, ALU.mult)
                    k.dma(y_out[t0:t0 + n, :], yo[b][0:n, :], q="act")

        k.s.emit()
    return nc


_NC = {}


def _host_consts():
    pos = np.concatenate([np.arange(NPT), np.tile(2048 + np.arange(4), 16)]).astype(np.float32)
    half = 8
    inv = (500000.0 ** (-(np.arange(half, dtype=np.float32) / half))).astype(np.float32)
    ang = pos[:, None] * inv[None, :]
    rope = np.concatenate([np.cos(ang), np.sin(ang)], axis=1).astype(np.float32)
    diag = np.zeros((128, 32), np.float32)
    for gq in range(4):
        for c in range(16):
            diag[32 * gq + c, c] = 1.0
    NEG = -30000.0
    masks = np.zeros((128, 384), np.float32)
    kk = np.arange(128)[:, None]
    qq = np.arange(128)[None, :]
    masks[:, 0:128] = np.where(kk <= qq, 0.0, NEG)
    masks[:, 128:256] = np.where(kk >= qq, 0.0, NEG)
    k64 = np.arange(128)[:, None]
    q64 = np.arange(64)[None, :]
    same = (k64 // 4 == q64 // 4) & (k64 < 64)
    masks[:, 256:320] = np.where(same & (k64 % 4 <= q64 % 4), 0.0, NEG)
    masks[:, 320:384] = np.where(k64 == q64, 0.0, NEG)
    return rope, diag, masks


def kernel(_dbg=False, **inp):
    if _dbg not in _NC:
        _NC[_dbg] = build(_dbg)
    nc = _NC[_dbg]
    f = lambda a: np.ascontiguousarray(np.asarray(a, dtype=np.float32))
    rope, diag, masks = _host_consts()
    ident = np.eye(128, dtype=np.float32)
    w_in = f(inp["w_in"][0])
    g_attn = f(np.asarray(inp["g_attn_norm"][0]).reshape(8, 128).T)
    xp = np.asarray(inp["x_prompt"])
    xs = np.asarray(inp["x_sample"])
    s_are = f(np.asarray(inp["ssm_a_re"][0]).T)
    s_aim = f(np.asarray(inp["ssm_a_im"][0]).T)
    s_ldt = f(np.broadcast_to(np.asarray(inp["ssm_log_dt"][0])[None, :], (64, 32)))
    s_bre = f(np.transpose(np.asarray(inp["ssm_b_re"][0]), (1, 0, 2)))
    s_bim = f(np.transpose(np.asarray(inp["ssm_b_im"][0]), (1, 0, 2)))
    s_cre = f(np.transpose(np.asarray(inp["ssm_c_re"][0]), (2, 0, 1)))
    s_cim = f(np.transpose(np.asarray(inp["ssm_c_im"][0]), (2, 0, 1)))
    dd = np.asarray(inp["ssm_d"][0])
    s_d = np.zeros((128, 8), np.float32)
    for g in range(32):
        s_d[32 * (g % 4):32 * (g % 4) + 16, g // 4] = dd[g]
    st_all = np.asarray(inp["state_ssm"][0])
    common = {"rope": rope, "ident_in": ident, "diag_in": diag, "g_attn_in": g_attn, "w_in": w_in,
              "s_are": s_are, "s_aim": s_aim, "s_ldt": s_ldt, "s_bre": s_bre, "s_bim": s_bim,
              "s_cre": s_cre, "s_cim": s_cim, "s_d": s_d, "masks_in": masks}
    weg = np.asarray(inp["w_exp_gate"][0]).reshape(32, 8, 128, 256).transpose(0, 2, 1, 3).reshape(32, 128, 2048)
    weu = np.asarray(inp["w_exp_up"][0]).reshape(32, 8, 128, 256).transpose(0, 2, 1, 3).reshape(32, 128, 2048)
    wed = np.asarray(inp["w_exp_down"][0]).reshape(32, 2, 128, 1024).transpose(0, 2, 1, 3).reshape(32, 128, 2048)
    w_all = f(np.concatenate([weg, weu, wed], axis=2).reshape(4096, 6144))
    tri = (np.arange(128)[:, None] < np.arange(128)[None, :]).astype(np.float32)
    bidx = f(np.broadcast_to(np.arange(49, dtype=np.float32)[None, :, None], (128, 49, 32)))
    pcol = np.arange(128, dtype=np.float32).reshape(128, 1)
    common.update({"w_ab": f(inp["w_attn_branch"][0]), "w_glu": f(inp["w_glu"][0]), "w_out": f(inp["w_out"][0]),
                   "g_ffn_in": f(np.asarray(inp["g_ffn_norm"][0]).reshape(8, 128).T),
                   "w_rg": f(inp["w_router_group"][0]), "w_re": f(inp["w_router_expert"][0]),
                   "rbias_in": f(np.broadcast_to(np.concatenate([np.asarray(inp["b_router_group"][0]), np.asarray(inp["b_router_expert"][0])])[None, :], (128, 36))),
                   "gfin_in": f(np.broadcast_to(np.asarray(inp["g_final"])[None, :], (128, 1024))),
                   "w_all": w_all, "tri_in": tri, "bidx_in": bidx, "pcol_in": pcol,
                   "gffrow_in": f(np.broadcast_to(np.asarray(inp["g_ffn_norm"][0])[None, :], (128, 1024)))})
    c128 = np.asarray(inp["cache_kv_w128"][0])
    c512 = np.asarray(inp["cache_kv_w512"][0])
    c2048 = np.asarray(inp["cache_kv_w2048"][0])
    in_maps = []
    for c in range(8):
        xc = np.concatenate([xp[c], xs[16 * c:16 * c + 16].reshape(64, D)], axis=0)
        h0 = f(np.transpose(st_all[16 * c:16 * c + 16], (2, 3, 0, 1)))
        m = dict(common)
        sl = slice(16 * c, 16 * c + 16)
        m.update({"x": f(xc), "s_h0": h0,
                  "cache0": f(c128[sl]),
                  "cache1": f(c512[sl].reshape(16, 128, 4, 2, 8, 64)),
                  "cache2": f(c2048[sl].reshape(16, 128, 16, 2, 8, 64)[:, :, 0:4])})
        in_maps.append(m)
    res = run_bass_kernel_spmd(nc, in_maps, core_ids=list(range(8)))
    R = res.results
    if _dbg:
        kernel.dbg = R
    outs = []
    y_prompt = np.stack([R[c]["y_out"][0:NPT] for c in range(8)], axis=0)
    y_sample = np.concatenate([R[c]["y_out"][NPT:NT].reshape(16, 4, D) for c in range(8)], axis=0)
    outs += [y_prompt, y_sample]
    for g in range(3):
        outs.append(np.stack([R[c]["kvp%d" % g] for c in range(8)], axis=0)[None])
        outs.append(np.concatenate([R[c]["kvs%d" % g] for c in range(8)], axis=0)[None])
    sp = np.stack([np.transpose(R[c]["ssm_p"], (2, 0, 1)) for c in range(8)], axis=0)[None]
    ssv = np.concatenate([np.transpose(R[c]["ssm_s"], (2, 3, 0, 1)) for c in range(8)], axis=0)[None]
    outs.append(np.ascontiguousarray(sp.astype(np.float32)))
    outs.append(np.ascontiguousarray(ssv.astype(np.float32)))
    return tuple(outs)
```

```python
import numpy as np
from contextlib import ExitStack
import concourse.bass as bass
import concourse.mybir as mybir
from concourse.alu_op_type import AluOpType as ALU
from concourse.bass_utils import run_bass_kernel_spmd

F32 = mybir.dt.float32
BF16 = mybir.dt.bfloat16
I32 = mybir.dt.int32
AF = mybir.ActivationFunctionType
AX = mybir.AxisListType

NT = 2112
NPT = 2048
NS = 64
D = 1024
GROUPS = ((128, 1), (512, 4), (2048, 16))


class _Op:
    __slots__ = ("eng", "fn", "deps", "dma", "signal", "cnt", "sem", "val", "done", "isbar", "g")


class Sched:
    def __init__(self, nc, stack):
        self.nc = nc
        self.stack = stack
        self.eng = {"pe": nc.tensor, "act": nc.scalar, "dve": nc.vector, "pool": nc.gpsimd, "sp": nc.sync}
        self.ops = {e: [] for e in self.eng}
        self.state = {}
        self.dsem = {}
        self.last_dma = {}
        self.gcount = 0

    @staticmethod
    def _key(a):
        if isinstance(a, tuple):
            return a
        if isinstance(a, str):
            return (a, None)
        if hasattr(a, "tensor"):
            return (a.tensor.name, None)
        return (a.name, None)

    def _entries(self, key, create=True):
        name, sub = key
        d = self.state.setdefault(name, {})
        if create and sub not in d:
            d[sub] = [None, []]
        return [(s, e) for s, e in d.items() if s == sub or s is None or sub is None]

    def add(self, eng, fn, ins=(), outs=(), dma=False, semkey=None):
        op = _Op()
        op.eng, op.fn, op.dma, op.signal, op.deps = eng, fn, dma, False, set()
        op.cnt = op.val = 0
        op.sem = None
        for a in ins:
            if a is None:
                continue
            k = self._key(a)
            for s, e in self._entries(k):
                if e[0] is not None:
                    op.deps.add(e[0])
            self.state[k[0]][k[1]][1].append(op)
        for a in outs:
            if a is None:
                continue
            k = self._key(a)
            for s, e in self._entries(k):
                if e[0] is not None:
                    op.deps.add(e[0])
                for r in e[1]:
                    op.deps.add(r)
                if s == k[1] or k[1] is None:
                    e[0] = op
                    e[1] = []
            d = self.state[k[0]]
            for s in list(d.keys()):
                if s == k[1] or k[1] is None:
                    d[s] = [op, []]
        op.deps.discard(op)
        if dma:
            sk = self._key(semkey)
            if sk not in self.dsem:
                self.dsem[sk] = [self.stack.enter_context(self.nc.semaphore("d%d" % len(self.dsem))), 0]
            ent = self.dsem[sk]
            ent[1] += 16
            op.sem, op.val = ent[0], ent[1]
            self.last_dma[sk] = op
        op.g = self.gcount
        self.gcount += 1
        self.ops[eng].append(op)
        return op

    def interleave(self, g0, g1, g2):
        la, lb = g1 - g0, g2 - g1
        if la == 0 or lb == 0:
            return

        def pos(op):
            if op.g < g1:
                return (op.g - g0) * (la + lb) / la
            return (op.g - g1) * (la + lb) / lb + 0.5
        for e, lst in self.ops.items():
            head = [o for o in lst if getattr(o, "isbar", False) or o.g < g0]
            tail = [o for o in lst if not getattr(o, "isbar", False) and o.g >= g0]
            tail.sort(key=pos)
            self.ops[e] = head + tail

    def barrier(self, full=True):
        deps = set()
        for e, lst in self.ops.items():
            for op in reversed(lst):
                if not op.dma and not getattr(op, "isbar", False):
                    deps.add(op)
                    break
        bg = getattr(self, "bg_keys", set())
        deps |= set(op for sk, op in self.last_dma.items() if full or sk not in bg)
        for e in self.ops:
            op = _Op()
            op.eng, op.fn, op.dma, op.signal, op.deps = e, None, False, False, set(deps)
            op.cnt = op.val = 0
            op.sem = None
            op.isbar = True
            op.g = self.gcount
            self.ops[e].append(op)
        self.emit(final=False)

    def emit(self, final=True):
        nc = self.nc
        if not hasattr(self, "esem"):
            self.esem = {e: self.stack.enter_context(nc.semaphore("e_" + e)) for e in self.eng}
            self.ecount = {e: 0 for e in self.eng}
        esem = self.esem
        for e, lst in self.ops.items():
            for op in lst:
                for d in op.deps:
                    if d.dma or getattr(d, "done", False):
                        continue
                    if d.eng == "pe" and op.eng == "pe" and not op.dma:
                        continue
                    d.signal = True
        for e, lst in self.ops.items():
            c = self.ecount[e]
            for op in lst:
                if not op.dma and op.signal:
                    c += 1
                    op.sem, op.val = esem[e], c
            self.ecount[e] = c
        sched = self
        if not hasattr(self, "waited"):
            self.waited = {e: {} for e in self.eng}

        def run(e, h):
            waited = sched.waited[e]
            for op in sched.ops[e]:
                need = {}
                for d in op.deps:
                    if not d.dma and d.eng == "pe" and e == "pe" and not op.dma:
                        continue
                    if d.sem is None:
                        continue
                    sid = id(d.sem)
                    if sid not in need or need[sid][1] < d.val:
                        need[sid] = (d.sem, d.val)
                for sid, (s, v) in need.items():
                    if waited.get(sid, 0) < v:
                        h.wait_ge(s, v)
                        waited[sid] = v
                if op.fn is None:
                    continue
                ins = op.fn(h)
                if op.dma:
                    ins.then_inc(op.sem, 16)
                elif op.signal:
                    ins.then_inc(op.sem, 1)
            if e == "sp" and final:
                for sk, (s, tot) in sched.dsem.items():
                    if tot > 0:
                        h.wait_ge(s, tot)

        with nc.Block() as block:
            @block.tensor
            def _(h):
                run("pe", h)

            @block.scalar
            def _(h):
                run("act", h)

            @block.vector
            def _(h):
                run("dve", h)

            @block.gpsimd
            def _(h):
                run("pool", h)

            @block.sync
            def _(h):
                run("sp", h)
        for e in self.ops:
            for op in self.ops[e]:
                op.done = True
                op.fn = None
            self.ops[e] = []


class _Scope:
    def __init__(self, k, full=False):
        self.k = k
        self.full = full

    def __enter__(self):
        self.old = self.k.st
        self.es = ExitStack()
        self.es.__enter__()
        self.k.st = self.es
        return self

    def __exit__(self, *a):
        self.k.s.barrier(full=self.full)
        self.k.st = self.old
        return self.es.__exit__(*a)


def _is_sb(ap):
    return type(ap.tensor).__name__.startswith("SB")


class K:
    def __init__(self, nc, stack):
        self.nc = nc
        self.st = stack
        self.s = Sched(nc, stack)
        self.nps = 0

    def scope(self, full=False):
        return _Scope(self, full)

    def sb(self, name, shape, dt):
        return self.st.enter_context(self.nc.sbuf_tensor(name, list(shape), dt))

    def ps(self, name, shape, dt=F32):
        return self.st.enter_context(self.nc.psum_tensor(name, list(shape), dt))

    def dma(self, out, in_, q="sp", ins=None, outs=None, **kw):
        semkey = out if _is_sb(out) else in_
        if outs is not None and _is_sb(out):
            semkey = outs[0]
        elif ins is not None and not _is_sb(out):
            semkey = ins[0]
        return self.s.add(q, lambda h: h.dma_start(out=out, in_=in_, **kw),
                          ins if ins is not None else [in_], outs if outs is not None else [out],
                          dma=True, semkey=semkey)

    def mm(self, out, lhsT, rhs, start=True, stop=True, ins=None, outs=None, **kw):
        return self.s.add("pe", lambda h: h.matmul(out, lhsT, rhs, start=start, stop=stop, **kw),
                          ins if ins is not None else [lhsT, rhs], outs if outs is not None else [out])

    def tr(self, out, in_, ident, ins=None, outs=None):
        return self.s.add("pe", lambda h: h.transpose(out, in_, ident),
                          ins if ins is not None else [in_, ident], outs if outs is not None else [out])

    def act(self, out, in_, func, bias=None, scale=None, accum_out=None, ins=None, outs=None):
        kw = {}
        if bias is not None:
            kw["bias"] = bias
        if scale is not None:
            kw["scale"] = scale
        if accum_out is not None:
            kw["accum_out"] = accum_out
        i = [in_]
        for x in (bias, scale):
            if x is not None and not isinstance(x, (int, float)):
                i.append(x)
        o = [out] + ([accum_out] if accum_out is not None else [])
        return self.s.add("act", lambda h: h.activation(out, in_, func, **kw),
                          ins if ins is not None else i, outs if outs is not None else o)

    def tt(self, out, in0, in1, op, eng="dve", ins=None, outs=None):
        return self.s.add(eng, lambda h: h.tensor_tensor(out, in0, in1, op),
                          ins if ins is not None else [in0, in1], outs if outs is not None else [out])

    def ts(self, out, in0, s1, s2=None, op0=ALU.mult, op1=None, eng="dve", ins=None, outs=None):
        i = [in0] + [x for x in (s1, s2) if x is not None and not isinstance(x, (int, float))]
        if op1 is None:
            fn = lambda h: h.tensor_scalar(out, in0, s1, None, op0)
        else:
            fn = lambda h: h.tensor_scalar(out, in0, s1, s2, op0, op1)
        return self.s.add(eng, fn, ins if ins is not None else i, outs if outs is not None else [out])

    def stt(self, out, in0, scalar, in1, op0, op1, ins=None, outs=None):
        i = [in0, in1] + ([scalar] if not isinstance(scalar, (int, float)) else [])
        return self.s.add("dve", lambda h: h.scalar_tensor_tensor(out, in0, scalar, in1, op0, op1),
                          ins if ins is not None else i, outs if outs is not None else [out])

    def cp(self, out, in_, eng="dve", ins=None, outs=None):
        return self.s.add(eng, lambda h: h.tensor_copy(out, in_),
                          ins if ins is not None else [in_], outs if outs is not None else [out])

    def memset(self, ap, v, eng="dve"):
        return self.s.add(eng, lambda h: h.memset(ap, v), [], [ap])

    def recip(self, out, in_):
        return self.s.add("dve", lambda h: h.reciprocal(out, in_), [in_], [out])


TWO_PI = 6.283185
PW_SLOTS = list(range(9)) + [-4]


def build(dbg=False):
    nc = bass.Bass("TRN2", target_bir_lowering=False)
    dr = {}

    def din(name, shape, dt=F32):
        dr[name] = nc.dram_tensor(name, list(shape), dt, kind="ExternalInput").ap()
        return dr[name]

    def dout(name, shape, dt=F32):
        dr[name] = nc.dram_tensor(name, list(shape), dt, kind="ExternalOutput").ap()
        return dr[name]

    x = din("x", [NT, D])
    rope = din("rope", [NT, 16])
    ident_d = din("ident_in", [128, 128])
    diag_d = din("diag_in", [128, 32])
    g_attn = din("g_attn_in", [128, 8])
    w_in = din("w_in", [D, 7168])
    s_are = din("s_are", [64, 32]); s_aim = din("s_aim", [64, 32]); s_ldt = din("s_ldt", [64, 32])
    s_bre = din("s_bre", [64, 32, 16]); s_bim = din("s_bim", [64, 32, 16])
    s_cre = din("s_cre", [64, 32, 16]); s_cim = din("s_cim", [64, 32, 16])
    s_d = din("s_d", [128, 8])
    s_h0 = din("s_h0", [64, 2, 16, 32])
    masks_d = din("masks_in", [128, 384])
    w_ab = din("w_ab", [512, 1024]); w_glu = din("w_glu", [512, 2048]); w_out = din("w_out", [1024, 1024])
    g_ffn = din("g_ffn_in", [128, 8])
    w_rg = din("w_rg", [1024, 4]); w_re = din("w_re", [1024, 32]); rb_d = din("rbias_in", [128, 36])
    gfin_d = din("gfin_in", [128, 1024])
    w_all = din("w_all", [4096, 6144])
    tri_d = din("tri_in", [128, 128]); bidx_d = din("bidx_in", [128, 49, 32]); pcol_d = din("pcol_in", [128, 1])
    gffrow_d = din("gffrow_in", [128, 1024])
    hn_scr = nc.dram_tensor("hn_scr", [NT, D], BF16, kind="Internal").ap()
    rt_scr = nc.dram_tensor("rt_scr", [NT, 66], F32, kind="Internal").ap()
    sc_LAMr = nc.dram_tensor("sc_LAMr", [64, 10, 32], F32, kind="Internal").ap()
    sc_LAMi = nc.dram_tensor("sc_LAMi", [64, 10, 32], F32, kind="Internal").ap()
    sc_OutW = nc.dram_tensor("sc_OutW", [128, 8, 8, 4, 32], BF16, kind="Internal").ap()
    sc_Cm = nc.dram_tensor("sc_Cm", [128, 8, 4, 32], BF16, kind="Internal").ap()
    sc_KW = nc.dram_tensor("sc_KW", [128, 8, 8, 32], BF16, kind="Internal").ap()
    sc_SWc = nc.dram_tensor("sc_SWc", [128, 8, 8, 128], BF16, kind="Internal").ap()
    w_bf = nc.dram_tensor("w_bf", [4096, 6144], BF16, kind="Internal").ap()
    xs_scr = nc.dram_tensor("xs_scr", [49 * 256, D], BF16, kind="Internal").ap()
    ys_scr = nc.dram_tensor("ys_scr", [49 * 256, D], BF16, kind="Internal").ap()
    h_scr = nc.dram_tensor("h_scr", [NT, D], F32, kind="Internal").ap()
    y_out = dout("y_out", [NT, D])
    caches = [din("cache0", [16, 128, 2, 8, 64]), din("cache1", [16, 128, 4, 2, 8, 64]), din("cache2", [16, 128, 4, 2, 8, 64])]
    kvp = [dout("kvp0", [128, 2, 8, 64]), dout("kvp1", [512, 2, 8, 64]), dout("kvp2", [2048, 2, 8, 64])]
    kvs = [dout("kvs%d" % g, [16, 4, 2, 8, 64]) for g in range(3)]
    ssm_p = dout("ssm_p", [64, 2, 32])
    ssm_s = dout("ssm_s", [64, 2, 16, 32])
    if dbg:
        dbg_gy = dout("dbg_gy", [128, 8, NT], BF16)
        dbg_at = dout("dbg_at", [128, 4, NT], BF16)

    with ExitStack() as st:
        k = K(nc, st)
        ident = k.sb("ident", [128, 128], BF16)
        with k.scope():
            ident_f = k.sb("ident_f", [128, 128], F32)
            k.dma(ident_f[:], ident_d)
            k.cp(ident[:], ident_f[:])
        diag32 = k.sb("diag32", [128, 32], F32)
        k.dma(diag32[:], diag_d)
        gat = k.sb("gat", [128, 8], F32)
        k.dma(gat[:], g_attn)
        xnT = k.sb("xnT", [128, 8, NT], BF16)
        psb = [k.ps("psb%d" % i, [128, 512], F32) for i in range(8)]
        w_in_v = w_in.rearrange("(c p) n -> p c n", p=128)
        cvt = []
        cvs = {"e": 0}

        def conv_step(dep=None):
            e = cvs["e"]
            if e < 32:
                k.dma(cvt[e % 2][:].rearrange("p (a x) -> p a x", x=2048),
                      w_all[e * 128:(e + 1) * 128, :].rearrange("p (a x) -> p a x", x=2048), q="pool",
                      ins=([dep] if dep is not None else []))
            if 1 <= e <= 32:
                k.dma(w_bf[(e - 1) * 128:e * 128, :], cvt[(e - 1) % 2][:], q="pool", outs=[("w_bf", e)])
            cvs["e"] = e + 1

        tiles = [(i * 128, 128) for i in range(16)] + [(NPT, NS)]
        with k.scope():
            xt = [k.sb("xt%d" % i, [128, D], F32) for i in range(2)]
            xb = [k.sb("xb%d" % i, [128, D], BF16) for i in range(2)]
            junk = k.sb("junk", [128, D], BF16)
            ss = [k.sb("ss%d" % i, [128, 1], F32) for i in range(2)]
            rs = [k.sb("rs%d" % i, [128, 1], F32) for i in range(2)]
            _g0 = k.s.gcount
            LAMr = k.sb("LAMr", [64, 10, 32], F32); LAMi = k.sb("LAMi", [64, 10, 32], F32)
            OutW = k.sb("OutW", [128, 8, 8, 4, 32], BF16)
            Cm = k.sb("Cm", [128, 8, 4, 32], BF16)
            KW = k.sb("KW", [128, 8, 8, 32], BF16)
            SWc = k.sb("SWc", [128, 8, 8, 128], BF16)
            def T(name, shape, dt=F32):
                return k.sb(name, shape, dt)

            a_re = T("a_re", [64, 32]); a_im = T("a_im", [64, 32]); ldt = T("ldt", [64, 32])
            bre = T("bre", [64, 32, 16]); bim = T("bim", [64, 32, 16])
            cre = T("cre", [64, 32, 16]); cim = T("cim", [64, 32, 16])
            for t_, d_ in ((a_re, s_are), (a_im, s_aim), (ldt, s_ldt), (bre, s_bre), (bim, s_bim), (cre, s_cre), (cim, s_cim)):
                k.dma(t_[:], d_)
            dpad = T("dpad", [128, 8]); k.dma(dpad[:], s_d)
            lr = T("lr", [64, 32]); li = T("li", [64, 32])
            k.act(ldt[:], ldt[:], AF.Exp)
            k.tt(lr[:], a_re[:], ldt[:], ALU.mult)
            k.tt(li[:], a_im[:], ldt[:], ALU.mult)
            pass
            mag = T("mag", [64, 32]); yv = T("yv", [64, 32]); yi = T("yi", [64, 32], I32); yf = T("yf", [64, 32])
            fr = T("fr", [64, 32]); fc = T("fc", [64, 32]); msk = T("msk", [64, 32]); sn = T("sn", [64, 32]); cs = T("cs", [64, 32])
            for slot, tau in enumerate(PW_SLOTS):
                k.act(mag[:], lr[:], AF.Exp, scale=float(tau))
                k.ts(yv[:], li[:], float(tau) / (2 * np.pi), None, ALU.mult)
                k.cp(yi[:], yv[:])
                k.cp(yf[:], yi[:])
                k.tt(fr[:], yv[:], yf[:], ALU.subtract)
                k.ts(fc[:], fr[:], 0.25, None, ALU.add)
                k.ts(msk[:], fc[:], 0.5, None, ALU.is_gt)
                k.tt(fc[:], fc[:], msk[:], ALU.subtract)
                k.act(sn[:], fr[:], AF.Sin, scale=TWO_PI)
                k.act(cs[:], fc[:], AF.Sin, scale=TWO_PI)
                k.tt(LAMr[:, slot, :], mag[:], cs[:], ALU.mult)
                k.tt(LAMi[:, slot, :], mag[:], sn[:], ALU.mult)
            nre = T("nre", [64, 32]); den = T("den", [64, 32]); t0_ = T("t0_", [64, 32]); t1_ = T("t1_", [64, 32])
            fre = T("fre", [64, 32]); fim = T("fim", [64, 32])
            k.ts(nre[:], LAMr[:, 1, :], -1.0, None, ALU.add)
            k.tt(den[:], a_re[:], a_re[:], ALU.mult)
            k.tt(t0_[:], a_im[:], a_im[:], ALU.mult)
            k.tt(den[:], den[:], t0_[:], ALU.add)
            k.recip(den[:], den[:])
            k.tt(t0_[:], nre[:], a_re[:], ALU.mult)
            k.tt(t1_[:], LAMi[:, 1, :], a_im[:], ALU.mult)
            k.tt(t0_[:], t0_[:], t1_[:], ALU.add)
            k.tt(fre[:], t0_[:], den[:], ALU.mult)
            k.tt(t0_[:], LAMi[:, 1, :], a_re[:], ALU.mult)
            k.tt(t1_[:], nre[:], a_im[:], ALU.mult)
            k.tt(t0_[:], t0_[:], t1_[:], ALU.subtract)
            k.tt(fim[:], t0_[:], den[:], ALU.mult)
            bbr = T("bbr", [64, 32, 16]); bbi = T("bbi", [64, 32, 16])
            u1 = T("u1", [64, 32, 16]); u2 = T("u2", [64, 32, 16])

            def bc(ap2):
                return ap2.unsqueeze(2).to_broadcast([64, 32, 16])

            k.tt(u1[:], bre[:], bc(fre[:]), ALU.mult)
            k.tt(u2[:], bim[:], bc(fim[:]), ALU.mult)
            k.tt(bbr[:], u1[:], u2[:], ALU.subtract)
            k.tt(u1[:], bim[:], bc(fre[:]), ALU.mult)
            k.tt(u2[:], bre[:], bc(fim[:]), ALU.mult)
            k.tt(bbi[:], u1[:], u2[:], ALU.add)
            ZB = T("ZB", [128, 8, 8, 4, 32], BF16)
            pass
            pass
            k.memset(ZB[:], 0.0)
            k.memset(OutW[:], 0.0)
            k.memset(Cm[:], 0.0)

            def v4(ap3):
                return ap3.rearrange("p (c q) x -> p c q x", q=4)

            for tau in range(8):
                lrb, lib = bc(LAMr[:, tau, :]), bc(LAMi[:, tau, :])
                k.tt(u1[:], bbr[:], lrb, ALU.mult)
                k.tt(u2[:], bbi[:], lib, ALU.mult)
                k.tt(ZB[0:64, :, tau, :, 0:16], v4(u1[:]), v4(u2[:]), ALU.subtract, outs=[("ZB", tau)])
                k.tt(u1[:], bbi[:], lrb, ALU.mult)
                k.tt(u2[:], bbr[:], lib, ALU.mult)
                k.tt(ZB[64:128, :, tau, :, 0:16], v4(u1[:]), v4(u2[:]), ALU.add, outs=[("ZB", tau)])
            for j in range(8):
                lrb, lib = bc(LAMr[:, j + 1, :]), bc(LAMi[:, j + 1, :])
                k.tt(u1[:], cre[:], lrb, ALU.mult)
                k.tt(u2[:], cim[:], lib, ALU.mult)
                k.tt(OutW[0:64, :, j, :, 0:16], v4(u1[:]), v4(u2[:]), ALU.subtract, outs=[("OutW", j)])
                k.tt(u1[:], cre[:], lib, ALU.mult)
                k.tt(u2[:], cim[:], lrb, ALU.mult)
                k.tt(u1[:], u1[:], u2[:], ALU.add)
                k.ts(OutW[64:128, :, j, :, 0:16], v4(u1[:]), -1.0, None, ALU.mult, outs=[("OutW", j)])
            k.cp(Cm[0:64, :, :, 0:16], v4(cre[:]))
            k.ts(Cm[64:128, :, :, 0:16], v4(cim[:]), -1.0, None, ALU.mult)
            KWf = T("KWf", [128, 8, 8, 32], F32)
            pass
            for chunk in range(8):
                for tau in range(8):
                    slot = chunk * 8 + tau
                    bank = psb[2 + slot // 16]
                    c0 = (slot % 16) * 32
                    for gq in range(4):
                        k.mm(bank[32 * gq:32 * gq + 32, c0:c0 + 32], ZB[:, chunk, tau, gq, :], Cm[:, chunk, gq, :],
                             tile_position=(0, 32 * gq))
            for b4 in range(4):
                k.cp(KWf[:, 2 * b4:2 * b4 + 2, :, :], psb[2 + b4][:].rearrange("p (a t x) -> p a t x", a=2, t=8), eng="dve")
            for chunk in range(8):
                k.stt(KWf[:, chunk, 0, :], diag32[:], dpad[:, chunk:chunk + 1], KWf[:, chunk, 0, :], ALU.mult, ALU.add)
            k.cp(KW[:], KWf[:])
            pass
            for chunk in range(8):
                bankb = psb[4 + chunk % 4][:].bitcast(BF16)
                for i in range(8):
                    k.tr(bankb[:, i * 128:(i + 1) * 128], ZB[:, chunk, 7 - i, :, :].rearrange("p q x -> p (q x)"), ident[:])
                k.cp(SWc[:, chunk, :, :], bankb.rearrange("p (i x) -> p i x", i=8), eng=("dve" if chunk % 2 else "act") if False else "dve")

            for t_, d_ in ((LAMr, sc_LAMr), (LAMi, sc_LAMi), (OutW, sc_OutW), (Cm, sc_Cm), (KW, sc_KW), (SWc, sc_SWc)):
                k.s.add("pool", (lambda o_, i_: (lambda h: h.dma_start(out=o_, in_=i_)))(d_, t_[:]), [t_], [d_], dma=True, semkey="spill_st")
            _g1 = k.s.gcount
            for ti, (t0, n) in enumerate(tiles):
                b = ti % 2
                k.dma(xt[b][0:n, :], x[t0:t0 + n, :])
                k.act(junk[0:n, :], xt[b][0:n, :], AF.Square, accum_out=ss[b][0:n, :])
                k.ts(rs[b][0:n, :], ss[b][0:n, :], 1.0 / D, 1e-6, ALU.mult, ALU.add)
                k.act(rs[b][0:n, :], rs[b][0:n, :], AF.Sqrt)
                k.recip(rs[b][0:n, :], rs[b][0:n, :])
                k.act(xb[b][0:n, :], xt[b][0:n, :], AF.Copy, scale=rs[b][0:n, :])
                pt = psb[ti % 2]
                ptb = pt[:].bitcast(BF16)
                for c in range(8):
                    k.tr(ptb[:, c * 128:c * 128 + n], xb[b][0:n, c * 128:(c + 1) * 128], ident[0:n, 0:n])
                for c in range(8):
                    if c % 2 == 0:
                        k.act(xnT[:, c, t0:t0 + n], ptb[:, c * 128:c * 128 + n], AF.Copy, scale=gat[:, c:c + 1],
                              outs=[("xnT", ti)])
                    else:
                        k.ts(xnT[:, c, t0:t0 + n], ptb[:, c * 128:c * 128 + n], gat[:, c:c + 1], None, ALU.mult,
                             outs=[("xnT", ti)])

            k.s.interleave(_g0, _g1, k.s.gcount)
        with k.scope(full=True):
            attnT = k.sb("attnT", [128, 4, NT], BF16)
            with k.scope():
                wst = [k.sb("wst%d" % i, [128, 8, 256], F32) for i in range(3)]
                wbf = [k.sb("wbf0", [128, 8, 768], BF16)]
                qkf = [k.sb("qkf%d" % i, [128, 512], F32) for i in range(2)]
                vf = [k.sb("vf%d" % i, [128, 256], F32) for i in range(2)]
                qb = [k.sb("qb%d" % i, [128, 256], BF16) for i in range(2)]
                kb = [k.sb("kb%d" % i, [128, 256], BF16) for i in range(2)]
                rp = [k.sb("rp%d" % i, [128, 16], F32) for i in range(2)]
                tmp = [k.sb("rtmp%d" % i, [128, 8, 8], F32) for i in range(4)]
                mstage = k.sb("mstage", [128, 384], F32)
                maskb = k.sb("maskb", [128, 256], BF16)
                msamp = k.sb("msamp", [128, 2, 64], BF16)
                k.dma(mstage[:], masks_d)
                k.cp(maskb[:], mstage[:, 0:256])
                k.cp(msamp[:], mstage[:, 256:384].rearrange("p (a x) -> p a x", a=2))
                ones_f = k.sb("ones_f", [128, 64], F32)
                k.memset(ones_f[:], 1.0)
                QTz = k.sb("QTz", [128, 4, NT], BF16)
                KT = k.sb("KT", [128, 2, NT], BF16)
                Va = k.sb("Va", [128, 17, 4, 65], BF16)
                acc = k.sb("acc", [65, 4, NT], F32)
                PT = [k.sb("PT%d" % i, [128, 256], BF16) for i in range(4)]
                PTs4 = [k.sb("PTs%d" % i, [128, 64], BF16) for i in range(4)]
                cst = [k.sb("cst%d" % i, [128, 4, 2, 256], F32) for i in range(2)]
                kcb = [k.sb("kcb%d" % i, [128, 256], BF16) for i in range(4)]
                KcT = [k.sb("KcT%d" % i, [128, 2, 128], BF16) for i in range(4)]
                Vc = [k.sb("Vc%d" % i, [128, 4, 65], BF16) for i in range(4)]
                PTc = [k.sb("PTc%d" % i, [128, 4, 4], BF16) for i in range(4)]
                k.memset(QTz[:], 0.0)
                k.memset(Va[:], 1.0)
                for i in range(4):
                    k.memset(PTs4[i][:], 0.0, eng="pool")
                for i in range(4):
                    k.memset(Vc[i][:], 1.0, eng="pool")

                def pipeline(iters, skews):
                    n_ = len(iters)
                    for step in range(n_ + max(skews)):
                        for si, sk in enumerate(skews):
                            i_ = step - sk
                            if 0 <= i_ < n_:
                                iters[i_][si]()

                cnt = {"it": 0, "ai": 0, "ci": 0}
                for hh in range(2):
                    k.memset(acc[:], 0.0)
                    for g, (win, dil) in enumerate(GROUPS):
                        wb = wbf[0]
                        for j in range(3):
                            c0 = j * 1536 + g * 512 + hh * 256
                            k.dma(wst[j][:], w_in_v[:, :, c0:c0 + 256])
                            if j == 1:
                                k.act(wb[:, :, j * 256:(j + 1) * 256], wst[j][:], AF.Copy, outs=[(wb.name, j)])
                            else:
                                k.cp(wb[:, :, j * 256:(j + 1) * 256], wst[j][:], outs=[(wb.name, j)])
                        nblk = (NPT // dil) // 128
                        blocks = []
                        for r in range(dil):
                            for i in range(nblk):
                                blocks.append((r + dil * 128 * i, dil, 128, r, i))
                        blocks.append((NPT, 1, NS, None, None))

                        def mk_block(bi, tstart, tstep, n, r, i, hh=hh, g=g, win=win, dil=dil, wb=wb):
                            b = cnt["it"] % 2
                            cnt["it"] += 1
                            pq, pv = psb[2 + 2 * b], psb[3 + 2 * b]
                            tok = slice(tstart, tstart + tstep * (n - 1) + 1, tstep)
                            ptb = psb[6 + b][:].bitcast(BF16)
                            pc = slice(bi * 128, bi * 128 + n)

                            def s1():
                                for c in range(8):
                                    k.mm(pq[0:n, :], xnT[:, c, tok], wb[:, c, 0:512], start=(c == 0), stop=(c == 7),
                                         ins=["xnT", wb])
                                for c in range(8):
                                    k.mm(pv[0:n, 0:256], xnT[:, c, tok], wb[:, c, 512:768], start=(c == 0), stop=(c == 7),
                                         ins=["xnT", wb])
                                k.dma(rp[b][0:n, :], rope[tok, :])

                            def s2():
                                k.act(qkf[b][0:n, :], pq[0:n, :], AF.Copy)
                                k.act(vf[b][0:n, :], pv[0:n, 0:256], AF.Copy)
                                q3 = qkf[b][0:n, :].rearrange("p (h d) -> p h d", d=64)
                                x1, x2 = q3[:, :, 0:8], q3[:, :, 8:16]
                                cosb = rp[b][0:n, 0:8].unsqueeze(1).to_broadcast([n, 8, 8])
                                sinb = rp[b][0:n, 8:16].unsqueeze(1).to_broadcast([n, 8, 8])
                                t1, t2, t3, t4 = (t[0:n] for t in tmp)
                                k.tt(t1, x1, cosb, ALU.mult)
                                k.tt(t2, x2, sinb, ALU.mult)
                                k.tt(t3, x2, cosb, ALU.mult)
                                k.tt(t4, x1, sinb, ALU.mult)
                                k.tt(x1, t1, t2, ALU.subtract, outs=[qkf[b]])
                                k.tt(x2, t3, t4, ALU.add, outs=[qkf[b]])
                                kpart = qkf[b][0:n, 256:512].rearrange("p (h d) -> p h d", d=64)
                                vpart = vf[b][0:n, :].rearrange("p (h d) -> p h d", d=64)
                                hs = slice(hh * 4, hh * 4 + 4)
                                if r is None:
                                    dk = kvs[g][:, :, 0, hs, :].rearrange("s t h d -> (s t) h d")
                                    dv = kvs[g][:, :, 1, hs, :].rearrange("s t h d -> (s t) h d")
                                    k.dma(dk, kpart, q="pool")
                                    k.dma(dv, vpart, q="pool")
                                else:
                                    first = NPT - min(win, NPT)
                                    if tstart >= first:
                                        rows = slice(tstart - first, tstart - first + dil * 127 + 1, dil)
                                        k.dma(kvp[g][rows, 0, hs, :], kpart, q="pool")
                                        k.dma(kvp[g][rows, 1, hs, :], vpart, q="pool")
                                k.ts(qb[b][0:n, :], qkf[b][0:n, 0:256], 0.125, None, ALU.mult)
                                k.cp(kb[b][0:n, :], qkf[b][0:n, 256:512])
                                k.act(Va[0:n, bi, :, 0:64], vpart, AF.Copy, outs=[("Va", bi)])
                                for pr in range(2):
                                    k.tr(ptb[:, pr * 128:pr * 128 + n], qb[b][0:n, pr * 128:(pr + 1) * 128], ident[0:n, 0:n])
                                    k.tr(ptb[:, 256 + pr * 128:256 + pr * 128 + n], kb[b][0:n, pr * 128:(pr + 1) * 128],
                                         ident[0:n, 0:n])

                            def s3():
                                for pr in range(2):
                                    k.act(QTz[0:64, 2 * pr, pc], ptb[0:64, pr * 128:pr * 128 + n], AF.Copy, outs=[("QTz", bi)])
                                    k.cp(QTz[64:128, 2 * pr + 1, pc], ptb[64:128, pr * 128:pr * 128 + n], outs=[("QTz", bi)])
                                    if pr == 0:
                                        k.act(KT[:, pr, pc], ptb[:, 256 + pr * 128:256 + pr * 128 + n], AF.Copy, outs=[("KT", bi)])
                                    else:
                                        k.cp(KT[:, pr, pc], ptb[:, 256 + pr * 128:256 + pr * 128 + n], outs=[("KT", bi)])
                            return (s1, s2, s3)

                        pipeline([mk_block(bi, *blk) for bi, blk in enumerate(blocks)], (0, 1, 2))

                        def mk_att(h, r, i, g=g, dil=dil, nblk=nblk):
                            a = cnt["ai"] % 4
                            cnt["ai"] += 1
                            ps_s, ps_o = psb[a], psb[4 + a]
                            if r is None:
                                PTs = PTs4[a]
                                def a1():
                                    k.mm(ps_s[0:64, 0:64], KT[:, h // 2, NPT:NT], QTz[:, h, NPT:NT], start=True, stop=False,
                                         ins=["KT", "QTz"])
                                    k.mm(ps_s[0:64, 0:64], ident[:, 0:64], msamp[:, 0 if g == 0 else 1, :], start=False, stop=True)
                                    k.act(PTs[0:64, :], ps_s[0:64, 0:64], AF.Exp)

                                def a2():
                                    k.mm(ps_o[0:65, 0:64], Va[:, 16, h, :], PTs[:, :], ins=["Va", PTs])
                                    av = acc[0:65, h, NPT:NT]
                                    k.tt(av, av, ps_o[0:65, 0:64], ALU.add, ins=[acc, ps_o], outs=[acc])
                                return (a1, a2)
                            bi = r * nblk + i
                            nq = 2 if i + 1 < nblk else 1
                            kc = slice(bi * 128, bi * 128 + 128)
                            qc = slice(bi * 128, bi * 128 + 128 * nq)
                            N = 128 * nq

                            def a1():
                                k.mm(ps_s[:, 0:N], KT[:, h // 2, kc], QTz[:, h, qc], start=True, stop=False,
                                     ins=["KT", "QTz"])
                                k.mm(ps_s[:, 0:N], ident[:], maskb[:, 0:N], start=False, stop=True)
                                k.act(PT[a][:, 0:N], ps_s[:, 0:N], AF.Exp)

                            def a2():
                                k.mm(ps_o[0:65, 0:N], Va[:, bi, h, :], PT[a][:, 0:N], ins=["Va", PT[a]])
                                t0n = r + dil * 128 * i
                                av = acc[0:65, h, t0n:t0n + dil * (N - 1) + 1:dil]
                                k.tt(av, av, ps_o[0:65, 0:N], ALU.add, ins=[acc, ps_o], outs=[acc])
                            return (a1, a2)

                        its = [mk_att(h, r, i) for h in range(4) for r in range(dil) for i in range(nblk)]
                        its += [mk_att(h, None, None) for h in range(4)]
                        pipeline(its, (0, 2))

                        def mk_cache(s_, t_, nt_, nq, cb, g=g, hh=hh):
                            e = cnt["ci"] % 4
                            cnt["ci"] += 1
                            a = cnt["ai"] % 2
                            cnt["ai"] += 1
                            ps_s, ps_o = psb[a], psb[2 + a]
                            ptb = psb[6 + e % 2][:].bitcast(BF16)
                            q0 = NPT + 4 * s_ + (t_ if g > 0 else 0)

                            def c0():
                                if t_ == 0:
                                    if g == 0:
                                        k.dma(cst[cb][:, 0, :, :], caches[0][s_, :, :, hh * 4:hh * 4 + 4, :].rearrange("m a h d -> m a (h d)"))
                                    else:
                                        k.dma(cst[cb][:], caches[g][s_, :, :, :, hh * 4:hh * 4 + 4, :].rearrange("m t a h d -> m t a (h d)"))
                                k.cp(kcb[e][:], cst[cb][:, t_, 0, :])
                                k.act(Vc[e][:, :, 0:64], cst[cb][:, t_, 1, :].rearrange("p (h d) -> p h d", d=64), AF.Copy)
                                for pr in range(2):
                                    k.tr(ptb[:, pr * 128:(pr + 1) * 128], kcb[e][:, pr * 128:(pr + 1) * 128], ident[:])

                            def c1():
                                k.act(KcT[e][:], ptb[:, 0:256].rearrange("p (a x) -> p a x", a=2), AF.Copy)
                                for h in range(4):
                                    k.mm(ps_s[:, h * 4:h * 4 + nq], KcT[e][:, h // 2, :], QTz[:, h, q0:q0 + nq],
                                         start=True, stop=(g > 0), ins=[KcT[e], "QTz"])
                                    if g == 0:
                                        k.mm(ps_s[:, h * 4:h * 4 + nq], ident[:], maskb[:, 128:128 + nq], start=False, stop=True)
                                k.act(PTc[e][:, :, 0:nq], ps_s[:, 0:16].rearrange("p (h q) -> p h q", q=4)[:, :, 0:nq], AF.Exp)

                            def c2():
                                for h in range(4):
                                    k.mm(ps_o[0:65, h * 4:h * 4 + nq], Vc[e][:, h, :], PTc[e][:, h, 0:nq])
                                av = acc[0:65, :, q0:q0 + nq]
                                k.tt(av, av, ps_o[0:65, 0:16].rearrange("p (h q) -> p h q", q=4)[:, :, 0:nq], ALU.add,
                                     ins=[acc, ps_o], outs=[acc])
                            return (c0, c1, c2)

                        its = []
                        for s_ in range(16):
                            nt_, nq = (1, 4) if g == 0 else (4, 1)
                            for t_ in range(nt_):
                                its.append(mk_cache(s_, t_, nt_, nq, s_ % 2))
                        pipeline(its, (0, 1, 2))

                    for h in range(4):
                        k.recip(acc[64:65, h, :], acc[64:65, h, :])
                        for (t0, n) in [(i * 512, 512) for i in range(4)] + [(NPT, NS)]:
                            a = cnt["ai"] % 2
                            cnt["ai"] += 1
                            pbc = psb[a]
                            k.mm(pbc[0:64, 0:n], ones_f[64:65, 0:64], acc[64:65, h, t0:t0 + n], tile_position=(64, 0))
                            dst = attnT[(h % 2) * 64:(h % 2) * 64 + 64, hh * 2 + h // 2, t0:t0 + n]
                            k.tt(dst, acc[0:64, h, t0:t0 + n], pbc[0:64, 0:n], ALU.mult, outs=[attnT])
                if dbg:
                    k.dma(dbg_at, attnT[:], q="pool")
            uT = k.sb("uT", [128, 8, NT], BF16)
            cvt.extend([k.sb("cvt%d" % i, [128, 6144], BF16) for i in range(2)])
            k.s.bg_keys = set((c_.name, None) for c_ in cvt)

            with k.scope():
                LAMr = k.sb("LAMr_s", [64, 10, 32], F32); LAMi = k.sb("LAMi_s", [64, 10, 32], F32)
                OutW = k.sb("OutW_s", [128, 8, 8, 4, 32], BF16)
                Cm = k.sb("Cm_s", [128, 8, 4, 32], BF16)
                KW = k.sb("KW_s", [128, 8, 8, 32], BF16)
                SWc = k.sb("SWc_s", [128, 8, 8, 128], BF16)
                _lds = []
                for t_, d_ in ((LAMr, sc_LAMr), (LAMi, sc_LAMi), (OutW, sc_OutW), (Cm, sc_Cm), (KW, sc_KW), (SWc, sc_SWc)):
                    _lds.append(k.s.add("sp", (lambda o_, i_: (lambda h: h.dma_start(out=o_, in_=i_)))(t_[:], d_), [d_], [t_], dma=True, semkey="spill_ld"))
                for o_ in _lds:
                    o_.val = _lds[-1].val

                def T(name, shape, dt=F32):
                    return k.sb(name, shape, dt)

                with k.scope():
                    Wu = T("Wu", [128, 8, 8, 4, 32], BF16)
                    wst = [k.sb("wsu%d" % i, [128, 8, 256], F32) for i in range(2)]
                    k.memset(Wu[:], 0.0)
                    for h in range(2):
                        k.dma(wst[h][:], w_in_v[:, :, 4608 + 256 * h:4608 + 256 * h + 256])
                        for c in range(8):
                            k.cp(Wu[:, c, 4 * h:4 * h + 4, :, 0:16], wst[h][:, c, :].rearrange("p (a q x) -> p a q x", a=4, q=4),
                                 eng="dve")
                    pass
                    ranges = [(i * 512, 512) for i in range(4)] + [(NPT, NS)]
                    it = 0
                    for chunk in range(8):
                        for (t0, n) in ranges:
                            pb = psb[it % 4]
                            it += 1
                            for c in range(8):
                                k.mm(pb[:, 0:n], Wu[:, c, chunk, :, :].rearrange("p q x -> p (q x)"), xnT[:, c, t0:t0 + n],
                                     start=(c == 0), stop=(c == 7), ins=[Wu, "xnT"])
                            if it % 2:
                                k.act(uT[:, chunk, t0:t0 + n], pb[:, 0:n], AF.Copy, outs=[("uT", chunk)])
                            else:
                                k.cp(uT[:, chunk, t0:t0 + n], pb[:, 0:n], outs=[("uT", chunk)])
                HP = T("HP", [128, 32, 256], BF16)
                k.memset(HP[:, :, 0:1], 0.0)
                with k.scope():
                    SS = [T("SS0", [64, 64, 2, 32])]
                    Hr = T("Hr", [64, 64, 3, 32])
                    Z3 = T("Z3", [64, 3, 32]); k.memset(Z3[:], 0.0)
                    AR2 = T("AR2", [64, 2, 32]); AI2 = T("AI2", [64, 2, 32])
                    k.cp(AR2[:, 0, :], LAMr[:, 8, :]); k.cp(AR2[:, 1, :], LAMr[:, 8, :])
                    k.ts(AI2[:, 0, :], LAMi[:, 8, :], -1.0, None, ALU.mult); k.cp(AI2[:, 1, :], LAMi[:, 8, :])
                    sA = T("sA", [64, 2, 32]); sB = T("sB", [64, 2, 32])
                    fill = 0
                    for r in range(4):
                        S_ = SS[0]
                        for gq in range(4):
                            for ch in range(2):
                                bank = psb[fill % 8]
                                fill += 1
                                for cc in range(4):
                                    chunk = ch * 4 + cc
                                    for ri in range(2):
                                        c0 = (cc * 2 + ri) * 64
                                        for i in range(8):
                                            k.mm(bank[0:64, c0:c0 + 64],
                                                 SWc[32 * gq:32 * gq + 32, chunk, i, ri * 64:(ri + 1) * 64],
                                                 uT[32 * gq:32 * gq + 32, chunk, 512 * r + i:512 * r + 512:8],
                                                 start=(i == 0), stop=(i == 7), tile_position=(32 * gq, 0),
                                                 ins=[SWc, ("uT", chunk)])
                                g0 = gq + 16 * ch
                                src = bank[0:64, :].rearrange("p (c r k) -> p k r c", c=4, r=2)
                                if fill % 2:
                                    k.act(S_[:, :, :, g0:g0 + 13:4], src, AF.Copy, outs=[(S_.name, fill % 8)])
                                else:
                                    k.cp(S_[:, :, :, g0:g0 + 13:4], src, outs=[(S_.name, fill % 8)])
                        for kk in range(64):
                            prev = Z3[:] if (r == 0 and kk == 0) else (Hr[:, 63, :, :] if kk == 0 else Hr[:, kk - 1, :, :])
                            k.tt(sA[:], prev[:, 0:2, :], AR2[:], ALU.mult)
                            k.tt(sB[:], prev[:, 1:3, :], AI2[:], ALU.mult)
                            k.tt(sA[:], sA[:], sB[:], ALU.add)
                            k.tt(Hr[:, kk, 0:2, :], sA[:], S_[:, kk, :, :], ALU.add, ins=[sA, S_])
                            k.cp(Hr[:, kk, 2, :], Hr[:, kk, 0, :])
                            if kk % 32 == 31:
                                conv_step(Hr)
                        ncol = 64 if r < 3 else 63
                        k.cp(HP[0:64, :, 64 * r + 1:64 * r + 1 + ncol], Hr[:, 0:ncol, 0, :].rearrange("p k g -> p g k"))
                        k.cp(HP[64:128, :, 64 * r + 1:64 * r + 1 + ncol], Hr[:, 0:ncol, 1, :].rearrange("p k g -> p g k"))
                    k.dma(ssm_p, Hr[:, 63, 0:2, :], q="pool")
                HPs = T("HPs", [128, 32, 16], BF16)
                with k.scope():
                    SSs = T("SSs", [64, 2, 16, 32])
                    for gq in range(4):
                        for ri in range(2):
                            bank = psb[gq * 2 + ri]
                            for chunk in range(8):
                                for i in range(4):
                                    k.mm(bank[0:64, chunk * 16:(chunk + 1) * 16],
                                         SWc[32 * gq:32 * gq + 32, chunk, i, ri * 64:(ri + 1) * 64],
                                         uT[32 * gq:32 * gq + 32, chunk, NPT + i:NT:4],
                                         start=(i == 0), stop=(i == 3), tile_position=(32 * gq, 0),
                                         ins=[SWc, ("uT", chunk)])
                            k.cp(SSs[:, ri, :, gq:32:4], bank[0:64, 0:128].rearrange("p (c s) -> p s c", c=8))
                    h0P = T("h0P", [64, 2, 16, 32])
                    k.dma(h0P[:], s_h0)

                    def bs(ap2):
                        return ap2.unsqueeze(1).to_broadcast([64, 16, 32])

                    w1 = T("w1", [64, 16, 32]); w2 = T("w2", [64, 16, 32]); Wr_ = T("Wr_", [64, 16, 32]); Wi_ = T("Wi_", [64, 16, 32])
                    nsP = T("nsP", [64, 2, 16, 32])
                    ar8, ai8, l4r, l4i = bs(LAMr[:, 8, :]), bs(LAMi[:, 8, :]), bs(LAMr[:, 9, :]), bs(LAMi[:, 9, :])
                    k.tt(w1[:], h0P[:, 0], ar8, ALU.mult); k.tt(w2[:], h0P[:, 1], ai8, ALU.mult)
                    k.tt(w1[:], w1[:], w2[:], ALU.subtract); k.tt(Wr_[:], w1[:], SSs[:, 0], ALU.add)
                    k.tt(w1[:], h0P[:, 1], ar8, ALU.mult); k.tt(w2[:], h0P[:, 0], ai8, ALU.mult)
                    k.tt(w1[:], w1[:], w2[:], ALU.add); k.tt(Wi_[:], w1[:], SSs[:, 1], ALU.add)
                    k.tt(w1[:], Wr_[:], l4r, ALU.mult); k.tt(w2[:], Wi_[:], l4i, ALU.mult)
                    k.tt(nsP[:, 0], w1[:], w2[:], ALU.subtract)
                    k.tt(w1[:], Wi_[:], l4r, ALU.mult); k.tt(w2[:], Wr_[:], l4i, ALU.mult)
                    k.tt(nsP[:, 1], w1[:], w2[:], ALU.add)
                    k.dma(ssm_s, nsP[:], q="pool")
                    k.cp(HPs[0:64], h0P[:, 0].rearrange("p s g -> p g s"))
                    k.cp(HPs[64:128], h0P[:, 1].rearrange("p s g -> p g s"))
                KWbd = T("KWbd", [128, 8, 8, 128], BF16)
                k.memset(KWbd[:], 0.0)
                for gq in range(4):
                    k.cp(KWbd[32 * gq:32 * gq + 32, :, :, 32 * gq:32 * gq + 32], KW[32 * gq:32 * gq + 32, :, :, :])
                for chunk in range(8):
                    for j in range(8):
                        bank = psb[j]
                        for i in range(j + 1):
                            k.mm(bank[:, 0:256], KWbd[:, chunk, j - i, :], uT[:, chunk, i:NPT:8], start=(i == 0), stop=False,
                                 ins=[KWbd, ("uT", chunk)])
                            if j < 4:
                                k.mm(bank[:, 256:272], KWbd[:, chunk, j - i, :], uT[:, chunk, NPT + i:NT:4], start=False, stop=False,
                                     ins=[KWbd, ("uT", chunk)])
                    for gq in range(4):
                        g = chunk * 4 + gq
                        for j in range(8):
                            bank = psb[j]
                            k.mm(bank[32 * gq:32 * gq + 32, 0:256], OutW[:, chunk, j, gq, :], HP[:, g, :],
                                 start=False, stop=True, tile_position=(0, 32 * gq))
                            if j < 4:
                                k.mm(bank[32 * gq:32 * gq + 32, 256:272], OutW[:, chunk, j, gq, :], HPs[:, g, :],
                                     start=False, stop=True, tile_position=(0, 32 * gq))
                    for j in range(8):
                        k.act(uT[:, chunk, j:NPT:8], psb[j][:, 0:256], AF.Gelu_apprx_tanh, outs=[("uT", chunk)])
                        if j < 4:
                            k.act(uT[:, chunk, NPT + j:NT:4], psb[j][:, 256:272], AF.Gelu_apprx_tanh, outs=[("uT", chunk)])
                    conv_step(("uT", chunk))
                if dbg:
                    k.dma(dbg_gy, uT[:], q="pool")


            mergedT = k.sb("mergedT", [128, 8, NT], BF16)
            ranges = [(i * 512, 512) for i in range(4)] + [(NPT, NS)]
            wab_v = w_ab.rearrange("(c p) n -> p c n", p=128)
            wglu_v = w_glu.rearrange("(ch q c) n -> q c ch n", q=4, c=16)
            ring = [0]

            def nbank():
                ring[0] += 1
                return psb[ring[0] % 8]

            with k.scope():
                sAB = k.sb("sAB", [128, 4, 128], F32)
                sG = k.sb("sG", [128, 8, 2, 128], F32)
                sL = k.sb("sL", [128, 8, 2, 128], F32)
                k.memset(sL[:], 0.0)
                mw = [(k.sb("mAB%d" % i, [128, 4, 128], BF16), k.sb("mG%d" % i, [128, 8, 2, 128], BF16),
                       k.sb("mL%d" % i, [128, 8, 2, 128], BF16)) for i in range(2)]
                mt = [[k.sb("mt%d_%d" % (i, j), [128, 512], F32) for j in range(5)] for i in range(2)]
                mi = 0
                for fc in range(8):
                    AB, G_, L_ = mw[fc % 2]
                    k.dma(sAB[:], wab_v[:, :, fc * 128:(fc + 1) * 128])
                    for j in range(2):
                        k.dma(sG[:, :, j, :], w_in_v[:, :, 5120 + j * 1024 + fc * 128:5120 + j * 1024 + (fc + 1) * 128], outs=[("sG", j)])
                        for gq in range(4):
                            k.dma(sL[32 * gq:32 * gq + 16, :, j, :], wglu_v[gq, :, :, j * 1024 + fc * 128:j * 1024 + (fc + 1) * 128],
                                  outs=[("sL", j * 4 + gq)])
                    k.cp(AB[:], sAB[:])
                    k.act(G_[:], sG[:], AF.Copy)
                    k.cp(L_[:], sL[:])
                    for (t0, n) in ranges:
                        tl = slice(t0, t0 + n)
                        s1, s2, s3, m1, m2 = mt[mi % 2]
                        mi += 1
                        p_ao, p_ga, p_gs, p_la, p_lb = nbank(), nbank(), nbank(), nbank(), nbank()
                        for c in range(4):
                            k.mm(p_ao[:, 0:n], AB[:, c, :], attnT[:, c, tl], start=(c == 0), stop=(c == 3))
                        for j, pb in ((0, p_ga), (1, p_gs)):
                            for c in range(8):
                                k.mm(pb[:, 0:n], G_[:, c, j, :], xnT[:, c, tl], start=(c == 0), stop=(c == 7), ins=[G_, "xnT"])
                        for j, pb in ((0, p_la), (1, p_lb)):
                            for c in range(8):
                                k.mm(pb[:, 0:n], L_[:, c, j, :], uT[:, c, tl], start=(c == 0), stop=(c == 7), ins=[L_, "uT"])
                        k.act(s1[:, 0:n], p_ga[:, 0:n], AF.Sigmoid)
                        k.act(s2[:, 0:n], p_gs[:, 0:n], AF.Sigmoid)
                        k.act(s3[:, 0:n], p_lb[:, 0:n], AF.Sigmoid)
                        k.tt(m1[:, 0:n], p_ao[:, 0:n], s1[:, 0:n], ALU.mult)
                        k.tt(m2[:, 0:n], p_la[:, 0:n], s3[:, 0:n], ALU.mult)
                        k.tt(m2[:, 0:n], m2[:, 0:n], s2[:, 0:n], ALU.mult)
                        k.tt(mergedT[:, fc, tl], m1[:, 0:n], m2[:, 0:n], ALU.add, outs=[("mergedT", fc)])
                        if mi % 2 == 0:
                            conv_step(("mergedT", fc))
            while cvs["e"] <= 32:
                conv_step()
            with k.scope():
                wos = [k.sb("wos%d" % i, [128, 8, 256], F32) for i in range(2)]
                Wo = k.sb("Wo", [128, 8, 1024], BF16)
                wout_v = w_out.rearrange("(c p) n -> p c n", p=128)
                for j in range(4):
                    k.dma(wos[j % 2][:], wout_v[:, :, j * 256:(j + 1) * 256])
                    k.act(Wo[:, :, j * 256:(j + 1) * 256], wos[j % 2][:], AF.Copy, outs=[("Wo", j)])
                gffrow = k.sb("gffrow", [128, D], F32)
                k.dma(gffrow[:], gffrow_d)
                hnb = [k.sb("hnb%d" % i, [128, D], BF16) for i in range(2)]
                gff = k.sb("gff", [128, 8], F32)
                k.dma(gff[:], g_ffn)
                xt = [k.sb("xu%d" % i, [128, D], F32) for i in range(2)]
                ht = [k.sb("ht%d" % i, [128, D], F32) for i in range(2)]
                hb = [k.sb("hb%d" % i, [128, D], BF16) for i in range(2)]
                junk = k.sb("junk2", [128, D], BF16)
                ss = [k.sb("su%d" % i, [128, 1], F32) for i in range(2)]
                rs = [k.sb("ru%d" % i, [128, 1], F32) for i in range(2)]
                def mk_wo(ti, t0, n):
                    b = ti % 2
                    tl = slice(t0, t0 + n)
                    pbs = (nbank(), nbank())
                    ptb = nbank()[:].bitcast(BF16)

                    def w1():
                        k.dma(xt[b][0:n, :], x[tl, :])
                        for sl_ in range(2):
                            for c in range(8):
                                k.mm(pbs[sl_][0:n, :], mergedT[:, c, tl], Wo[:, c, sl_ * 512:(sl_ + 1) * 512],
                                     start=(c == 0), stop=(c == 7), ins=["mergedT", "Wo"])

                    def w2():
                        for sl_ in range(2):
                            k.tt(ht[b][0:n, sl_ * 512:(sl_ + 1) * 512], pbs[sl_][0:n, :], xt[b][0:n, sl_ * 512:(sl_ + 1) * 512], ALU.add)
                        k.dma(h_scr[tl, :], ht[b][0:n, :], q="pool")
                        k.act(junk[0:n, :], ht[b][0:n, :], AF.Square, accum_out=ss[b][0:n, :])
                        k.ts(rs[b][0:n, :], ss[b][0:n, :], 1.0 / D, 1e-6, ALU.mult, ALU.add)
                        k.act(rs[b][0:n, :], rs[b][0:n, :], AF.Sqrt)
                        k.recip(rs[b][0:n, :], rs[b][0:n, :])
                        k.act(hb[b][0:n, :], ht[b][0:n, :], AF.Copy, scale=rs[b][0:n, :])
                        k.tt(hnb[b][0:n, :], hb[b][0:n, :], gffrow[0:n, :], ALU.mult)
                        k.dma(hn_scr[tl, :], hnb[b][0:n, :], q="pool")
                        for c in range(8):
                            k.tr(ptb[:, c * 128:c * 128 + n], hb[b][0:n, c * 128:(c + 1) * 128], ident[0:n, 0:n])

                    def w3():
                        for c in range(8):
                            if c % 2 == 0:
                                k.act(xnT[:, c, tl], ptb[:, c * 128:c * 128 + n], AF.Copy, scale=gff[:, c:c + 1], outs=[("xnT", ti)])
                            else:
                                k.ts(xnT[:, c, tl], ptb[:, c * 128:c * 128 + n], gff[:, c:c + 1], None, ALU.mult, outs=[("xnT", ti)])

                    return (w1, w2, w3)

                wos_ = [mk_wo(ti, t0, n) for ti, (t0, n) in enumerate(tiles)]
                for step in range(len(wos_) + 2):
                    for si_, sk_ in enumerate((0, 1, 2)):
                        if 0 <= step - sk_ < len(wos_):
                            wos_[step - sk_][si_]()
        hnT = xnT
        with k.scope():
            gfin = k.sb("gfin", [128, D], F32)
            k.dma(gfin[:], gfin_d)
            A1s = k.sb("A1s", [128, 17, 32], F32); A2s = k.sb("A2s", [128, 17, 32], F32)
            WW = k.sb("WW", [128, 17, 2], F32)
            k.memset(A1s[:], 0.0); k.memset(A2s[:], 0.0); k.memset(WW[:], 0.0)
            with k.scope():
                rst = k.sb("rst", [128, 8, 36], F32)
                Wr = k.sb("Wr", [128, 8, 36], BF16)
                k.dma(rst[:, :, 0:4], w_rg.rearrange("(c p) n -> p c n", p=128))
                k.dma(rst[:, :, 4:36], w_re.rearrange("(c p) n -> p c n", p=128))
                k.cp(Wr[:], rst[:])
                rbias = k.sb("rbias", [128, 36], F32)
                k.dma(rbias[:], rb_d)
                LG = k.sb("LG", [128, 17, 36], F32)
                k.memset(LG[:], 0.0)
                for ti, (t0, n) in enumerate(tiles):
                    pb = nbank()
                    for c in range(8):
                        k.mm(pb[0:n, 0:36], hnT[:, c, t0:t0 + n], Wr[:, c, :], start=(c == 0), stop=(c == 7), ins=["xnT", Wr])
                    k.tt(LG[0:n, ti, :], pb[0:n, 0:36], rbias[0:n, :], ALU.add, outs=[("LG", ti)])

                def R(name, shape):
                    return k.sb(name, shape, F32)

                def red(out, in_, op):
                    return k.s.add("dve", lambda h: h.tensor_reduce(out, in_, AX.X, op), [in_], [out])

                def b3(ap2, n3):
                    return ap2.unsqueeze(2).to_broadcast([128, 17, n3])
                lg4 = LG[:, :, 0:4]
                le4 = LG[:, :, 4:36].rearrange("p t (g e) -> p t g e", e=8)
                mx = R("r_mx", [128, 17]); ohb = R("r_oh", [128, 17, 4]); e4b = R("r_e4", [128, 17, 4])
                se = R("r_se", [128, 17]); pg = R("r_pg", [128, 17])
                red(mx[:], lg4, ALU.max)
                k.tt(ohb[:], lg4, b3(mx[:], 4), ALU.is_equal)
                k.tt(e4b[:], lg4, b3(mx[:], 4), ALU.subtract)
                k.act(e4b[:], e4b[:], AF.Exp)
                red(se[:], e4b[:], ALU.add)
                k.recip(pg[:], se[:])
                legb = R("r_leg", [128, 17, 8]); tmp8 = R("r_t8", [128, 17, 8])
                for g_ in range(4):
                    ohg = ohb[:, :, g_:g_ + 1].to_broadcast([128, 17, 8])
                    if g_ == 0:
                        k.tt(legb[:], le4[:, :, 0, :], ohg, ALU.mult)
                    else:
                        k.tt(tmp8[:], le4[:, :, g_, :], ohg, ALU.mult)
                        k.tt(legb[:], legb[:], tmp8[:], ALU.add)
                v1 = R("r_v1", [128, 17]); v2 = R("r_v2", [128, 17]); m1b = R("r_m1", [128, 17, 8]); m2b = R("r_m2", [128, 17, 8])
                red(v1[:], legb[:], ALU.max)
                k.tt(m1b[:], legb[:], b3(v1[:], 8), ALU.is_equal)
                k.stt(tmp8[:], m1b[:], -1.0e30, legb[:], ALU.mult, ALU.add)
                red(v2[:], tmp8[:], ALU.max)
                k.tt(m2b[:], tmp8[:], b3(v2[:], 8), ALU.is_equal)
                ex = R("r_ex", [128, 17]); w1_ = R("r_w1", [128, 17]); w2_ = R("r_w2", [128, 17])
                k.tt(ex[:], v2[:], v1[:], ALU.subtract)
                k.act(ex[:], ex[:], AF.Exp)
                k.ts(w1_[:], ex[:], 1.0, None, ALU.add)
                k.recip(w1_[:], w1_[:])
                k.tt(w2_[:], ex[:], w1_[:], ALU.mult)
                k.tt(WW[:, :, 0], w1_[:], pg[:], ALU.mult)
                k.tt(WW[:, :, 1], w2_[:], pg[:], ALU.mult)
                for g_ in range(4):
                    ohg = ohb[:, :, g_:g_ + 1].to_broadcast([128, 17, 8])
                    k.tt(A1s[:, :, g_ * 8:(g_ + 1) * 8], m1b[:], ohg, ALU.mult)
                    k.tt(A2s[:, :, g_ * 8:(g_ + 1) * 8], m2b[:], ohg, ALU.mult)
                k.memset(A1s[NS:128, 16, :], 0.0)
                k.memset(A2s[NS:128, 16, :], 0.0)
                k.memset(WW[NS:128, 16, :], 0.0)
            NB = 49
            SLi = [k.sb("SLi%d" % i, [128, 17], I32) for i in range(2)]
            BEi = k.sb("BEi", [128, NB], I32)
            with k.scope():
                Ab = k.sb("Ab", [128, 17, 32], BF16)
                Asum = k.sb("Asum", [128, 17, 32], F32)
                k.tt(Asum[:], A1s[:], A2s[:], ALU.add)
                k.cp(Ab[:], Asum[:])
                onesb = k.sb("onesb", [128, 128], BF16); k.memset(onesb[:], 1.0)
                trif = k.sb("trif", [128, 128], F32); trib = k.sb("trib", [128, 128], BF16)
                k.dma(trif[:], tri_d); k.cp(trib[:], trif[:])
                CS = k.sb("CS", [128, 17, 32], F32); RK = k.sb("RK", [128, 17, 32], F32); OFF = k.sb("OFF", [128, 17, 32], F32)
                Abf = Ab[:].rearrange("p t e -> p (t e)")
                for (lhs, dst) in ((onesb, CS), (trib, RK)):
                    p0, p1 = nbank(), nbank()
                    k.mm(p0[:, 0:512], lhs[:], Abf[:, 0:512])
                    k.mm(p1[:, 0:32], lhs[:], Abf[:, 512:544])
                    dflat = dst[:].rearrange("p t e -> p (t e)")
                    k.cp(dflat[:, 0:512], p0[:, 0:512])
                    k.cp(dflat[:, 512:544], p1[:, 0:32])
                k.memset(OFF[:, 0, :], 0.0)
                for ti in range(1, 17):
                    k.tt(OFF[:, ti, :], OFF[:, ti - 1, :], CS[:, ti - 1, :], ALU.add)
                CNT = k.sb("CNT", [128, 32], F32); NBK = k.sb("NBK", [128, 32], F32); NBI = k.sb("NBI", [128, 32], I32)
                PEND = k.sb("PEND", [128, 32], F32); PST = k.sb("PST", [128, 32], F32); ONE32 = k.sb("ONE32", [128, 32], F32)
                k.memset(ONE32[:], 1.0)
                k.tt(CNT[:], OFF[:, 16, :], CS[:, 16, :], ALU.add)
                k.ts(NBK[:], CNT[:], 1.0 / 256.0, 255.0 / 256.0 - 0.498046875, ALU.mult, ALU.add)
                k.cp(NBI[:], NBK[:])
                k.cp(NBK[:], NBI[:])
                k.s.add("dve", lambda h: h.tensor_tensor_scan(PEND[:], ONE32[:], NBK[:], 0.0, ALU.mult, ALU.add), [ONE32, NBK], [PEND])
                k.tt(PST[:], PEND[:], NBK[:], ALU.subtract)
                SLT = k.sb("SLT", [128, 17, 32], F32)
                k.tt(SLT[:], OFF[:], RK[:], ALU.add)
                k.ts(PST[:], PST[:], 256.0, None, ALU.mult)
                k.tt(SLT[:], SLT[:], PST[:].unsqueeze(1).to_broadcast([128, 17, 32]), ALU.add)
                SL = [k.sb("SL%d" % i, [128, 17], F32) for i in range(2)]
                for i_, Ax in enumerate((A1s, A2s)):
                    k.tt(Asum[:], Ax[:], SLT[:], ALU.mult)
                    k.s.add("dve", (lambda o_, i2: (lambda h: h.tensor_reduce(o_, i2, AX.X, ALU.add)))(SL[i_][:], Asum[:]), [Asum], [SL[i_]])
                    k.cp(SLi[i_][:], SL[i_][:])
                BIX = k.sb("BIX", [128, NB, 32], F32)
                k.dma(BIX[:], bidx_d)
                k.tt(BIX[:], PEND[:].unsqueeze(1).to_broadcast([128, NB, 32]), BIX[:], ALU.is_le)
                BE = k.sb("BE", [128, NB], F32)
                k.s.add("dve", lambda h: h.tensor_reduce(BE[:], BIX[:], AX.X, ALU.add), [BIX], [BE])
                pcol = k.sb("pcol", [128, 1], F32); k.dma(pcol[:], pcol_d)
                k.ts(BE[:], BE[:], 128.0, pcol[:, 0:1], ALU.mult, ALU.add)
                k.cp(BEi[:], BE[:])
            with k.scope():
                hld = [k.sb("hld%d" % i, [128, D], BF16) for i in range(4)]
                for ti, (t0, n) in enumerate(tiles):
                    hb_ = hld[ti % 4]
                    k.dma(hb_[0:n, :], hn_scr[t0:t0 + n, :])
                    for i_ in range(2):
                        k.s.add("pool", (lambda src, idx: (lambda h: h.indirect_dma_start(
                            out=xs_scr[:, :], out_offset=bass.IndirectOffsetOnAxis(ap=idx, axis=0), in_=src, in_offset=None)))(
                            hb_[0:n, :], SLi[i_][0:n, ti:ti + 1]), [hb_, SLi[i_]], [("xs_scr", 2 * ti + i_)], dma=True, semkey=(hb_.name, i_))
                wbe = [k.sb("wbe%d" % i, [128, 6144], BF16) for i in range(4)]
                for i in range(4):
                    k.memset(wbe[i][:], 0.0)
                xsb = [k.sb("xsb%d" % i, [128, 2, D], BF16) for i in range(3)]
                xsT = [k.sb("xsT%d" % i, [128, 8, 256], BF16) for i in range(2)]
                hmT = [k.sb("hmT%d" % i, [128, 2, 256], BF16) for i in range(2)]
                sgt = [k.sb("sgt%d" % i, [128, 256], F32) for i in range(2)]
                ybt = [k.sb("ybt%d" % i, [128, D], BF16) for i in range(2)]
                _bcc = {}

                def _bc(h):
                    if "r" not in _bcc:
                        _bcc["r"] = h.to_reg(4095)
                    return _bcc["r"]

                def mk_blk(bidx):
                    wb_ = wbe[bidx % 4]
                    Wg_ = wb_[:, 0:2048].rearrange("p (c n) -> p c n", c=8)
                    Wu_ = wb_[:, 2048:4096].rearrange("p (c n) -> p c n", c=8)
                    Wd_ = wb_[:, 4096:6144].rearrange("p (c n) -> p c n", c=2)
                    xb_, xT_, hm_ = xsb[bidx % 3], xsT[bidx % 2], hmT[bidx % 2]

                    def bl():
                        k.dma(xb_[:], xs_scr[bidx * 256:(bidx + 1) * 256, :].rearrange("(s p) d -> p s d", p=128), ins=["xs_scr"])

                    def bg():
                        k.s.add("pool", (lambda idx_: (lambda h: h.indirect_dma_start(
                            out=wb_[:, :], out_offset=None, in_=w_bf[:, :], in_offset=bass.IndirectOffsetOnAxis(ap=idx_, axis=0),
                            bounds_check=_bc(h), oob_is_err=False)))(BEi[:, bidx:bidx + 1]), [BEi, "w_bf"], [wb_], dma=True, semkey=wb_)

                    def b0():
                        for sub in range(2):
                            ptb = nbank()[:].bitcast(BF16)
                            for c in range(8):
                                k.tr(ptb[:, c * 128:(c + 1) * 128], xb_[:, sub, c * 128:(c + 1) * 128], ident[:])
                            if sub == 0:
                                k.act(xT_[:, :, 0:128], ptb.rearrange("p (c x) -> p c x", c=8), AF.Copy)
                            else:
                                k.cp(xT_[:, :, 128:256], ptb.rearrange("p (c x) -> p c x", c=8))

                    def b1():
                        for fcx in range(2):
                            pg_, pu_ = nbank(), nbank()
                            for c in range(8):
                                k.mm(pg_[:, 0:256], Wg_[:, c, fcx * 128:(fcx + 1) * 128], xT_[:, c, :], start=(c == 0), stop=(c == 7))
                            for c in range(8):
                                k.mm(pu_[:, 0:256], Wu_[:, c, fcx * 128:(fcx + 1) * 128], xT_[:, c, :], start=(c == 0), stop=(c == 7))
                            sg_ = sgt[fcx]
                            k.act(sg_[:], pg_[:, 0:256], AF.Silu)
                            k.tt(hm_[:, fcx, :], sg_[:], pu_[:, 0:256], ALU.mult)

                    def b2():
                        for sub in range(2):
                            yb_ = ybt[sub]
                            for sl_ in range(2):
                                py = nbank()
                                for fcx in range(2):
                                    k.mm(py[:, :], hm_[:, fcx, sub * 128:(sub + 1) * 128], Wd_[:, fcx, sl_ * 512:(sl_ + 1) * 512],
                                         start=(fcx == 0), stop=(fcx == 1))
                                if sub == 0:
                                    k.act(yb_[:, sl_ * 512:(sl_ + 1) * 512], py[:, :], AF.Copy)
                                else:
                                    k.cp(yb_[:, sl_ * 512:(sl_ + 1) * 512], py[:, :])
                            k.dma(ys_scr[bidx * 256 + sub * 128:bidx * 256 + (sub + 1) * 128, :], yb_[:], outs=["ys_scr"],
                                  q="act")
                    return (bl, bg, b0, b1, b2)

                blks = [mk_blk(b_) for b_ in range(NB)]
                for step in range(NB + 4):
                    for si_, sk_ in enumerate((0, 1, 2, 3, 4)):
                        if 0 <= step - sk_ < NB:
                            blks[step - sk_][si_]()
            with k.scope():
                ygt = [k.sb("ygt%d" % i, [128, D], BF16) for i in range(4)]
                hfin = [k.sb("hfin%d" % i, [128, D], F32) for i in range(2)]
                yo = [k.sb("yo%d" % i, [128, D], F32) for i in range(2)]
                junk = k.sb("junk3", [128, D], BF16)
                fs = [k.sb("fs%d" % i, [128, 1], F32) for i in range(2)]
                for ti, (t0, n) in enumerate(tiles):
                    b = ti % 2
                    k.dma(hfin[b][0:n, :], h_scr[t0:t0 + n, :])
                    for i_ in range(2):
                        yg = ygt[(2 * ti + i_) % 4]
                        k.s.add("pool", (lambda dst, idx_: (lambda h: h.indirect_dma_start(
                            out=dst, out_offset=None, in_=ys_scr[:, :], in_offset=bass.IndirectOffsetOnAxis(ap=idx_, axis=0))))(
                            yg[0:n, :], SLi[i_][0:n, ti:ti + 1]), ["ys_scr", SLi[i_]], [yg], dma=True, semkey=yg)
                        k.stt(hfin[b][0:n, :], yg[0:n, :], WW[0:n, ti, i_:i_ + 1], hfin[b][0:n, :], ALU.mult, ALU.add)
                    k.act(junk[0:n, :], hfin[b][0:n, :], AF.Square, accum_out=fs[b][0:n, :])
                    k.ts(fs[b][0:n, :], fs[b][0:n, :], 1.0 / D, 1e-6, ALU.mult, ALU.add)
                    k.act(fs[b][0:n, :], fs[b][0:n, :], AF.Sqrt)
                    k.recip(fs[b][0:n, :], fs[b][0:n, :])
                    k.stt(yo[b][0:n, :], hfin[b][0:n, :], fs[b][0:n, :], gfin[0:n, :], ALU.mult, ALU.mult)
                    k.dma(y_out[t0:t0 + n, :], yo[b][0:n, :], q="act")

        k.s.emit()
    return nc


_NC = {}


def _host_consts():
    pos = np.concatenate([np.arange(NPT), np.tile(2048 + np.arange(4), 16)]).astype(np.float32)
    half = 8
    inv = (500000.0 ** (-(np.arange(half, dtype=np.float32) / half))).astype(np.float32)
    ang = pos[:, None] * inv[None, :]
    rope = np.concatenate([np.cos(ang), np.sin(ang)], axis=1).astype(np.float32)
    diag = np.zeros((128, 32), np.float32)
    for gq in range(4):
        for c in range(16):
            diag[32 * gq + c, c] = 1.0
    NEG = -30000.0
    masks = np.zeros((128, 384), np.float32)
    kk = np.arange(128)[:, None]
    qq = np.arange(128)[None, :]
    masks[:, 0:128] = np.where(kk <= qq, 0.0, NEG)
    masks[:, 128:256] = np.where(kk >= qq, 0.0, NEG)
    k64 = np.arange(128)[:, None]
    q64 = np.arange(64)[None, :]
    same = (k64 // 4 == q64 // 4) & (k64 < 64)
    masks[:, 256:320] = np.where(same & (k64 % 4 <= q64 % 4), 0.0, NEG)
    masks[:, 320:384] = np.where(k64 == q64, 0.0, NEG)
    return rope, diag, masks


def kernel(_dbg=False, **inp):
    if _dbg not in _NC:
        _NC[_dbg] = build(_dbg)
    nc = _NC[_dbg]
    f = lambda a: np.ascontiguousarray(np.asarray(a, dtype=np.float32))
    rope, diag, masks = _host_consts()
    ident = np.eye(128, dtype=np.float32)
    w_in = f(inp["w_in"][0])
    g_attn = f(np.asarray(inp["g_attn_norm"][0]).reshape(8, 128).T)
    xp = np.asarray(inp["x_prompt"])
    xs = np.asarray(inp["x_sample"])
    s_are = f(np.asarray(inp["ssm_a_re"][0]).T)
    s_aim = f(np.asarray(inp["ssm_a_im"][0]).T)
    s_ldt = f(np.broadcast_to(np.asarray(inp["ssm_log_dt"][0])[None, :], (64, 32)))
    s_bre = f(np.transpose(np.asarray(inp["ssm_b_re"][0]), (1, 0, 2)))
    s_bim = f(np.transpose(np.asarray(inp["ssm_b_im"][0]), (1, 0, 2)))
    s_cre = f(np.transpose(np.asarray(inp["ssm_c_re"][0]), (2, 0, 1)))
    s_cim = f(np.transpose(np.asarray(inp["ssm_c_im"][0]), (2, 0, 1)))
    dd = np.asarray(inp["ssm_d"][0])
    s_d = np.zeros((128, 8), np.float32)
    for g in range(32):
        s_d[32 * (g % 4):32 * (g % 4) + 16, g // 4] = dd[g]
    st_all = np.asarray(inp["state_ssm"][0])
    common = {"rope": rope, "ident_in": ident, "diag_in": diag, "g_attn_in": g_attn, "w_in": w_in,
              "s_are": s_are, "s_aim": s_aim, "s_ldt": s_ldt, "s_bre": s_bre, "s_bim": s_bim,
              "s_cre": s_cre, "s_cim": s_cim, "s_d": s_d, "masks_in": masks}
    weg = np.asarray(inp["w_exp_gate"][0]).reshape(32, 8, 128, 256).transpose(0, 2, 1, 3).reshape(32, 128, 2048)
    weu = np.asarray(inp["w_exp_up"][0]).reshape(32, 8, 128, 256).transpose(0, 2, 1, 3).reshape(32, 128, 2048)
    wed = np.asarray(inp["w_exp_down"][0]).reshape(32, 2, 128, 1024).transpose(0, 2, 1, 3).reshape(32, 128, 2048)
    w_all = f(np.concatenate([weg, weu, wed], axis=2).reshape(4096, 6144))
    tri = (np.arange(128)[:, None] < np.arange(128)[None, :]).astype(np.float32)
    bidx = f(np.broadcast_to(np.arange(49, dtype=np.float32)[None, :, None], (128, 49, 32)))
    pcol = np.arange(128, dtype=np.float32).reshape(128, 1)
    common.update({"w_ab": f(inp["w_attn_branch"][0]), "w_glu": f(inp["w_glu"][0]), "w_out": f(inp["w_out"][0]),
                   "g_ffn_in": f(np.asarray(inp["g_ffn_norm"][0]).reshape(8, 128).T),
                   "w_rg": f(inp["w_router_group"][0]), "w_re": f(inp["w_router_expert"][0]),
                   "rbias_in": f(np.broadcast_to(np.concatenate([np.asarray(inp["b_router_group"][0]), np.asarray(inp["b_router_expert"][0])])[None, :], (128, 36))),
                   "gfin_in": f(np.broadcast_to(np.asarray(inp["g_final"])[None, :], (128, 1024))),
                   "w_all": w_all, "tri_in": tri, "bidx_in": bidx, "pcol_in": pcol,
                   "gffrow_in": f(np.broadcast_to(np.asarray(inp["g_ffn_norm"][0])[None, :], (128, 1024)))})
    c128 = np.asarray(inp["cache_kv_w128"][0])
    c512 = np.asarray(inp["cache_kv_w512"][0])
    c2048 = np.asarray(inp["cache_kv_w2048"][0])
    in_maps = []
    for c in range(8):
        xc = np.concatenate([xp[c], xs[16 * c:16 * c + 16].reshape(64, D)], axis=0)
        h0 = f(np.transpose(st_all[16 * c:16 * c + 16], (2, 3, 0, 1)))
        m = dict(common)
        sl = slice(16 * c, 16 * c + 16)
        m.update({"x": f(xc), "s_h0": h0,
                  "cache0": f(c128[sl]),
                  "cache1": f(c512[sl].reshape(16, 128, 4, 2, 8, 64)),
                  "cache2": f(c2048[sl].reshape(16, 128, 16, 2, 8, 64)[:, :, 0:4])})
        in_maps.append(m)
    res = run_bass_kernel_spmd(nc, in_maps, core_ids=list(range(8)))
    R = res.results
    if _dbg:
        kernel.dbg = R
    outs = []
    y_prompt = np.stack([R[c]["y_out"][0:NPT] for c in range(8)], axis=0)
    y_sample = np.concatenate([R[c]["y_out"][NPT:NT].reshape(16, 4, D) for c in range(8)], axis=0)
    outs += [y_prompt, y_sample]
    for g in range(3):
        outs.append(np.stack([R[c]["kvp%d" % g] for c in range(8)], axis=0)[None])
        outs.append(np.concatenate([R[c]["kvs%d" % g] for c in range(8)], axis=0)[None])
    sp = np.stack([np.transpose(R[c]["ssm_p"], (2, 0, 1)) for c in range(8)], axis=0)[None]
    ssv = np.concatenate([np.transpose(R[c]["ssm_s"], (2, 3, 0, 1)) for c in range(8)], axis=0)[None]
    outs.append(np.ascontiguousarray(sp.astype(np.float32)))
    outs.append(np.ascontiguousarray(ssv.astype(np.float32)))
    return tuple(outs)
```

```python
import numpy as np
from contextlib import ExitStack
import concourse.bass as bass
import concourse.mybir as mybir
from concourse.alu_op_type import AluOpType as ALU
from concourse.bass_utils import run_bass_kernel_spmd

F32 = mybir.dt.float32
BF16 = mybir.dt.bfloat16
I32 = mybir.dt.int32
AF = mybir.ActivationFunctionType
AX = mybir.AxisListType

NT = 2112
NPT = 2048
NS = 64
D = 1024
GROUPS = ((128, 1), (512, 4), (2048, 16))


class _Op:
    __slots__ = ("eng", "fn", "deps", "dma", "signal", "cnt", "sem", "val", "done", "isbar", "g")


class Sched:
    def __init__(self, nc, stack):
        self.nc = nc
        self.stack = stack
        self.eng = {"pe": nc.tensor, "act": nc.scalar, "dve": nc.vector, "pool": nc.gpsimd, "sp": nc.sync}
        self.ops = {e: [] for e in self.eng}
        self.state = {}
        self.dsem = {}
        self.last_dma = {}
        self.gcount = 0

    @staticmethod
    def _key(a):
        if isinstance(a, tuple):
            return a
        if isinstance(a, str):
            return (a, None)
        if hasattr(a, "tensor"):
            return (a.tensor.name, None)
        return (a.name, None)

    def _entries(self, key, create=True):
        name, sub = key
        d = self.state.setdefault(name, {})
        if create and sub not in d:
            d[sub] = [None, []]
        return [(s, e) for s, e in d.items() if s == sub or s is None or sub is None]

    def add(self, eng, fn, ins=(), outs=(), dma=False, semkey=None):
        op = _Op()
        op.eng, op.fn, op.dma, op.signal, op.deps = eng, fn, dma, False, set()
        op.cnt = op.val = 0
        op.sem = None
        for a in ins:
            if a is None:
                continue
            k = self._key(a)
            for s, e in self._entries(k):
                if e[0] is not None:
                    op.deps.add(e[0])
            self.state[k[0]][k[1]][1].append(op)
        for a in outs:
            if a is None:
                continue
            k = self._key(a)
            for s, e in self._entries(k):
                if e[0] is not None:
                    op.deps.add(e[0])
                for r in e[1]:
                    op.deps.add(r)
                if s == k[1] or k[1] is None:
                    e[0] = op
                    e[1] = []
            d = self.state[k[0]]
            for s in list(d.keys()):
                if s == k[1] or k[1] is None:
                    d[s] = [op, []]
        op.deps.discard(op)
        if dma:
            sk = self._key(semkey)
            if sk not in self.dsem:
                self.dsem[sk] = [self.stack.enter_context(self.nc.semaphore("d%d" % len(self.dsem))), 0]
            ent = self.dsem[sk]
            ent[1] += 16
            op.sem, op.val = ent[0], ent[1]
            self.last_dma[sk] = op
        op.g = self.gcount
        self.gcount += 1
        self.ops[eng].append(op)
        return op

    def interleave(self, g0, g1, g2):
        la, lb = g1 - g0, g2 - g1
        if la == 0 or lb == 0:
            return

        def pos(op):
            if op.g < g1:
                return (op.g - g0) * (la + lb) / la
            return (op.g - g1) * (la + lb) / lb + 0.5
        for e, lst in self.ops.items():
            head = [o for o in lst if getattr(o, "isbar", False) or o.g < g0]
            tail = [o for o in lst if not getattr(o, "isbar", False) and o.g >= g0]
            tail.sort(key=pos)
            self.ops[e] = head + tail

    def barrier(self, full=True):
        deps = set()
        for e, lst in self.ops.items():
            for op in reversed(lst):
                if not op.dma and not getattr(op, "isbar", False):
                    deps.add(op)
                    break
        bg = getattr(self, "bg_keys", set())
        deps |= set(op for sk, op in self.last_dma.items() if full or sk not in bg)
        for e in self.ops:
            op = _Op()
            op.eng, op.fn, op.dma, op.signal, op.deps = e, None, False, False, set(deps)
            op.cnt = op.val = 0
            op.sem = None
            op.isbar = True
            op.g = self.gcount
            self.ops[e].append(op)
        self.emit(final=False)

    def emit(self, final=True):
        nc = self.nc
        if not hasattr(self, "esem"):
            self.esem = {e: self.stack.enter_context(nc.semaphore("e_" + e)) for e in self.eng}
            self.ecount = {e: 0 for e in self.eng}
        esem = self.esem
        for e, lst in self.ops.items():
            for op in lst:
                for d in op.deps:
                    if d.dma or getattr(d, "done", False):
                        continue
                    if d.eng == "pe" and op.eng == "pe" and not op.dma:
                        continue
                    d.signal = True
        for e, lst in self.ops.items():
            c = self.ecount[e]
            for op in lst:
                if not op.dma and op.signal:
                    c += 1
                    op.sem, op.val = esem[e], c
            self.ecount[e] = c
        sched = self
        if not hasattr(self, "waited"):
            self.waited = {e: {} for e in self.eng}

        def run(e, h):
            waited = sched.waited[e]
            for op in sched.ops[e]:
                need = {}
                for d in op.deps:
                    if not d.dma and d.eng == "pe" and e == "pe" and not op.dma:
                        continue
                    if d.sem is None:
                        continue
                    sid = id(d.sem)
                    if sid not in need or need[sid][1] < d.val:
                        need[sid] = (d.sem, d.val)
                for sid, (s, v) in need.items():
                    if waited.get(sid, 0) < v:
                        h.wait_ge(s, v)
                        waited[sid] = v
                if op.fn is None:
                    continue
                ins = op.fn(h)
                if op.dma:
                    ins.then_inc(op.sem, 16)
                elif op.signal:
                    ins.then_inc(op.sem, 1)
            if e == "sp" and final:
                for sk, (s, tot) in sched.dsem.items():
                    if tot > 0:
                        h.wait_ge(s, tot)

        with nc.Block() as block:
            @block.tensor
            def _(h):
                run("pe", h)

            @block.scalar
            def _(h):
                run("act", h)

            @block.vector
            def _(h):
                run("dve", h)

            @block.gpsimd
            def _(h):
                run("pool", h)

            @block.sync
            def _(h):
                run("sp", h)
        for e in self.ops:
            for op in self.ops[e]:
                op.done = True
                op.fn = None
            self.ops[e] = []


class _Scope:
    def __init__(self, k, full=False):
        self.k = k
        self.full = full

    def __enter__(self):
        self.old = self.k.st
        self.es = ExitStack()
        self.es.__enter__()
        self.k.st = self.es
        return self

    def __exit__(self, *a):
        self.k.s.barrier(full=self.full)
        self.k.st = self.old
        return self.es.__exit__(*a)


def _is_sb(ap):
    return type(ap.tensor).__name__.startswith("SB")


class K:
    def __init__(self, nc, stack):
        self.nc = nc
        self.st = stack
        self.s = Sched(nc, stack)
        self.nps = 0

    def scope(self, full=False):
        return _Scope(self, full)

    def sb(self, name, shape, dt):
        return self.st.enter_context(self.nc.sbuf_tensor(name, list(shape), dt))

    def ps(self, name, shape, dt=F32):
        return self.st.enter_context(self.nc.psum_tensor(name, list(shape), dt))

    def dma(self, out, in_, q="sp", ins=None, outs=None, **kw):
        semkey = out if _is_sb(out) else in_
        if outs is not None and _is_sb(out):
            semkey = outs[0]
        elif ins is not None and not _is_sb(out):
            semkey = ins[0]
        return self.s.add(q, lambda h: h.dma_start(out=out, in_=in_, **kw),
                          ins if ins is not None else [in_], outs if outs is not None else [out],
                          dma=True, semkey=semkey)

    def mm(self, out, lhsT, rhs, start=True, stop=True, ins=None, outs=None, **kw):
        return self.s.add("pe", lambda h: h.matmul(out, lhsT, rhs, start=start, stop=stop, **kw),
                          ins if ins is not None else [lhsT, rhs], outs if outs is not None else [out])

    def tr(self, out, in_, ident, ins=None, outs=None):
        return self.s.add("pe", lambda h: h.transpose(out, in_, ident),
                          ins if ins is not None else [in_, ident], outs if outs is not None else [out])

    def act(self, out, in_, func, bias=None, scale=None, accum_out=None, ins=None, outs=None):
        kw = {}
        if bias is not None:
            kw["bias"] = bias
        if scale is not None:
            kw["scale"] = scale
        if accum_out is not None:
            kw["accum_out"] = accum_out
        i = [in_]
        for x in (bias, scale):
            if x is not None and not isinstance(x, (int, float)):
                i.append(x)
        o = [out] + ([accum_out] if accum_out is not None else [])
        return self.s.add("act", lambda h: h.activation(out, in_, func, **kw),
                          ins if ins is not None else i, outs if outs is not None else o)

    def tt(self, out, in0, in1, op, eng="dve", ins=None, outs=None):
        return self.s.add(eng, lambda h: h.tensor_tensor(out, in0, in1, op),
                          ins if ins is not None else [in0, in1], outs if outs is not None else [out])

    def ts(self, out, in0, s1, s2=None, op0=ALU.mult, op1=None, eng="dve", ins=None, outs=None):
        i = [in0] + [x for x in (s1, s2) if x is not None and not isinstance(x, (int, float))]
        if op1 is None:
            fn = lambda h: h.tensor_scalar(out, in0, s1, None, op0)
        else:
            fn = lambda h: h.tensor_scalar(out, in0, s1, s2, op0, op1)
        return self.s.add(eng, fn, ins if ins is not None else i, outs if outs is not None else [out])

    def stt(self, out, in0, scalar, in1, op0, op1, ins=None, outs=None):
        i = [in0, in1] + ([scalar] if not isinstance(scalar, (int, float)) else [])
        return self.s.add("dve", lambda h: h.scalar_tensor_tensor(out, in0, scalar, in1, op0, op1),
                          ins if ins is not None else i, outs if outs is not None else [out])

    def cp(self, out, in_, eng="dve", ins=None, outs=None):
        return self.s.add(eng, lambda h: h.tensor_copy(out, in_),
                          ins if ins is not None else [in_], outs if outs is not None else [out])

    def memset(self, ap, v, eng="dve"):
        return self.s.add(eng, lambda h: h.memset(ap, v), [], [ap])

    def recip(self, out, in_):
        return self.s.add("dve", lambda h: h.reciprocal(out, in_), [in_], [out])


TWO_PI = 6.283185
PW_SLOTS = list(range(9)) + [-4]


def build(dbg=False):
    nc = bass.Bass("TRN2", target_bir_lowering=False)
    dr = {}

    def din(name, shape, dt=F32):
        dr[name] = nc.dram_tensor(name, list(shape), dt, kind="ExternalInput").ap()
        return dr[name]

    def dout(name, shape, dt=F32):
        dr[name] = nc.dram_tensor(name, list(shape), dt, kind="ExternalOutput").ap()
        return dr[name]

    x = din("x", [NT, D])
    rope = din("rope", [NT, 16])
    ident_d = din("ident_in", [128, 128])
    diag_d = din("diag_in", [128, 32])
    g_attn = din("g_attn_in", [128, 8])
    w_in = din("w_in", [D, 7168])
    s_are = din("s_are", [64, 32]); s_aim = din("s_aim", [64, 32]); s_ldt = din("s_ldt", [64, 32])
    s_bre = din("s_bre", [64, 32, 16]); s_bim = din("s_bim", [64, 32, 16])
    s_cre = din("s_cre", [64, 32, 16]); s_cim = din("s_cim", [64, 32, 16])
    s_d = din("s_d", [128, 8])
    s_h0 = din("s_h0", [64, 2, 16, 32])
    masks_d = din("masks_in", [128, 384])
    w_ab = din("w_ab", [512, 1024]); w_glu = din("w_glu", [512, 2048]); w_out = din("w_out", [1024, 1024])
    g_ffn = din("g_ffn_in", [128, 8])
    w_rg = din("w_rg", [1024, 4]); w_re = din("w_re", [1024, 32]); rb_d = din("rbias_in", [128, 36])
    gfin_d = din("gfin_in", [128, 1024])
    w_all = din("w_all", [4096, 6144])
    tri_d = din("tri_in", [128, 128]); bidx_d = din("bidx_in", [128, 49, 32]); pcol_d = din("pcol_in", [128, 1])
    gffrow_d = din("gffrow_in", [128, 1024])
    hn_scr = nc.dram_tensor("hn_scr", [NT, D], BF16, kind="Internal").ap()
    rt_scr = nc.dram_tensor("rt_scr", [NT, 66], F32, kind="Internal").ap()
    sc_LAMr = nc.dram_tensor("sc_LAMr", [64, 10, 32], F32, kind="Internal").ap()
    sc_LAMi = nc.dram_tensor("sc_LAMi", [64, 10, 32], F32, kind="Internal").ap()
    sc_OutW = nc.dram_tensor("sc_OutW", [128, 8, 8, 4, 32], BF16, kind="Internal").ap()
    sc_Cm = nc.dram_tensor("sc_Cm", [128, 8, 4, 32], BF16, kind="Internal").ap()
    sc_KW = nc.dram_tensor("sc_KW", [128, 8, 8, 32], BF16, kind="Internal").ap()
    sc_SWc = nc.dram_tensor("sc_SWc", [128, 8, 8, 128], BF16, kind="Internal").ap()
    w_bf = nc.dram_tensor("w_bf", [4096, 6144], BF16, kind="Internal").ap()
    xs_scr = nc.dram_tensor("xs_scr", [49 * 256, D], BF16, kind="Internal").ap()
    ys_scr = nc.dram_tensor("ys_scr", [49 * 256, D], BF16, kind="Internal").ap()
    h_scr = nc.dram_tensor("h_scr", [NT, D], F32, kind="Internal").ap()
    y_out = dout("y_out", [NT, D])
    caches = [din("cache0", [16, 128, 2, 8, 64]), din("cache1", [16, 128, 4, 2, 8, 64]), din("cache2", [16, 128, 4, 2, 8, 64])]
    kvp = [dout("kvp0", [128, 2, 8, 64]), dout("kvp1", [512, 2, 8, 64]), dout("kvp2", [2048, 2, 8, 64])]
    kvs = [dout("kvs%d" % g, [16, 4, 2, 8, 64]) for g in range(3)]
    ssm_p = dout("ssm_p", [64, 2, 32])
    ssm_s = dout("ssm_s", [64, 2, 16, 32])
    if dbg:
        dbg_gy = dout("dbg_gy", [128, 8, NT], BF16)
        dbg_at = dout("dbg_at", [128, 4, NT], BF16)

    with ExitStack() as st:
        k = K(nc, st)
        ident = k.sb("ident", [128, 128], BF16)
        with k.scope():
            ident_f = k.sb("ident_f", [128, 128], F32)
            k.dma(ident_f[:], ident_d)
            k.cp(ident[:], ident_f[:])
        diag32 = k.sb("diag32", [128, 32], F32)
        k.dma(diag32[:], diag_d)
        gat = k.sb("gat", [128, 8], F32)
        k.dma(gat[:], g_attn)
        xnT = k.sb("xnT", [128, 8, NT], BF16)
        psb = [k.ps("psb%d" % i, [128, 512], F32) for i in range(8)]
        w_in_v = w_in.rearrange("(c p) n -> p c n", p=128)
        cvt = []
        cvs = {"e": 0}

        def conv_step(dep=None):
            e = cvs["e"]
            if e < 32:
                k.dma(cvt[e % 2][:].rearrange("p (a x) -> p a x", x=2048),
                      w_all[e * 128:(e + 1) * 128, :].rearrange("p (a x) -> p a x", x=2048), q="pool",
                      ins=([dep] if dep is not None else []))
            if 1 <= e <= 32:
                k.dma(w_bf[(e - 1) * 128:e * 128, :], cvt[(e - 1) % 2][:], q="pool", outs=[("w_bf", e)])
            cvs["e"] = e + 1

        tiles = [(i * 128, 128) for i in range(16)] + [(NPT, NS)]
        with k.scope():
            xt = [k.sb("xt%d" % i, [128, D], F32) for i in range(2)]
            xb = [k.sb("xb%d" % i, [128, D], BF16) for i in range(2)]
            junk = k.sb("junk", [128, D], BF16)
            ss = [k.sb("ss%d" % i, [128, 1], F32) for i in range(2)]
            rs = [k.sb("rs%d" % i, [128, 1], F32) for i in range(2)]
            _g0 = k.s.gcount
            LAMr = k.sb("LAMr", [64, 10, 32], F32); LAMi = k.sb("LAMi", [64, 10, 32], F32)
            OutW = k.sb("OutW", [128, 8, 8, 4, 32], BF16)
            Cm = k.sb("Cm", [128, 8, 4, 32], BF16)
            KW = k.sb("KW", [128, 8, 8, 32], BF16)
            SWc = k.sb("SWc", [128, 8, 8, 128], BF16)
            def T(name, shape, dt=F32):
                return k.sb(name, shape, dt)

            a_re = T("a_re", [64, 32]); a_im = T("a_im", [64, 32]); ldt = T("ldt", [64, 32])
            bre = T("bre", [64, 32, 16]); bim = T("bim", [64, 32, 16])
            cre = T("cre", [64, 32, 16]); cim = T("cim", [64, 32, 16])
            for t_, d_ in ((a_re, s_are), (a_im, s_aim), (ldt, s_ldt), (bre, s_bre), (bim, s_bim), (cre, s_cre), (cim, s_cim)):
                k.dma(t_[:], d_)
            dpad = T("dpad", [128, 8]); k.dma(dpad[:], s_d)
            lr = T("lr", [64, 32]); li = T("li", [64, 32])
            k.act(ldt[:], ldt[:], AF.Exp)
            k.tt(lr[:], a_re[:], ldt[:], ALU.mult)
            k.tt(li[:], a_im[:], ldt[:], ALU.mult)
            pass
            mag = T("mag", [64, 32]); yv = T("yv", [64, 32]); yi = T("yi", [64, 32], I32); yf = T("yf", [64, 32])
            fr = T("fr", [64, 32]); fc = T("fc", [64, 32]); msk = T("msk", [64, 32]); sn = T("sn", [64, 32]); cs = T("cs", [64, 32])
            for slot, tau in enumerate(PW_SLOTS):
                k.act(mag[:], lr[:], AF.Exp, scale=float(tau))
                k.ts(yv[:], li[:], float(tau) / (2 * np.pi), None, ALU.mult)
                k.cp(yi[:], yv[:])
                k.cp(yf[:], yi[:])
                k.tt(fr[:], yv[:], yf[:], ALU.subtract)
                k.ts(fc[:], fr[:], 0.25, None, ALU.add)
                k.ts(msk[:], fc[:], 0.5, None, ALU.is_gt)
                k.tt(fc[:], fc[:], msk[:], ALU.subtract)
                k.act(sn[:], fr[:], AF.Sin, scale=TWO_PI)
                k.act(cs[:], fc[:], AF.Sin, scale=TWO_PI)
                k.tt(LAMr[:, slot, :], mag[:], cs[:], ALU.mult)
                k.tt(LAMi[:, slot, :], mag[:], sn[:], ALU.mult)
            nre = T("nre", [64, 32]); den = T("den", [64, 32]); t0_ = T("t0_", [64, 32]); t1_ = T("t1_", [64, 32])
            fre = T("fre", [64, 32]); fim = T("fim", [64, 32])
            k.ts(nre[:], LAMr[:, 1, :], -1.0, None, ALU.add)
            k.tt(den[:], a_re[:], a_re[:], ALU.mult)
            k.tt(t0_[:], a_im[:], a_im[:], ALU.mult)
            k.tt(den[:], den[:], t0_[:], ALU.add)
            k.recip(den[:], den[:])
            k.tt(t0_[:], nre[:], a_re[:], ALU.mult)
            k.tt(t1_[:], LAMi[:, 1, :], a_im[:], ALU.mult)
            k.tt(t0_[:], t0_[:], t1_[:], ALU.add)
            k.tt(fre[:], t0_[:], den[:], ALU.mult)
            k.tt(t0_[:], LAMi[:, 1, :], a_re[:], ALU.mult)
            k.tt(t1_[:], nre[:], a_im[:], ALU.mult)
            k.tt(t0_[:], t0_[:], t1_[:], ALU.subtract)
            k.tt(fim[:], t0_[:], den[:], ALU.mult)
            bbr = T("bbr", [64, 32, 16]); bbi = T("bbi", [64, 32, 16])
            u1 = T("u1", [64, 32, 16]); u2 = T("u2", [64, 32, 16])

            def bc(ap2):
                return ap2.unsqueeze(2).to_broadcast([64, 32, 16])

            k.tt(u1[:], bre[:], bc(fre[:]), ALU.mult)
            k.tt(u2[:], bim[:], bc(fim[:]), ALU.mult)
            k.tt(bbr[:], u1[:], u2[:], ALU.subtract)
            k.tt(u1[:], bim[:], bc(fre[:]), ALU.mult)
            k.tt(u2[:], bre[:], bc(fim[:]), ALU.mult)
            k.tt(bbi[:], u1[:], u2[:], ALU.add)
            ZB = T("ZB", [128, 8, 8, 4, 32], BF16)
            pass
            pass
            k.memset(ZB[:], 0.0)
            k.memset(OutW[:], 0.0)
            k.memset(Cm[:], 0.0)

            def v4(ap3):
                return ap3.rearrange("p (c q) x -> p c q x", q=4)

            for tau in range(8):
                lrb, lib = bc(LAMr[:, tau, :]), bc(LAMi[:, tau, :])
                k.tt(u1[:], bbr[:], lrb, ALU.mult)
                k.tt(u2[:], bbi[:], lib, ALU.mult)
                k.tt(ZB[0:64, :, tau, :, 0:16], v4(u1[:]), v4(u2[:]), ALU.subtract, outs=[("ZB", tau)])
                k.tt(u1[:], bbi[:], lrb, ALU.mult)
                k.tt(u2[:], bbr[:], lib, ALU.mult)
                k.tt(ZB[64:128, :, tau, :, 0:16], v4(u1[:]), v4(u2[:]), ALU.add, outs=[("ZB", tau)])
            for j in range(8):
                lrb, lib = bc(LAMr[:, j + 1, :]), bc(LAMi[:, j + 1, :])
                k.tt(u1[:], cre[:], lrb, ALU.mult)
                k.tt(u2[:], cim[:], lib, ALU.mult)
                k.tt(OutW[0:64, :, j, :, 0:16], v4(u1[:]), v4(u2[:]), ALU.subtract, outs=[("OutW", j)])
                k.tt(u1[:], cre[:], lib, ALU.mult)
                k.tt(u2[:], cim[:], lrb, ALU.mult)
                k.tt(u1[:], u1[:], u2[:], ALU.add)
                k.ts(OutW[64:128, :, j, :, 0:16], v4(u1[:]), -1.0, None, ALU.mult, outs=[("OutW", j)])
            k.cp(Cm[0:64, :, :, 0:16], v4(cre[:]))
            k.ts(Cm[64:128, :, :, 0:16], v4(cim[:]), -1.0, None, ALU.mult)
            KWf = T("KWf", [128, 8, 8, 32], F32)
            pass
            for chunk in range(8):
                for tau in range(8):
                    slot = chunk * 8 + tau
                    bank = psb[2 + slot // 16]
                    c0 = (slot % 16) * 32
                    for gq in range(4):
                        k.mm(bank[32 * gq:32 * gq + 32, c0:c0 + 32], ZB[:, chunk, tau, gq, :], Cm[:, chunk, gq, :],
                             tile_position=(0, 32 * gq))
            for b4 in range(4):
                k.cp(KWf[:, 2 * b4:2 * b4 + 2, :, :], psb[2 + b4][:].rearrange("p (a t x) -> p a t x", a=2, t=8), eng="dve")
            for chunk in range(8):
                k.stt(KWf[:, chunk, 0, :], diag32[:], dpad[:, chunk:chunk + 1], KWf[:, chunk, 0, :], ALU.mult, ALU.add)
            k.cp(KW[:], KWf[:])
            pass
            for chunk in range(8):
                bankb = psb[4 + chunk % 4][:].bitcast(BF16)
                for i in range(8):
                    k.tr(bankb[:, i * 128:(i + 1) * 128], ZB[:, chunk, 7 - i, :, :].rearrange("p q x -> p (q x)"), ident[:])
                k.cp(SWc[:, chunk, :, :], bankb.rearrange("p (i x) -> p i x", i=8), eng=("dve" if chunk % 2 else "act") if False else "dve")

            for t_, d_ in ((LAMr, sc_LAMr), (LAMi, sc_LAMi), (OutW, sc_OutW), (Cm, sc_Cm), (KW, sc_KW), (SWc, sc_SWc)):
                k.s.add("pool", (lambda o_, i_: (lambda h: h.dma_start(out=o_, in_=i_)))(d_, t_[:]), [t_], [d_], dma=True, semkey="spill_st")
            _g1 = k.s.gcount
            for ti, (t0, n) in enumerate(tiles):
                b = ti % 2
                k.dma(xt[b][0:n, :], x[t0:t0 + n, :])
                k.act(junk[0:n, :], xt[b][0:n, :], AF.Square, accum_out=ss[b][0:n, :])
                k.ts(rs[b][0:n, :], ss[b][0:n, :], 1.0 / D, 1e-6, ALU.mult, ALU.add)
                k.act(rs[b][0:n, :], rs[b][0:n, :], AF.Sqrt)
                k.recip(rs[b][0:n, :], rs[b][0:n, :])
                k.act(xb[b][0:n, :], xt[b][0:n, :], AF.Copy, scale=rs[b][0:n, :])
                pt = psb[ti % 2]
                ptb = pt[:].bitcast(BF16)
                for c in range(8):
                    k.tr(ptb[:, c * 128:c * 128 + n], xb[b][0:n, c * 128:(c + 1) * 128], ident[0:n, 0:n])
                for c in range(8):
                    if c % 2 == 0:
                        k.act(xnT[:, c, t0:t0 + n], ptb[:, c * 128:c * 128 + n], AF.Copy, scale=gat[:, c:c + 1],
                              outs=[("xnT", ti)])
                    else:
                        k.ts(xnT[:, c, t0:t0 + n], ptb[:, c * 128:c * 128 + n], gat[:, c:c + 1], None, ALU.mult,
                             outs=[("xnT", ti)])

            k.s.interleave(_g0, _g1, k.s.gcount)
        with k.scope(full=True):
            attnT = k.sb("attnT", [128, 4, NT], BF16)
            with k.scope():
                wst = [k.sb("wst%d" % i, [128, 8, 256], F32) for i in range(3)]
                wbf = [k.sb("wbf0", [128, 8, 768], BF16)]
                qkf = [k.sb("qkf%d" % i, [128, 512], F32) for i in range(2)]
                vf = [k.sb("vf%d" % i, [128, 256], F32) for i in range(2)]
                qb = [k.sb("qb%d" % i, [128, 256], BF16) for i in range(2)]
                kb = [k.sb("kb%d" % i, [128, 256], BF16) for i in range(2)]
                rp = [k.sb("rp%d" % i, [128, 16], F32) for i in range(2)]
                tmp = [k.sb("rtmp%d" % i, [128, 8, 8], F32) for i in range(4)]
                mstage = k.sb("mstage", [128, 384], F32)
                maskb = k.sb("maskb", [128, 256], BF16)
                msamp = k.sb("msamp", [128, 2, 64], BF16)
                k.dma(mstage[:], masks_d)
                k.cp(maskb[:], mstage[:, 0:256])
                k.cp(msamp[:], mstage[:, 256:384].rearrange("p (a x) -> p a x", a=2))
                ones_f = k.sb("ones_f", [128, 64], F32)
                k.memset(ones_f[:], 1.0)
                QTz = k.sb("QTz", [128, 4, NT], BF16)
                KT = k.sb("KT", [128, 2, NT], BF16)
                Va = k.sb("Va", [128, 17, 4, 65], BF16)
                acc = k.sb("acc", [65, 4, NT], F32)
                PT = [k.sb("PT%d" % i, [128, 256], BF16) for i in range(4)]
                PTs4 = [k.sb("PTs%d" % i, [128, 64], BF16) for i in range(4)]
                cst = [k.sb("cst%d" % i, [128, 4, 2, 256], F32) for i in range(2)]
                kcb = [k.sb("kcb%d" % i, [128, 256], BF16) for i in range(4)]
                KcT = [k.sb("KcT%d" % i, [128, 2, 128], BF16) for i in range(4)]
                Vc = [k.sb("Vc%d" % i, [128, 4, 65], BF16) for i in range(4)]
                PTc = [k.sb("PTc%d" % i, [128, 4, 4], BF16) for i in range(4)]
                k.memset(QTz[:], 0.0)
                k.memset(Va[:], 1.0)
                for i in range(4):
                    k.memset(PTs4[i][:], 0.0, eng="pool")
                for i in range(4):
                    k.memset(Vc[i][:], 1.0, eng="pool")

                def pipeline(iters, skews):
                    n_ = len(iters)
                    for step in range(n_ + max(skews)):
                        for si, sk in enumerate(skews):
                            i_ = step - sk
                            if 0 <= i_ < n_:
                                iters[i_][si]()

                cnt = {"it": 0, "ai": 0, "ci": 0}
                for hh in range(2):
                    k.memset(acc[:], 0.0)
                    for g, (win, dil) in enumerate(GROUPS):
                        wb = wbf[0]
                        for j in range(3):
                            c0 = j * 1536 + g * 512 + hh * 256
                            k.dma(wst[j][:], w_in_v[:, :, c0:c0 + 256])
                            if j == 1:
                                k.act(wb[:, :, j * 256:(j + 1) * 256], wst[j][:], AF.Copy, outs=[(wb.name, j)])
                            else:
                                k.cp(wb[:, :, j * 256:(j + 1) * 256], wst[j][:], outs=[(wb.name, j)])
                        nblk = (NPT // dil) // 128
                        blocks = []
                        for r in range(dil):
                            for i in range(nblk):
                                blocks.append((r + dil * 128 * i, dil, 128, r, i))
                        blocks.append((NPT, 1, NS, None, None))

                        def mk_block(bi, tstart, tstep, n, r, i, hh=hh, g=g, win=win, dil=dil, wb=wb):
                            b = cnt["it"] % 2
                            cnt["it"] += 1
                            pq, pv = psb[2 + 2 * b], psb[3 + 2 * b]
                            tok = slice(tstart, tstart + tstep * (n - 1) + 1, tstep)
                            ptb = psb[6 + b][:].bitcast(BF16)
                            pc = slice(bi * 128, bi * 128 + n)

                            def s1():
                                for c in range(8):
                                    k.mm(pq[0:n, :], xnT[:, c, tok], wb[:, c, 0:512], start=(c == 0), stop=(c == 7),
                                         ins=["xnT", wb])
                                for c in range(8):
                                    k.mm(pv[0:n, 0:256], xnT[:, c, tok], wb[:, c, 512:768], start=(c == 0), stop=(c == 7),
                                         ins=["xnT", wb])
                                k.dma(rp[b][0:n, :], rope[tok, :])

                            def s2():
                                k.act(qkf[b][0:n, :], pq[0:n, :], AF.Copy)
                                k.act(vf[b][0:n, :], pv[0:n, 0:256], AF.Copy)
                                q3 = qkf[b][0:n, :].rearrange("p (h d) -> p h d", d=64)
                                x1, x2 = q3[:, :, 0:8], q3[:, :, 8:16]
                                cosb = rp[b][0:n, 0:8].unsqueeze(1).to_broadcast([n, 8, 8])
                                sinb = rp[b][0:n, 8:16].unsqueeze(1).to_broadcast([n, 8, 8])
                                t1, t2, t3, t4 = (t[0:n] for t in tmp)
                                k.tt(t1, x1, cosb, ALU.mult)
                                k.tt(t2, x2, sinb, ALU.mult)
                                k.tt(t3, x2, cosb, ALU.mult)
                                k.tt(t4, x1, sinb, ALU.mult)
                                k.tt(x1, t1, t2, ALU.subtract, outs=[qkf[b]])
                                k.tt(x2, t3, t4, ALU.add, outs=[qkf[b]])
                                kpart = qkf[b][0:n, 256:512].rearrange("p (h d) -> p h d", d=64)
                                vpart = vf[b][0:n, :].rearrange("p (h d) -> p h d", d=64)
                                hs = slice(hh * 4, hh * 4 + 4)
                                if r is None:
                                    dk = kvs[g][:, :, 0, hs, :].rearrange("s t h d -> (s t) h d")
                                    dv = kvs[g][:, :, 1, hs, :].rearrange("s t h d -> (s t) h d")
                                    k.dma(dk, kpart, q="pool")
                                    k.dma(dv, vpart, q="pool")
                                else:
                                    first = NPT - min(win, NPT)
                                    if tstart >= first:
                                        rows = slice(tstart - first, tstart - first + dil * 127 + 1, dil)
                                        k.dma(kvp[g][rows, 0, hs, :], kpart, q="pool")
                                        k.dma(kvp[g][rows, 1, hs, :], vpart, q="pool")
                                k.ts(qb[b][0:n, :], qkf[b][0:n, 0:256], 0.125, None, ALU.mult)
                                k.cp(kb[b][0:n, :], qkf[b][0:n, 256:512])
                                k.act(Va[0:n, bi, :, 0:64], vpart, AF.Copy, outs=[("Va", bi)])
                                for pr in range(2):
                                    k.tr(ptb[:, pr * 128:pr * 128 + n], qb[b][0:n, pr * 128:(pr + 1) * 128], ident[0:n, 0:n])
                                    k.tr(ptb[:, 256 + pr * 128:256 + pr * 128 + n], kb[b][0:n, pr * 128:(pr + 1) * 128],
                                         ident[0:n, 0:n])

                            def s3():
                                for pr in range(2):
                                    k.act(QTz[0:64, 2 * pr, pc], ptb[0:64, pr * 128:pr * 128 + n], AF.Copy, outs=[("QTz", bi)])
                                    k.cp(QTz[64:128, 2 * pr + 1, pc], ptb[64:128, pr * 128:pr * 128 + n], outs=[("QTz", bi)])
                                    if pr == 0:
                                        k.act(KT[:, pr, pc], ptb[:, 256 + pr * 128:256 + pr * 128 + n], AF.Copy, outs=[("KT", bi)])
                                    else:
                                        k.cp(KT[:, pr, pc], ptb[:, 256 + pr * 128:256 + pr * 128 + n], outs=[("KT", bi)])
                            return (s1, s2, s3)

                        pipeline([mk_block(bi, *blk) for bi, blk in enumerate(blocks)], (0, 1, 2))

                        def mk_att(h, r, i, g=g, dil=dil, nblk=nblk):
                            a = cnt["ai"] % 4
                            cnt["ai"] += 1
                            ps_s, ps_o = psb[a], psb[4 + a]
                            if r is None:
                                PTs = PTs4[a]
                                def a1():
                                    k.mm(ps_s[0:64, 0:64], KT[:, h // 2, NPT:NT], QTz[:, h, NPT:NT], start=True, stop=False,
                                         ins=["KT", "QTz"])
                                    k.mm(ps_s[0:64, 0:64], ident[:, 0:64], msamp[:, 0 if g == 0 else 1, :], start=False, stop=True)
                                    k.act(PTs[0:64, :], ps_s[0:64, 0:64], AF.Exp)

                                def a2():
                                    k.mm(ps_o[0:65, 0:64], Va[:, 16, h, :], PTs[:, :], ins=["Va", PTs])
                                    av = acc[0:65, h, NPT:NT]
                                    k.tt(av, av, ps_o[0:65, 0:64], ALU.add, ins=[acc, ps_o], outs=[acc])
                                return (a1, a2)
                            bi = r * nblk + i
                            nq = 2 if i + 1 < nblk else 1
                            kc = slice(bi * 128, bi * 128 + 128)
                            qc = slice(bi * 128, bi * 128 + 128 * nq)
                            N = 128 * nq

                            def a1():
                                k.mm(ps_s[:, 0:N], KT[:, h // 2, kc], QTz[:, h, qc], start=True, stop=False,
                                     ins=["KT", "QTz"])
                                k.mm(ps_s[:, 0:N], ident[:], maskb[:, 0:N], start=False, stop=True)
                                k.act(PT[a][:, 0:N], ps_s[:, 0:N], AF.Exp)

                            def a2():
                                k.mm(ps_o[0:65, 0:N], Va[:, bi, h, :], PT[a][:, 0:N], ins=["Va", PT[a]])
                                t0n = r + dil * 128 * i
                                av = acc[0:65, h, t0n:t0n + dil * (N - 1) + 1:dil]
                                k.tt(av, av, ps_o[0:65, 0:N], ALU.add, ins=[acc, ps_o], outs=[acc])
                            return (a1, a2)

                        its = [mk_att(h, r, i) for h in range(4) for r in range(dil) for i in range(nblk)]
                        its += [mk_att(h, None, None) for h in range(4)]
                        pipeline(its, (0, 3))

                        def mk_cache(s_, t_, nt_, nq, cb, g=g, hh=hh):
                            e = cnt["ci"] % 4
                            cnt["ci"] += 1
                            a = cnt["ai"] % 2
                            cnt["ai"] += 1
                            ps_s, ps_o = psb[a], psb[2 + a]
                            ptb = psb[6 + e % 2][:].bitcast(BF16)
                            q0 = NPT + 4 * s_ + (t_ if g > 0 else 0)

                            def c0():
                                if t_ == 0:
                                    if g == 0:
                                        k.dma(cst[cb][:, 0, :, :], caches[0][s_, :, :, hh * 4:hh * 4 + 4, :].rearrange("m a h d -> m a (h d)"))
                                    else:
                                        k.dma(cst[cb][:], caches[g][s_, :, :, :, hh * 4:hh * 4 + 4, :].rearrange("m t a h d -> m t a (h d)"))
                                k.cp(kcb[e][:], cst[cb][:, t_, 0, :], eng="pool")
                                k.cp(Vc[e][:, :, 0:64], cst[cb][:, t_, 1, :].rearrange("p (h d) -> p h d", d=64))
                                for pr in range(2):
                                    k.tr(ptb[:, pr * 128:(pr + 1) * 128], kcb[e][:, pr * 128:(pr + 1) * 128], ident[:])

                            def c1():
                                k.act(KcT[e][:], ptb[:, 0:256].rearrange("p (a x) -> p a x", a=2), AF.Copy)
                                for h in range(4):
                                    k.mm(ps_s[:, h * 4:h * 4 + nq], KcT[e][:, h // 2, :], QTz[:, h, q0:q0 + nq],
                                         start=True, stop=(g > 0), ins=[KcT[e], "QTz"])
                                    if g == 0:
                                        k.mm(ps_s[:, h * 4:h * 4 + nq], ident[:], maskb[:, 128:128 + nq], start=False, stop=True)
                                k.act(PTc[e][:, :, 0:nq], ps_s[:, 0:16].rearrange("p (h q) -> p h q", q=4)[:, :, 0:nq], AF.Exp)

                            def c2():
                                for h in range(4):
                                    k.mm(ps_o[0:65, h * 4:h * 4 + nq], Vc[e][:, h, :], PTc[e][:, h, 0:nq])
                                av = acc[0:65, :, q0:q0 + nq]
                                k.tt(av, av, ps_o[0:65, 0:16].rearrange("p (h q) -> p h q", q=4)[:, :, 0:nq], ALU.add,
                                     ins=[acc, ps_o], outs=[acc])
                            return (c0, c1, c2)

                        its = []
                        for s_ in range(16):
                            nt_, nq = (1, 4) if g == 0 else (4, 1)
                            for t_ in range(nt_):
                                its.append(mk_cache(s_, t_, nt_, nq, s_ % 2))
                        pipeline(its, (0, 1, 2))

                    for h in range(4):
                        k.recip(acc[64:65, h, :], acc[64:65, h, :])
                        for (t0, n) in [(i * 512, 512) for i in range(4)] + [(NPT, NS)]:
                            a = cnt["ai"] % 2
                            cnt["ai"] += 1
                            pbc = psb[a]
                            k.mm(pbc[0:64, 0:n], ones_f[64:65, 0:64], acc[64:65, h, t0:t0 + n], tile_position=(64, 0))
                            dst = attnT[(h % 2) * 64:(h % 2) * 64 + 64, hh * 2 + h // 2, t0:t0 + n]
                            k.tt(dst, acc[0:64, h, t0:t0 + n], pbc[0:64, 0:n], ALU.mult, outs=[attnT])
                if dbg:
                    k.dma(dbg_at, attnT[:], q="pool")
            uT = k.sb("uT", [128, 8, NT], BF16)
            cvt.extend([k.sb("cvt%d" % i, [128, 6144], BF16) for i in range(2)])
            k.s.bg_keys = set((c_.name, None) for c_ in cvt)

            with k.scope():
                LAMr = k.sb("LAMr_s", [64, 10, 32], F32); LAMi = k.sb("LAMi_s", [64, 10, 32], F32)
                OutW = k.sb("OutW_s", [128, 8, 8, 4, 32], BF16)
                Cm = k.sb("Cm_s", [128, 8, 4, 32], BF16)
                KW = k.sb("KW_s", [128, 8, 8, 32], BF16)
                SWc = k.sb("SWc_s", [128, 8, 8, 128], BF16)
                _lds = []
                for t_, d_ in ((LAMr, sc_LAMr), (LAMi, sc_LAMi), (OutW, sc_OutW), (Cm, sc_Cm), (KW, sc_KW), (SWc, sc_SWc)):
                    _lds.append(k.s.add("sp", (lambda o_, i_: (lambda h: h.dma_start(out=o_, in_=i_)))(t_[:], d_), [d_], [t_], dma=True, semkey="spill_ld"))
                for o_ in _lds:
                    o_.val = _lds[-1].val

                def T(name, shape, dt=F32):
                    return k.sb(name, shape, dt)

                with k.scope():
                    Wu = T("Wu", [128, 8, 8, 4, 32], BF16)
                    wst = [k.sb("wsu%d" % i, [128, 8, 256], F32) for i in range(2)]
                    k.memset(Wu[:], 0.0)
                    for h in range(2):
                        k.dma(wst[h][:], w_in_v[:, :, 4608 + 256 * h:4608 + 256 * h + 256])
                        for c in range(8):
                            k.cp(Wu[:, c, 4 * h:4 * h + 4, :, 0:16], wst[h][:, c, :].rearrange("p (a q x) -> p a q x", a=4, q=4),
                                 eng="dve")
                    pass
                    ranges = [(i * 512, 512) for i in range(4)] + [(NPT, NS)]
                    it = 0
                    for chunk in range(8):
                        for (t0, n) in ranges:
                            pb = psb[it % 4]
                            it += 1
                            for c in range(8):
                                k.mm(pb[:, 0:n], Wu[:, c, chunk, :, :].rearrange("p q x -> p (q x)"), xnT[:, c, t0:t0 + n],
                                     start=(c == 0), stop=(c == 7), ins=[Wu, "xnT"])
                            if it % 2:
                                k.act(uT[:, chunk, t0:t0 + n], pb[:, 0:n], AF.Copy, outs=[("uT", chunk)])
                            else:
                                k.cp(uT[:, chunk, t0:t0 + n], pb[:, 0:n], outs=[("uT", chunk)])
                HP = T("HP", [128, 32, 256], BF16)
                k.memset(HP[:, :, 0:1], 0.0)
                with k.scope():
                    SS = [T("SS0", [64, 64, 2, 32])]
                    Hr = T("Hr", [64, 64, 3, 32])
                    Z3 = T("Z3", [64, 3, 32]); k.memset(Z3[:], 0.0)
                    AR2 = T("AR2", [64, 2, 32]); AI2 = T("AI2", [64, 2, 32])
                    k.cp(AR2[:, 0, :], LAMr[:, 8, :]); k.cp(AR2[:, 1, :], LAMr[:, 8, :])
                    k.ts(AI2[:, 0, :], LAMi[:, 8, :], -1.0, None, ALU.mult); k.cp(AI2[:, 1, :], LAMi[:, 8, :])
                    sA = T("sA", [64, 2, 32]); sB = T("sB", [64, 2, 32])
                    fill = 0
                    for r in range(4):
                        S_ = SS[0]
                        for gq in range(4):
                            for ch in range(2):
                                bank = psb[fill % 8]
                                fill += 1
                                for cc in range(4):
                                    chunk = ch * 4 + cc
                                    for ri in range(2):
                                        c0 = (cc * 2 + ri) * 64
                                        for i in range(8):
                                            k.mm(bank[0:64, c0:c0 + 64],
                                                 SWc[32 * gq:32 * gq + 32, chunk, i, ri * 64:(ri + 1) * 64],
                                                 uT[32 * gq:32 * gq + 32, chunk, 512 * r + i:512 * r + 512:8],
                                                 start=(i == 0), stop=(i == 7), tile_position=(32 * gq, 0),
                                                 ins=[SWc, ("uT", chunk)])
                                g0 = gq + 16 * ch
                                src = bank[0:64, :].rearrange("p (c r k) -> p k r c", c=4, r=2)
                                if fill % 2:
                                    k.act(S_[:, :, :, g0:g0 + 13:4], src, AF.Copy, outs=[(S_.name, fill % 8)])
                                else:
                                    k.cp(S_[:, :, :, g0:g0 + 13:4], src, outs=[(S_.name, fill % 8)])
                        for kk in range(64):
                            prev = Z3[:] if (r == 0 and kk == 0) else (Hr[:, 63, :, :] if kk == 0 else Hr[:, kk - 1, :, :])
                            k.tt(sA[:], prev[:, 0:2, :], AR2[:], ALU.mult)
                            k.tt(sB[:], prev[:, 1:3, :], AI2[:], ALU.mult)
                            k.tt(sA[:], sA[:], sB[:], ALU.add)
                            k.tt(Hr[:, kk, 0:2, :], sA[:], S_[:, kk, :, :], ALU.add, ins=[sA, S_])
                            k.cp(Hr[:, kk, 2, :], Hr[:, kk, 0, :])
                            if kk % 32 == 31:
                                conv_step(Hr)
                        ncol = 64 if r < 3 else 63
                        k.cp(HP[0:64, :, 64 * r + 1:64 * r + 1 + ncol], Hr[:, 0:ncol, 0, :].rearrange("p k g -> p g k"))
                        k.cp(HP[64:128, :, 64 * r + 1:64 * r + 1 + ncol], Hr[:, 0:ncol, 1, :].rearrange("p k g -> p g k"))
                    k.dma(ssm_p, Hr[:, 63, 0:2, :], q="pool")
                HPs = T("HPs", [128, 32, 16], BF16)
                with k.scope():
                    SSs = T("SSs", [64, 2, 16, 32])
                    for gq in range(4):
                        for ri in range(2):
                            bank = psb[gq * 2 + ri]
                            for chunk in range(8):
                                for i in range(4):
                                    k.mm(bank[0:64, chunk * 16:(chunk + 1) * 16],
                                         SWc[32 * gq:32 * gq + 32, chunk, i, ri * 64:(ri + 1) * 64],
                                         uT[32 * gq:32 * gq + 32, chunk, NPT + i:NT:4],
                                         start=(i == 0), stop=(i == 3), tile_position=(32 * gq, 0),
                                         ins=[SWc, ("uT", chunk)])
                            k.cp(SSs[:, ri, :, gq:32:4], bank[0:64, 0:128].rearrange("p (c s) -> p s c", c=8))
                    h0P = T("h0P", [64, 2, 16, 32])
                    k.dma(h0P[:], s_h0)

                    def bs(ap2):
                        return ap2.unsqueeze(1).to_broadcast([64, 16, 32])

                    w1 = T("w1", [64, 16, 32]); w2 = T("w2", [64, 16, 32]); Wr_ = T("Wr_", [64, 16, 32]); Wi_ = T("Wi_", [64, 16, 32])
                    nsP = T("nsP", [64, 2, 16, 32])
                    ar8, ai8, l4r, l4i = bs(LAMr[:, 8, :]), bs(LAMi[:, 8, :]), bs(LAMr[:, 9, :]), bs(LAMi[:, 9, :])
                    k.tt(w1[:], h0P[:, 0], ar8, ALU.mult); k.tt(w2[:], h0P[:, 1], ai8, ALU.mult)
                    k.tt(w1[:], w1[:], w2[:], ALU.subtract); k.tt(Wr_[:], w1[:], SSs[:, 0], ALU.add)
                    k.tt(w1[:], h0P[:, 1], ar8, ALU.mult); k.tt(w2[:], h0P[:, 0], ai8, ALU.mult)
                    k.tt(w1[:], w1[:], w2[:], ALU.add); k.tt(Wi_[:], w1[:], SSs[:, 1], ALU.add)
                    k.tt(w1[:], Wr_[:], l4r, ALU.mult); k.tt(w2[:], Wi_[:], l4i, ALU.mult)
                    k.tt(nsP[:, 0], w1[:], w2[:], ALU.subtract)
                    k.tt(w1[:], Wi_[:], l4r, ALU.mult); k.tt(w2[:], Wr_[:], l4i, ALU.mult)
                    k.tt(nsP[:, 1], w1[:], w2[:], ALU.add)
                    k.dma(ssm_s, nsP[:], q="pool")
                    k.cp(HPs[0:64], h0P[:, 0].rearrange("p s g -> p g s"))
                    k.cp(HPs[64:128], h0P[:, 1].rearrange("p s g -> p g s"))
                KWbd = T("KWbd", [128, 8, 8, 128], BF16)
                k.memset(KWbd[:], 0.0)
                for gq in range(4):
                    k.cp(KWbd[32 * gq:32 * gq + 32, :, :, 32 * gq:32 * gq + 32], KW[32 * gq:32 * gq + 32, :, :, :])
                for chunk in range(8):
                    for j in range(8):
                        bank = psb[j]
                        for i in range(j + 1):
                            k.mm(bank[:, 0:256], KWbd[:, chunk, j - i, :], uT[:, chunk, i:NPT:8], start=(i == 0), stop=False,
                                 ins=[KWbd, ("uT", chunk)])
                            if j < 4:
                                k.mm(bank[:, 256:272], KWbd[:, chunk, j - i, :], uT[:, chunk, NPT + i:NT:4], start=False, stop=False,
                                     ins=[KWbd, ("uT", chunk)])
                    for gq in range(4):
                        g = chunk * 4 + gq
                        for j in range(8):
                            bank = psb[j]
                            k.mm(bank[32 * gq:32 * gq + 32, 0:256], OutW[:, chunk, j, gq, :], HP[:, g, :],
                                 start=False, stop=True, tile_position=(0, 32 * gq))
                            if j < 4:
                                k.mm(bank[32 * gq:32 * gq + 32, 256:272], OutW[:, chunk, j, gq, :], HPs[:, g, :],
                                     start=False, stop=True, tile_position=(0, 32 * gq))
                    for j in range(8):
                        k.act(uT[:, chunk, j:NPT:8], psb[j][:, 0:256], AF.Gelu_apprx_tanh, outs=[("uT", chunk)])
                        if j < 4:
                            k.act(uT[:, chunk, NPT + j:NT:4], psb[j][:, 256:272], AF.Gelu_apprx_tanh, outs=[("uT", chunk)])
                    conv_step(("uT", chunk))
                if dbg:
                    k.dma(dbg_gy, uT[:], q="pool")


            mergedT = k.sb("mergedT", [128, 8, NT], BF16)
            ranges = [(i * 512, 512) for i in range(4)] + [(NPT, NS)]
            wab_v = w_ab.rearrange("(c p) n -> p c n", p=128)
            wglu_v = w_glu.rearrange("(ch q c) n -> q c ch n", q=4, c=16)
            ring = [0]

            def nbank():
                ring[0] += 1
                return psb[ring[0] % 8]

            with k.scope():
                sAB = k.sb("sAB", [128, 4, 128], F32)
                sG = k.sb("sG", [128, 8, 2, 128], F32)
                sL = k.sb("sL", [128, 8, 2, 128], F32)
                k.memset(sL[:], 0.0)
                mw = [(k.sb("mAB%d" % i, [128, 4, 128], BF16), k.sb("mG%d" % i, [128, 8, 2, 128], BF16),
                       k.sb("mL%d" % i, [128, 8, 2, 128], BF16)) for i in range(2)]
                mt = [[k.sb("mt%d_%d" % (i, j), [128, 512], F32) for j in range(5)] for i in range(2)]
                mi = 0
                for fc in range(8):
                    AB, G_, L_ = mw[fc % 2]
                    k.dma(sAB[:], wab_v[:, :, fc * 128:(fc + 1) * 128])
                    for j in range(2):
                        k.dma(sG[:, :, j, :], w_in_v[:, :, 5120 + j * 1024 + fc * 128:5120 + j * 1024 + (fc + 1) * 128], outs=[("sG", j)])
                        for gq in range(4):
                            k.dma(sL[32 * gq:32 * gq + 16, :, j, :], wglu_v[gq, :, :, j * 1024 + fc * 128:j * 1024 + (fc + 1) * 128],
                                  outs=[("sL", j * 4 + gq)])
                    k.cp(AB[:], sAB[:])
                    k.act(G_[:], sG[:], AF.Copy)
                    k.cp(L_[:], sL[:])
                    for (t0, n) in ranges:
                        tl = slice(t0, t0 + n)
                        s1, s2, s3, m1, m2 = mt[mi % 2]
                        mi += 1
                        p_ao, p_ga, p_gs, p_la, p_lb = nbank(), nbank(), nbank(), nbank(), nbank()
                        for c in range(4):
                            k.mm(p_ao[:, 0:n], AB[:, c, :], attnT[:, c, tl], start=(c == 0), stop=(c == 3))
                        for j, pb in ((0, p_ga), (1, p_gs)):
                            for c in range(8):
                                k.mm(pb[:, 0:n], G_[:, c, j, :], xnT[:, c, tl], start=(c == 0), stop=(c == 7), ins=[G_, "xnT"])
                        for j, pb in ((0, p_la), (1, p_lb)):
                            for c in range(8):
                                k.mm(pb[:, 0:n], L_[:, c, j, :], uT[:, c, tl], start=(c == 0), stop=(c == 7), ins=[L_, "uT"])
                        k.act(s1[:, 0:n], p_ga[:, 0:n], AF.Sigmoid)
                        k.act(s2[:, 0:n], p_gs[:, 0:n], AF.Sigmoid)
                        k.act(s3[:, 0:n], p_lb[:, 0:n], AF.Sigmoid)
                        k.tt(m1[:, 0:n], p_ao[:, 0:n], s1[:, 0:n], ALU.mult)
                        k.tt(m2[:, 0:n], p_la[:, 0:n], s3[:, 0:n], ALU.mult)
                        k.tt(m2[:, 0:n], m2[:, 0:n], s2[:, 0:n], ALU.mult)
                        k.tt(mergedT[:, fc, tl], m1[:, 0:n], m2[:, 0:n], ALU.add, outs=[("mergedT", fc)])
                        if mi % 2 == 0:
                            conv_step(("mergedT", fc))
            while cvs["e"] <= 32:
                conv_step()
            with k.scope():
                wos = [k.sb("wos%d" % i, [128, 8, 256], F32) for i in range(2)]
                Wo = k.sb("Wo", [128, 8, 1024], BF16)
                wout_v = w_out.rearrange("(c p) n -> p c n", p=128)
                for j in range(4):
                    k.dma(wos[j % 2][:], wout_v[:, :, j * 256:(j + 1) * 256])
                    k.act(Wo[:, :, j * 256:(j + 1) * 256], wos[j % 2][:], AF.Copy, outs=[("Wo", j)])
                gffrow = k.sb("gffrow", [128, D], F32)
                k.dma(gffrow[:], gffrow_d)
                hnb = [k.sb("hnb%d" % i, [128, D], BF16) for i in range(2)]
                gff = k.sb("gff", [128, 8], F32)
                k.dma(gff[:], g_ffn)
                xt = [k.sb("xu%d" % i, [128, D], F32) for i in range(2)]
                ht = [k.sb("ht%d" % i, [128, D], F32) for i in range(2)]
                hb = [k.sb("hb%d" % i, [128, D], BF16) for i in range(2)]
                junk = k.sb("junk2", [128, D], BF16)
                ss = [k.sb("su%d" % i, [128, 1], F32) for i in range(2)]
                rs = [k.sb("ru%d" % i, [128, 1], F32) for i in range(2)]
                def mk_wo(ti, t0, n):
                    b = ti % 2
                    tl = slice(t0, t0 + n)
                    pbs = (nbank(), nbank())
                    ptb = nbank()[:].bitcast(BF16)

                    def w1():
                        k.dma(xt[b][0:n, :], x[tl, :])
                        for sl_ in range(2):
                            for c in range(8):
                                k.mm(pbs[sl_][0:n, :], mergedT[:, c, tl], Wo[:, c, sl_ * 512:(sl_ + 1) * 512],
                                     start=(c == 0), stop=(c == 7), ins=["mergedT", "Wo"])

                    def w2():
                        for sl_ in range(2):
                            k.tt(ht[b][0:n, sl_ * 512:(sl_ + 1) * 512], pbs[sl_][0:n, :], xt[b][0:n, sl_ * 512:(sl_ + 1) * 512], ALU.add)
                        k.dma(h_scr[tl, :], ht[b][0:n, :], q="pool")
                        k.act(junk[0:n, :], ht[b][0:n, :], AF.Square, accum_out=ss[b][0:n, :])
                        k.ts(rs[b][0:n, :], ss[b][0:n, :], 1.0 / D, 1e-6, ALU.mult, ALU.add)
                        k.act(rs[b][0:n, :], rs[b][0:n, :], AF.Sqrt)
                        k.recip(rs[b][0:n, :], rs[b][0:n, :])
                        k.act(hb[b][0:n, :], ht[b][0:n, :], AF.Copy, scale=rs[b][0:n, :])
                        k.tt(hnb[b][0:n, :], hb[b][0:n, :], gffrow[0:n, :], ALU.mult)
                        k.dma(hn_scr[tl, :], hnb[b][0:n, :], q="pool")
                        for c in range(8):
                            k.tr(ptb[:, c * 128:c * 128 + n], hb[b][0:n, c * 128:(c + 1) * 128], ident[0:n, 0:n])

                    def w3():
                        for c in range(8):
                            if c % 2 == 0:
                                k.act(xnT[:, c, tl], ptb[:, c * 128:c * 128 + n], AF.Copy, scale=gff[:, c:c + 1], outs=[("xnT", ti)])
                            else:
                                k.ts(xnT[:, c, tl], ptb[:, c * 128:c * 128 + n], gff[:, c:c + 1], None, ALU.mult, outs=[("xnT", ti)])

                    return (w1, w2, w3)

                wos_ = [mk_wo(ti, t0, n) for ti, (t0, n) in enumerate(tiles)]
                for step in range(len(wos_) + 2):
                    for si_, sk_ in enumerate((0, 1, 2)):
                        if 0 <= step - sk_ < len(wos_):
                            wos_[step - sk_][si_]()
        hnT = xnT
        with k.scope():
            gfin = k.sb("gfin", [128, D], F32)
            k.dma(gfin[:], gfin_d)
            A1s = k.sb("A1s", [128, 17, 32], F32); A2s = k.sb("A2s", [128, 17, 32], F32)
            WW = k.sb("WW", [128, 17, 2], F32)
            k.memset(A1s[:], 0.0); k.memset(A2s[:], 0.0); k.memset(WW[:], 0.0)
            with k.scope():
                rst = k.sb("rst", [128, 8, 36], F32)
                Wr = k.sb("Wr", [128, 8, 36], BF16)
                k.dma(rst[:, :, 0:4], w_rg.rearrange("(c p) n -> p c n", p=128))
                k.dma(rst[:, :, 4:36], w_re.rearrange("(c p) n -> p c n", p=128))
                k.cp(Wr[:], rst[:])
                rbias = k.sb("rbias", [128, 36], F32)
                k.dma(rbias[:], rb_d)
                LG = k.sb("LG", [128, 17, 36], F32)
                k.memset(LG[:], 0.0)
                for ti, (t0, n) in enumerate(tiles):
                    pb = nbank()
                    for c in range(8):
                        k.mm(pb[0:n, 0:36], hnT[:, c, t0:t0 + n], Wr[:, c, :], start=(c == 0), stop=(c == 7), ins=["xnT", Wr])
                    k.tt(LG[0:n, ti, :], pb[0:n, 0:36], rbias[0:n, :], ALU.add, outs=[("LG", ti)])

                def R(name, shape):
                    return k.sb(name, shape, F32)

                def red(out, in_, op):
                    return k.s.add("dve", lambda h: h.tensor_reduce(out, in_, AX.X, op), [in_], [out])

                def b3(ap2, n3):
                    return ap2.unsqueeze(2).to_broadcast([128, 17, n3])
                lg4 = LG[:, :, 0:4]
                le4 = LG[:, :, 4:36].rearrange("p t (g e) -> p t g e", e=8)
                mx = R("r_mx", [128, 17]); ohb = R("r_oh", [128, 17, 4]); e4b = R("r_e4", [128, 17, 4])
                se = R("r_se", [128, 17]); pg = R("r_pg", [128, 17])
                red(mx[:], lg4, ALU.max)
                k.tt(ohb[:], lg4, b3(mx[:], 4), ALU.is_equal)
                k.tt(e4b[:], lg4, b3(mx[:], 4), ALU.subtract)
                k.act(e4b[:], e4b[:], AF.Exp)
                red(se[:], e4b[:], ALU.add)
                k.recip(pg[:], se[:])
                legb = R("r_leg", [128, 17, 8]); tmp8 = R("r_t8", [128, 17, 8])
                for g_ in range(4):
                    ohg = ohb[:, :, g_:g_ + 1].to_broadcast([128, 17, 8])
                    if g_ == 0:
                        k.tt(legb[:], le4[:, :, 0, :], ohg, ALU.mult)
                    else:
                        k.tt(tmp8[:], le4[:, :, g_, :], ohg, ALU.mult)
                        k.tt(legb[:], legb[:], tmp8[:], ALU.add)
                v1 = R("r_v1", [128, 17]); v2 = R("r_v2", [128, 17]); m1b = R("r_m1", [128, 17, 8]); m2b = R("r_m2", [128, 17, 8])
                red(v1[:], legb[:], ALU.max)
                k.tt(m1b[:], legb[:], b3(v1[:], 8), ALU.is_equal)
                k.stt(tmp8[:], m1b[:], -1.0e30, legb[:], ALU.mult, ALU.add)
                red(v2[:], tmp8[:], ALU.max)
                k.tt(m2b[:], tmp8[:], b3(v2[:], 8), ALU.is_equal)
                ex = R("r_ex", [128, 17]); w1_ = R("r_w1", [128, 17]); w2_ = R("r_w2", [128, 17])
                k.tt(ex[:], v2[:], v1[:], ALU.subtract)
                k.act(ex[:], ex[:], AF.Exp)
                k.ts(w1_[:], ex[:], 1.0, None, ALU.add)
                k.recip(w1_[:], w1_[:])
                k.tt(w2_[:], ex[:], w1_[:], ALU.mult)
                k.tt(WW[:, :, 0], w1_[:], pg[:], ALU.mult)
                k.tt(WW[:, :, 1], w2_[:], pg[:], ALU.mult)
                for g_ in range(4):
                    ohg = ohb[:, :, g_:g_ + 1].to_broadcast([128, 17, 8])
                    k.tt(A1s[:, :, g_ * 8:(g_ + 1) * 8], m1b[:], ohg, ALU.mult)
                    k.tt(A2s[:, :, g_ * 8:(g_ + 1) * 8], m2b[:], ohg, ALU.mult)
                k.memset(A1s[NS:128, 16, :], 0.0)
                k.memset(A2s[NS:128, 16, :], 0.0)
                k.memset(WW[NS:128, 16, :], 0.0)
            NB = 49
            SLi = [k.sb("SLi%d" % i, [128, 17], I32) for i in range(2)]
            BEi = k.sb("BEi", [128, NB], I32)
            with k.scope():
                Ab = k.sb("Ab", [128, 17, 32], BF16)
                Asum = k.sb("Asum", [128, 17, 32], F32)
                k.tt(Asum[:], A1s[:], A2s[:], ALU.add)
                k.cp(Ab[:], Asum[:])
                onesb = k.sb("onesb", [128, 128], BF16); k.memset(onesb[:], 1.0)
                trif = k.sb("trif", [128, 128], F32); trib = k.sb("trib", [128, 128], BF16)
                k.dma(trif[:], tri_d); k.cp(trib[:], trif[:])
                CS = k.sb("CS", [128, 17, 32], F32); RK = k.sb("RK", [128, 17, 32], F32); OFF = k.sb("OFF", [128, 17, 32], F32)
                Abf = Ab[:].rearrange("p t e -> p (t e)")
                for (lhs, dst) in ((onesb, CS), (trib, RK)):
                    p0, p1 = nbank(), nbank()
                    k.mm(p0[:, 0:512], lhs[:], Abf[:, 0:512])
                    k.mm(p1[:, 0:32], lhs[:], Abf[:, 512:544])
                    dflat = dst[:].rearrange("p t e -> p (t e)")
                    k.cp(dflat[:, 0:512], p0[:, 0:512])
                    k.cp(dflat[:, 512:544], p1[:, 0:32])
                k.memset(OFF[:, 0, :], 0.0)
                for ti in range(1, 17):
                    k.tt(OFF[:, ti, :], OFF[:, ti - 1, :], CS[:, ti - 1, :], ALU.add)
                CNT = k.sb("CNT", [128, 32], F32); NBK = k.sb("NBK", [128, 32], F32); NBI = k.sb("NBI", [128, 32], I32)
                PEND = k.sb("PEND", [128, 32], F32); PST = k.sb("PST", [128, 32], F32); ONE32 = k.sb("ONE32", [128, 32], F32)
                k.memset(ONE32[:], 1.0)
                k.tt(CNT[:], OFF[:, 16, :], CS[:, 16, :], ALU.add)
                k.ts(NBK[:], CNT[:], 1.0 / 256.0, 255.0 / 256.0 - 0.498046875, ALU.mult, ALU.add)
                k.cp(NBI[:], NBK[:])
                k.cp(NBK[:], NBI[:])
                k.s.add("dve", lambda h: h.tensor_tensor_scan(PEND[:], ONE32[:], NBK[:], 0.0, ALU.mult, ALU.add), [ONE32, NBK], [PEND])
                k.tt(PST[:], PEND[:], NBK[:], ALU.subtract)
                SLT = k.sb("SLT", [128, 17, 32], F32)
                k.tt(SLT[:], OFF[:], RK[:], ALU.add)
                k.ts(PST[:], PST[:], 256.0, None, ALU.mult)
                k.tt(SLT[:], SLT[:], PST[:].unsqueeze(1).to_broadcast([128, 17, 32]), ALU.add)
                SL = [k.sb("SL%d" % i, [128, 17], F32) for i in range(2)]
                for i_, Ax in enumerate((A1s, A2s)):
                    k.tt(Asum[:], Ax[:], SLT[:], ALU.mult)
                    k.s.add("dve", (lambda o_, i2: (lambda h: h.tensor_reduce(o_, i2, AX.X, ALU.add)))(SL[i_][:], Asum[:]), [Asum], [SL[i_]])
                    k.cp(SLi[i_][:], SL[i_][:])
                BIX = k.sb("BIX", [128, NB, 32], F32)
                k.dma(BIX[:], bidx_d)
                k.tt(BIX[:], PEND[:].unsqueeze(1).to_broadcast([128, NB, 32]), BIX[:], ALU.is_le)
                BE = k.sb("BE", [128, NB], F32)
                k.s.add("dve", lambda h: h.tensor_reduce(BE[:], BIX[:], AX.X, ALU.add), [BIX], [BE])
                pcol = k.sb("pcol", [128, 1], F32); k.dma(pcol[:], pcol_d)
                k.ts(BE[:], BE[:], 128.0, pcol[:, 0:1], ALU.mult, ALU.add)
                k.cp(BEi[:], BE[:])
            with k.scope():
                hld = [k.sb("hld%d" % i, [128, D], BF16) for i in range(4)]
                for ti, (t0, n) in enumerate(tiles):
                    hb_ = hld[ti % 4]
                    k.dma(hb_[0:n, :], hn_scr[t0:t0 + n, :])
                    for i_ in range(2):
                        k.s.add("pool", (lambda src, idx: (lambda h: h.indirect_dma_start(
                            out=xs_scr[:, :], out_offset=bass.IndirectOffsetOnAxis(ap=idx, axis=0), in_=src, in_offset=None)))(
                            hb_[0:n, :], SLi[i_][0:n, ti:ti + 1]), [hb_, SLi[i_]], [("xs_scr", 2 * ti + i_)], dma=True, semkey=(hb_.name, i_))
                wbe = [k.sb("wbe%d" % i, [128, 6144], BF16) for i in range(4)]
                for i in range(4):
                    k.memset(wbe[i][:], 0.0)
                xsb = [k.sb("xsb%d" % i, [128, 2, D], BF16) for i in range(3)]
                xsT = [k.sb("xsT%d" % i, [128, 8, 256], BF16) for i in range(2)]
                hmT = [k.sb("hmT%d" % i, [128, 2, 256], BF16) for i in range(2)]
                sgt = [k.sb("sgt%d" % i, [128, 256], F32) for i in range(2)]
                ybt = [k.sb("ybt%d" % i, [128, D], BF16) for i in range(2)]
                _bcc = {}

                def _bc(h):
                    if "r" not in _bcc:
                        _bcc["r"] = h.to_reg(4095)
                    return _bcc["r"]

                def mk_blk(bidx):
                    wb_ = wbe[bidx % 4]
                    Wg_ = wb_[:, 0:2048].rearrange("p (c n) -> p c n", c=8)
                    Wu_ = wb_[:, 2048:4096].rearrange("p (c n) -> p c n", c=8)
                    Wd_ = wb_[:, 4096:6144].rearrange("p (c n) -> p c n", c=2)
                    xb_, xT_, hm_ = xsb[bidx % 3], xsT[bidx % 2], hmT[bidx % 2]

                    def bl():
                        k.dma(xb_[:], xs_scr[bidx * 256:(bidx + 1) * 256, :].rearrange("(s p) d -> p s d", p=128), ins=["xs_scr"])

                    def bg():
                        k.s.add("pool", (lambda idx_: (lambda h: h.indirect_dma_start(
                            out=wb_[:, :], out_offset=None, in_=w_bf[:, :], in_offset=bass.IndirectOffsetOnAxis(ap=idx_, axis=0),
                            bounds_check=_bc(h), oob_is_err=False)))(BEi[:, bidx:bidx + 1]), [BEi, "w_bf"], [wb_], dma=True, semkey=wb_)

                    def b0():
                        for sub in range(2):
                            ptb = nbank()[:].bitcast(BF16)
                            for c in range(8):
                                k.tr(ptb[:, c * 128:(c + 1) * 128], xb_[:, sub, c * 128:(c + 1) * 128], ident[:])
                            if sub == 0:
                                k.act(xT_[:, :, 0:128], ptb.rearrange("p (c x) -> p c x", c=8), AF.Copy)
                            else:
                                k.cp(xT_[:, :, 128:256], ptb.rearrange("p (c x) -> p c x", c=8))

                    def b1():
                        for fcx in range(2):
                            pg_, pu_ = nbank(), nbank()
                            for c in range(8):
                                k.mm(pg_[:, 0:256], Wg_[:, c, fcx * 128:(fcx + 1) * 128], xT_[:, c, :], start=(c == 0), stop=(c == 7))
                            for c in range(8):
                                k.mm(pu_[:, 0:256], Wu_[:, c, fcx * 128:(fcx + 1) * 128], xT_[:, c, :], start=(c == 0), stop=(c == 7))
                            sg_ = sgt[fcx]
                            k.act(sg_[:], pg_[:, 0:256], AF.Silu)
                            k.tt(hm_[:, fcx, :], sg_[:], pu_[:, 0:256], ALU.mult)

                    def b2():
                        for sub in range(2):
                            yb_ = ybt[sub]
                            for sl_ in range(2):
                                py = nbank()
                                for fcx in range(2):
                                    k.mm(py[:, :], hm_[:, fcx, sub * 128:(sub + 1) * 128], Wd_[:, fcx, sl_ * 512:(sl_ + 1) * 512],
                                         start=(fcx == 0), stop=(fcx == 1))
                                if sub == 0:
                                    k.act(yb_[:, sl_ * 512:(sl_ + 1) * 512], py[:, :], AF.Copy)
                                else:
                                    k.cp(yb_[:, sl_ * 512:(sl_ + 1) * 512], py[:, :])
                            k.dma(ys_scr[bidx * 256 + sub * 128:bidx * 256 + (sub + 1) * 128, :], yb_[:], outs=["ys_scr"],
                                  q="act")
                    return (bl, bg, b0, b1, b2)

                blks = [mk_blk(b_) for b_ in range(NB)]
                for step in range(NB + 4):
                    for si_, sk_ in enumerate((0, 1, 2, 3, 4)):
                        if 0 <= step - sk_ < NB:
                            blks[step - sk_][si_]()
            with k.scope():
                ygt = [k.sb("ygt%d" % i, [128, D], BF16) for i in range(4)]
                hfin = [k.sb("hfin%d" % i, [128, D], F32) for i in range(2)]
                yo = [k.sb("yo%d" % i, [128, D], F32) for i in range(2)]
                junk = k.sb("junk3", [128, D], BF16)
                fs = [k.sb("fs%d" % i, [128, 1], F32) for i in range(2)]
                for ti, (t0, n) in enumerate(tiles):
                    b = ti % 2
                    k.dma(hfin[b][0:n, :], h_scr[t0:t0 + n, :])
                    for i_ in range(2):
                        yg = ygt[(2 * ti + i_) % 4]
                        k.s.add("pool", (lambda dst, idx_: (lambda h: h.indirect_dma_start(
                            out=dst, out_offset=None, in_=ys_scr[:, :], in_offset=bass.IndirectOffsetOnAxis(ap=idx_, axis=0))))(
                            yg[0:n, :], SLi[i_][0:n, ti:ti + 1]), ["ys_scr", SLi[i_]], [yg], dma=True, semkey=yg)
                        k.stt(hfin[b][0:n, :], yg[0:n, :], WW[0:n, ti, i_:i_ + 1], hfin[b][0:n, :], ALU.mult, ALU.add)
                    k.act(junk[0:n, :], hfin[b][0:n, :], AF.Square, accum_out=fs[b][0:n, :])
                    k.ts(fs[b][0:n, :], fs[b][0:n, :], 1.0 / D, 1e-6, ALU.mult, ALU.add)
                    k.act(fs[b][0:n, :], fs[b][0:n, :], AF.Sqrt)
                    k.recip(fs[b][0:n, :], fs[b][0:n, :])
                    k.stt(yo[b][0:n, :], hfin[b][0:n, :], fs[b][0:n, :], gfin[0:n, :], ALU.mult, ALU.mult)
                    k.dma(y_out[t0:t0 + n, :], yo[b][0:n, :], q="act")

        k.s.emit()
    return nc


_NC = {}


def _host_consts():
    pos = np.concatenate([np.arange(NPT), np.tile(2048 + np.arange(4), 16)]).astype(np.float32)
    half = 8
    inv = (500000.0 ** (-(np.arange(half, dtype=np.float32) / half))).astype(np.float32)
    ang = pos[:, None] * inv[None, :]
    rope = np.concatenate([np.cos(ang), np.sin(ang)], axis=1).astype(np.float32)
    diag = np.zeros((128, 32), np.float32)
    for gq in range(4):
        for c in range(16):
            diag[32 * gq + c, c] = 1.0
    NEG = -30000.0
    masks = np.zeros((128, 384), np.float32)
    kk = np.arange(128)[:, None]
    qq = np.arange(128)[None, :]
    masks[:, 0:128] = np.where(kk <= qq, 0.0, NEG)
    masks[:, 128:256] = np.where(kk >= qq, 0.0, NEG)
    k64 = np.arange(128)[:, None]
    q64 = np.arange(64)[None, :]
    same = (k64 // 4 == q64 // 4) & (k64 < 64)
    masks[:, 256:320] = np.where(same & (k64 % 4 <= q64 % 4), 0.0, NEG)
    masks[:, 320:384] = np.where(k64 == q64, 0.0, NEG)
    return rope, diag, masks


def kernel(_dbg=False, **inp):
    if _dbg not in _NC:
        _NC[_dbg] = build(_dbg)
    nc = _NC[_dbg]
    f = lambda a: np.ascontiguousarray(np.asarray(a, dtype=np.float32))
    rope, diag, masks = _host_consts()
    ident = np.eye(128, dtype=np.float32)
    w_in = f(inp["w_in"][0])
    g_attn = f(np.asarray(inp["g_attn_norm"][0]).reshape(8, 128).T)
    xp = np.asarray(inp["x_prompt"])
    xs = np.asarray(inp["x_sample"])
    s_are = f(np.asarray(inp["ssm_a_re"][0]).T)
    s_aim = f(np.asarray(inp["ssm_a_im"][0]).T)
    s_ldt = f(np.broadcast_to(np.asarray(inp["ssm_log_dt"][0])[None, :], (64, 32)))
    s_bre = f(np.transpose(np.asarray(inp["ssm_b_re"][0]), (1, 0, 2)))
    s_bim = f(np.transpose(np.asarray(inp["ssm_b_im"][0]), (1, 0, 2)))
    s_cre = f(np.transpose(np.asarray(inp["ssm_c_re"][0]), (2, 0, 1)))
    s_cim = f(np.transpose(np.asarray(inp["ssm_c_im"][0]), (2, 0, 1)))
    dd = np.asarray(inp["ssm_d"][0])
    s_d = np.zeros((128, 8), np.float32)
    for g in range(32):
        s_d[32 * (g % 4):32 * (g % 4) + 16, g // 4] = dd[g]
    st_all = np.asarray(inp["state_ssm"][0])
    common = {"rope": rope, "ident_in": ident, "diag_in": diag, "g_attn_in": g_attn, "w_in": w_in,
              "s_are": s_are, "s_aim": s_aim, "s_ldt": s_ldt, "s_bre": s_bre, "s_bim": s_bim,
              "s_cre": s_cre, "s_cim": s_cim, "s_d": s_d, "masks_in": masks}
    weg = np.asarray(inp["w_exp_gate"][0]).reshape(32, 8, 128, 256).transpose(0, 2, 1, 3).reshape(32, 128, 2048)
    weu = np.asarray(inp["w_exp_up"][0]).reshape(32, 8, 128, 256).transpose(0, 2, 1, 3).reshape(32, 128, 2048)
    wed = np.asarray(inp["w_exp_down"][0]).reshape(32, 2, 128, 1024).transpose(0, 2, 1, 3).reshape(32, 128, 2048)
    w_all = f(np.concatenate([weg, weu, wed], axis=2).reshape(4096, 6144))
    tri = (np.arange(128)[:, None] < np.arange(128)[None, :]).astype(np.float32)
    bidx = f(np.broadcast_to(np.arange(49, dtype=np.float32)[None, :, None], (128, 49, 32)))
    pcol = np.arange(128, dtype=np.float32).reshape(128, 1)
    common.update({"w_ab": f(inp["w_attn_branch"][0]), "w_glu": f(inp["w_glu"][0]), "w_out": f(inp["w_out"][0]),
                   "g_ffn_in": f(np.asarray(inp["g_ffn_norm"][0]).reshape(8, 128).T),
                   "w_rg": f(inp["w_router_group"][0]), "w_re": f(inp["w_router_expert"][0]),
                   "rbias_in": f(np.broadcast_to(np.concatenate([np.asarray(inp["b_router_group"][0]), np.asarray(inp["b_router_expert"][0])])[None, :], (128, 36))),
                   "gfin_in": f(np.broadcast_to(np.asarray(inp["g_final"])[None, :], (128, 1024))),
                   "w_all": w_all, "tri_in": tri, "bidx_in": bidx, "pcol_in": pcol,
                   "gffrow_in": f(np.broadcast_to(np.asarray(inp["g_ffn_norm"][0])[None, :], (128, 1024)))})
    c128 = np.asarray(inp["cache_kv_w128"][0])
    c512 = np.asarray(inp["cache_kv_w512"][0])
    c2048 = np.asarray(inp["cache_kv_w2048"][0])
    in_maps = []
    for c in range(8):
        xc = np.concatenate([xp[c], xs[16 * c:16 * c + 16].reshape(64, D)], axis=0)
        h0 = f(np.transpose(st_all[16 * c:16 * c + 16], (2, 3, 0, 1)))
        m = dict(common)
        sl = slice(16 * c, 16 * c + 16)
        m.update({"x": f(xc), "s_h0": h0,
                  "cache0": f(c128[sl]),
                  "cache1": f(c512[sl].reshape(16, 128, 4, 2, 8, 64)),
                  "cache2": f(c2048[sl].reshape(16, 128, 16, 2, 8, 64)[:, :, 0:4])})
        in_maps.append(m)
    res = run_bass_kernel_spmd(nc, in_maps, core_ids=list(range(8)))
    R = res.results
    if _dbg:
        kernel.dbg = R
    outs = []
    y_prompt = np.stack([R[c]["y_out"][0:NPT] for c in range(8)], axis=0)
    y_sample = np.concatenate([R[c]["y_out"][NPT:NT].reshape(16, 4, D) for c in range(8)], axis=0)
    outs += [y_prompt, y_sample]
    for g in range(3):
        outs.append(np.stack([R[c]["kvp%d" % g] for c in range(8)], axis=0)[None])
        outs.append(np.concatenate([R[c]["kvs%d" % g] for c in range(8)], axis=0)[None])
    sp = np.stack([np.transpose(R[c]["ssm_p"], (2, 0, 1)) for c in range(8)], axis=0)[None]
    ssv = np.concatenate([np.transpose(R[c]["ssm_s"], (2, 3, 0, 1)) for c in range(8)], axis=0)[None]
    outs.append(np.ascontiguousarray(sp.astype(np.float32)))
    outs.append(np.ascontiguousarray(ssv.astype(np.float32)))
    return tuple(outs)
```
